# Optimizing a Trainium2 kernel written in Bass

```python
import math
import jax
import jax.numpy as jnp
from jax import lax
import numpy as np

D_MODEL = 1024
BATCH = 8
SEQ = 4096
DEPTH = 4

GRID_W = 64
CTX_LEN = 256
N_EVEN = (DEPTH + 1) // 2
N_ODD = DEPTH // 2

GDN_HEAD_DIM = 128
GDN_WIDTH = D_MODEL // 2
GDN_HEADS = GDN_WIDTH // GDN_HEAD_DIM
GDN_CHUNK = 64
SHORT_CONV = 3

HYENA_WIDTH = D_MODEL // 2
HYENA_ORDER = 2
HYENA_EMB = 33
HYENA_HIDDEN = 64
HYENA_DECAY_TARGET = 1e-2
HYENA_FAST_DECAY_PCT = 0.3
HYENA_SLOW_DECAY_PCT = 1.5
HYENA_MIN_DECAY = math.log(HYENA_DECAY_TARGET) / HYENA_SLOW_DECAY_PCT
HYENA_MAX_DECAY = math.log(HYENA_DECAY_TARGET) / HYENA_FAST_DECAY_PCT

N_CONV = 3 * GDN_WIDTH + 3 * HYENA_WIDTH
N_IN = N_CONV + GDN_WIDTH + 4 * GDN_HEADS

DIFF_HEAD_DIM = 64
DIFF_HEADS = D_MODEL // (2 * DIFF_HEAD_DIM)
DIFF_V_DIM = 2 * DIFF_HEAD_DIM
DIFF_QK_WIDTH = DIFF_HEADS * 2 * DIFF_HEAD_DIM
DIFF_V_WIDTH = DIFF_HEADS * DIFF_V_DIM
Q_BLOCK = 128
ROPE_BASE = 10000.0
ROPE_FREQS = DIFF_HEAD_DIM // 4

PEER_HEADS = 8
PEER_KEYS = 128
PEER_TOPK = 16
PEER_QDIM = 256
N_EXPERTS = PEER_KEYS * PEER_KEYS
PEER_TOKEN_BLOCK = 128
NORM_EPS = 1e-6

kernel_name = 'hybrid_gdn_hyena_diffattn_peer_dit'


def rmsnorm(x, g):
    xf = x.astype(jnp.float32)
    y = xf * lax.rsqrt(jnp.mean(xf * xf, axis=-1, keepdims=True) + NORM_EPS)
    return (y * g.astype(jnp.float32)).astype(x.dtype)


def modulate(h, shift, scale):
    return h * (1.0 + scale) + shift


def l2norm(x):
    x = x.astype(jnp.float32)
    return x * lax.rsqrt(jnp.sum(x * x, axis=-1, keepdims=True) + NORM_EPS)


def depthwise_conv(x, w):
    return lax.conv_general_dilated(x, w[:, None, :].astype(x.dtype), window_strides=(1,),
                                    padding=[(SHORT_CONV // 2, SHORT_CONV // 2)],
                                    dimension_numbers=('NWC', 'WIO', 'NWC'),
                                    feature_group_count=x.shape[-1])


def gated_delta_chunked(q, k, v, beta, g, s0):
    f32 = jnp.float32
    B, H, L, dk = q.shape
    dv = v.shape[-1]
    C = GDN_CHUNK
    n = L // C
    q, k, v, beta, g = [t.astype(f32).reshape(B, H, n, C, *t.shape[3:]) for t in (q, k, v, beta, g)]
    G = jnp.cumsum(g, axis=-1)
    causal = jnp.tril(jnp.ones((C, C), bool))
    strict = jnp.tril(jnp.ones((C, C), bool), -1)
    gdiff = G[..., :, None] - G[..., None, :]
    decay = jnp.where(causal, jnp.exp(jnp.where(causal, gdiff, 0.0)), 0.0)
    kk = jnp.einsum('bhncd,bhnjd->bhncj', k, k)
    a_mat = jnp.where(strict, beta[..., :, None] * kk * decay, 0.0)
    eye = jnp.eye(C, dtype=f32)
    rhs = jnp.concatenate([beta[..., None] * v, (beta * jnp.exp(G))[..., None] * k], axis=-1)
    sol = lax.linalg.triangular_solve(eye + a_mat, rhs, left_side=True, lower=True, unit_diagonal=True)
    w_intra, k_cum = sol[..., :dv], sol[..., dv:]
    qk = jnp.einsum('bhncd,bhnjd->bhncj', q, k) * decay
    q_dec = q * jnp.exp(G)[..., None]
    k_dec = k * jnp.exp(G[..., -1:] - G)[..., None]
    chunk_decay = jnp.exp(G[..., -1])

    def step(s, inp):
        w_i, kc_i, qk_i, qd_i, kd_i, cd_i = inp
        w = w_i - jnp.einsum('bhcd,bhde->bhce', kc_i, s)
        o = jnp.einsum('bhcd,bhde->bhce', qd_i, s) + jnp.einsum('bhcj,bhje->bhce', qk_i, w)
        s = s * cd_i[..., None, None] + jnp.einsum('bhcd,bhce->bhde', kd_i, w)
        return s, o

    xs = tuple(jnp.moveaxis(t, 2, 0) for t in (w_intra, k_cum, qk, q_dec, k_dec, chunk_decay))
    s_fin, o = lax.scan(step, s0.astype(f32), xs)
    o = jnp.moveaxis(o, 0, 2).reshape(B, H, L, dv)
    return o, s_fin


def gdn_inputs(qkv, ba, a_log, dt_bias):
    B, L, _ = qkv.shape
    qkv = jnp.transpose(jax.nn.silu(qkv).reshape(B, L, 3, GDN_HEADS, GDN_HEAD_DIM), (2, 0, 3, 1, 4))
    q = l2norm(qkv[0]) * (GDN_HEAD_DIM ** -0.5)
    k = l2norm(qkv[1])
    v = qkv[2]
    ba = ba.astype(jnp.float32).reshape(B, L, 2, 2, GDN_HEADS)
    beta = jax.nn.sigmoid(ba[:, :, 0])
    g = -jnp.exp(a_log.astype(jnp.float32)) * jax.nn.softplus(ba[:, :, 1] + dt_bias.astype(jnp.float32))
    return q, k, v, jnp.transpose(beta, (2, 0, 3, 1)), jnp.transpose(g, (2, 0, 3, 1))


def gdn_bidir(q, k, v, beta, g, s0):
    o_f, s_f = gated_delta_chunked(q, k, v, beta[0], g[0], s0[0])
    fl = lambda t: jnp.flip(t, axis=2)
    o_b, s_b = gated_delta_chunked(fl(q), fl(k), fl(v), fl(beta[1]), fl(g[1]), s0[1])
    return o_f + fl(o_b), jnp.stack([s_f, s_b])


def hyena_filters(L, w1, b1, w2, b2, w3, b3, w4, freq):
    f32 = jnp.float32
    t = jnp.linspace(0.0, 1.0, L, dtype=f32)
    bands = (HYENA_EMB - 1) // 2
    wpos = 2.0 * math.pi * jnp.arange(L, dtype=f32) / L
    fb = jnp.linspace(1e-4, bands - 1, bands, dtype=f32)
    z = jnp.concatenate([t[:, None], jnp.cos(wpos[:, None] * fb), -jnp.sin(wpos[:, None] * fb)], axis=-1)
    h = jnp.sin(freq * (z @ w1 + b1))
    h = jnp.sin(freq * (h @ w2 + b2))
    h = jnp.sin(freq * (h @ w3 + b3))
    h = (h @ w4).astype(f32).reshape(L, HYENA_ORDER, 2, HYENA_WIDTH)
    deltas = jnp.linspace(HYENA_MIN_DECAY, HYENA_MAX_DECAY, HYENA_WIDTH, dtype=f32)
    window = jnp.exp(-t[:, None] * jnp.abs(deltas))
    return h * window[:, None, None, :]


def two_sided_long_conv(u, h_fwd, h_bwd, bias):
    B, L, C = u.shape
    filt2 = jnp.concatenate([h_fwd, jnp.zeros((1, C), jnp.float32), jnp.flip(h_bwd[1:], axis=0)], axis=0)
    uf = jnp.fft.rfft(u.astype(jnp.float32), n=2 * L, axis=1)
    ff = jnp.fft.rfft(filt2, n=2 * L, axis=0)
    y = jnp.fft.irfft(uf * ff[None], n=2 * L, axis=1)[:, :L]
    return (y + u.astype(jnp.float32) * bias.astype(jnp.float32)).astype(u.dtype)


def hyena(xh, filt, bias):
    z = xh[:, :, 2]
    for o in range(HYENA_ORDER):
        z = xh[:, :, o] * two_sided_long_conv(z, filt[:, o, 0], filt[:, o, 1], bias[o])
    return z


def even_mixer(h_lat, h_ctx, need_ctx, w_in, conv_w, a_log, dt_bias, gdn_gain,
               hf_w1, hf_b1, hf_w2, hf_b2, hf_w3, hf_b3, hf_w4, hf_freq, hy_bias, w_out):
    def project(h):
        B, L, _ = h.shape
        p = h @ w_in
        cv = depthwise_conv(p[..., :N_CONV], conv_w)
        return (cv[..., :3 * GDN_WIDTH], cv[..., 3 * GDN_WIDTH:].reshape(B, L, 3, HYENA_WIDTH),
                p[..., N_CONV:N_CONV + GDN_WIDTH], p[..., N_CONV + GDN_WIDTH:])

    def gdn_out(o, z):
        B, H, L, dv = o.shape
        o = jnp.transpose(o, (0, 2, 1, 3)).astype(z.dtype)
        o = rmsnorm(o, gdn_gain) * jax.nn.silu(z.reshape(B, L, H, dv))
        return o.reshape(B, L, H * dv)

    def hyena_branch(xh):
        filt = hyena_filters(xh.shape[1], hf_w1, hf_b1, hf_w2, hf_b2, hf_w3, hf_b3, hf_w4, hf_freq)
        return hyena(xh, filt, hy_bias)

    B = h_ctx.shape[0]
    qkv_c, hy_c, z_c, ba_c = project(h_ctx)
    q, k, v, beta, g = gdn_inputs(qkv_c, ba_c, a_log, dt_bias)
    s0 = jnp.zeros((2, B, GDN_HEADS, GDN_HEAD_DIM, GDN_HEAD_DIM), jnp.float32)
    o_c, s_c = gdn_bidir(q, k, v, beta, g, s0)
    qkv_l, hy_l, z_l, ba_l = project(h_lat)
    q, k, v, beta, g = gdn_inputs(qkv_l, ba_l, a_log, dt_bias)
    o_l, _ = gdn_bidir(q, k, v, beta, g, s_c)
    y_lat = jnp.concatenate([gdn_out(o_l, z_l), hyena_branch(hy_l)], axis=-1) @ w_out
    y_ctx = None
    if need_ctx:
        y_ctx = jnp.concatenate([gdn_out(o_c, z_c), hyena_branch(hy_c)], axis=-1) @ w_out
    return y_lat, y_ctx


def axial_rope(L):
    rows = L // GRID_W
    r, cidx = jnp.meshgrid(jnp.arange(rows), jnp.arange(GRID_W), indexing='ij')
    pos = jnp.stack([r.reshape(-1), cidx.reshape(-1)], axis=-1).astype(jnp.float32)
    inv = ROPE_BASE ** (-jnp.arange(ROPE_FREQS, dtype=jnp.float32) / ROPE_FREQS)
    ang = pos[:, :, None] * inv
    return jnp.cos(ang), jnp.sin(ang)


def apply_rope(x, cos, sin):
    xs = x.reshape(*x.shape[:-1], 2, 2, ROPE_FREQS)
    x1, x2 = xs[..., 0, :], xs[..., 1, :]
    cs = cos[None, :, None, None].astype(x.dtype)
    sn = sin[None, :, None, None].astype(x.dtype)
    return jnp.stack([x1 * cs - x2 * sn, x2 * cs + x1 * sn], axis=-2).reshape(x.shape)


def odd_mixer(h_lat, h_ctx, need_ctx, layer_idx, w_qkv, lam_q1, lam_k1, lam_q2, lam_k2, subln, w_out):
    f32 = jnp.float32
    lam_init = 0.8 - 0.6 * math.exp(-0.3 * layer_idx)
    lam = (jnp.exp(jnp.sum(lam_q1.astype(f32) * lam_k1.astype(f32)))
           - jnp.exp(jnp.sum(lam_q2.astype(f32) * lam_k2.astype(f32))) + lam_init)
    scale = DIFF_HEAD_DIM ** -0.5

    def project(h):
        B, L, _ = h.shape
        p = h @ w_qkv
        q = p[..., :DIFF_QK_WIDTH].reshape(B, L, DIFF_HEADS, 2, DIFF_HEAD_DIM)
        k = p[..., DIFF_QK_WIDTH:2 * DIFF_QK_WIDTH].reshape(B, L, DIFF_HEADS, 2, DIFF_HEAD_DIM)
        v = p[..., 2 * DIFF_QK_WIDTH:].reshape(B, L, DIFF_HEADS, DIFF_V_DIM)
        return q, k, v

    to_bhm = lambda t: jnp.transpose(t, (0, 2, 3, 1, 4))
    to_bh = lambda t: jnp.transpose(t, (0, 2, 1, 3))

    def attend(qb, kk, vv):
        s = jnp.einsum('bhmqd,bhmkd->bhmqk', qb, kk).astype(f32) * scale
        p = jax.nn.softmax(s, axis=-1)
        a = p[:, :, 0] - lam * p[:, :, 1]
        return jnp.einsum('bhqk,bhkd->bhqd', a.astype(vv.dtype), vv)

    def head_out(o):
        B, H, L, dv = o.shape
        o = rmsnorm(jnp.transpose(o, (0, 2, 1, 3)), subln) * (1.0 - lam_init)
        return o.reshape(B, L, H * dv) @ w_out

    q_c, k_c, v_c = project(h_ctx)
    q_l, k_l, v_l = project(h_lat)
    B, L, _ = h_lat.shape
    cos, sin = axial_rope(L)
    q_l = apply_rope(q_l, cos, sin)
    k_l = apply_rope(k_l, cos, sin)
    k_all = jnp.concatenate([to_bhm(k_c), to_bhm(k_l)], axis=3)
    v_all = jnp.concatenate([to_bh(v_c), to_bh(v_l)], axis=2)
    nb = L // Q_BLOCK
    qb = jnp.moveaxis(to_bhm(q_l).reshape(B, DIFF_HEADS, 2, nb, Q_BLOCK, DIFF_HEAD_DIM), 3, 0)
    o_l = lax.map(lambda blk: attend(blk, k_all, v_all), qb)
    o_l = jnp.moveaxis(o_l, 0, 2).reshape(B, DIFF_HEADS, L, DIFF_V_DIM)
    y_lat = head_out(o_l)
    y_ctx = None
    if need_ctx:
        y_ctx = head_out(attend(to_bhm(q_c), to_bhm(k_c), to_bh(v_c)))
    return y_lat, y_ctx


def peer_ffn(h, w_q, keys, u_tab, v_tab):
    T, D = h.shape
    blocks = h.reshape(T // PEER_TOKEN_BLOCK, PEER_TOKEN_BLOCK, D)

    def one(hb):
        t = hb.shape[0]
        q = (hb @ w_q).reshape(t, PEER_HEADS, 2, PEER_QDIM // 2)
        s = jnp.einsum('thps,hpns->thpn', q, keys).astype(jnp.float32)
        sv, si = lax.top_k(s, PEER_TOPK)
        cand = (sv[:, :, 0, :, None] + sv[:, :, 1, None, :]).reshape(t, PEER_HEADS, PEER_TOPK * PEER_TOPK)
        cidx = (si[:, :, 0, :, None] * PEER_KEYS + si[:, :, 1, None, :]).reshape(t, PEER_HEADS, PEER_TOPK * PEER_TOPK)
        top_s, pos = lax.top_k(cand, PEER_TOPK)
        idx = jnp.take_along_axis(cidx, pos, axis=-1)
        gate = jax.nn.softmax(top_s, axis=-1)
        u = u_tab[idx]
        act = jax.nn.gelu(jnp.einsum('td,thkd->thk', hb, u).astype(jnp.float32), approximate=False)
        coef = (gate * act).astype(hb.dtype)
        return jnp.einsum('thk,thkd->td', coef, v_tab[idx])

    return lax.map(one, blocks).reshape(T, D)


def setup_inputs(seed: int = 0) -> dict:
    key = jax.random.key(seed)
    keys = iter(jax.random.split(key, 48))
    f32 = jnp.float32
    D = D_MODEL

    def nrm(shape, scale):
        return jax.random.normal(next(keys), shape, f32) * scale

    def gain(shape):
        return 1.0 + nrm(shape, 0.02)

    x = nrm((BATCH, SEQ, D), 1.0)
    c = nrm((BATCH, D), 1.0)
    ctx = nrm((BATCH, CTX_LEN, D), 1.0)
    c_ctx = nrm((D,), 1.0)
    norm1 = gain((DEPTH, D))
    norm2 = gain((DEPTH, D))
    w_ada = nrm((DEPTH, D, 6 * D), 0.5 * D ** -0.5)
    b_ada = nrm((DEPTH, 6 * D), 0.01)
    w_in = nrm((N_EVEN, D, N_IN), D ** -0.5)
    conv_w = nrm((N_EVEN, SHORT_CONV, N_CONV), SHORT_CONV ** -0.5)
    a_log = jnp.log(jax.random.uniform(next(keys), (N_EVEN, 2, GDN_HEADS), f32, minval=1.0, maxval=16.0))
    dt = jnp.exp(jax.random.uniform(next(keys), (N_EVEN, 2, GDN_HEADS), f32,
                                    minval=math.log(1e-3), maxval=math.log(1e-1)))
    dt_bias = dt + jnp.log(-jnp.expm1(-dt))
    gdn_gain = gain((N_EVEN, GDN_HEAD_DIM))
    hf_w1 = nrm((N_EVEN, HYENA_EMB, HYENA_HIDDEN), HYENA_EMB ** -0.5)
    hf_b1 = nrm((N_EVEN, HYENA_HIDDEN), 0.1)
    hf_w2 = nrm((N_EVEN, HYENA_HIDDEN, HYENA_HIDDEN), HYENA_HIDDEN ** -0.5)
    hf_b2 = nrm((N_EVEN, HYENA_HIDDEN), 0.1)
    hf_w3 = nrm((N_EVEN, HYENA_HIDDEN, HYENA_HIDDEN), HYENA_HIDDEN ** -0.5)
    hf_b3 = nrm((N_EVEN, HYENA_HIDDEN), 0.1)
    hf_w4 = nrm((N_EVEN, HYENA_HIDDEN, HYENA_ORDER * 2 * HYENA_WIDTH), 0.02)
    hf_freq = 1.0 + nrm((N_EVEN, HYENA_HIDDEN), 0.1)
    hy_bias = nrm((N_EVEN, HYENA_ORDER, HYENA_WIDTH), 0.5)
    w_out_even = nrm((N_EVEN, GDN_WIDTH + HYENA_WIDTH, D), (GDN_WIDTH + HYENA_WIDTH) ** -0.5)
    w_qkv = nrm((N_ODD, D, 2 * DIFF_QK_WIDTH + DIFF_V_WIDTH), D ** -0.5)
    lam_q1 = nrm((N_ODD, DIFF_HEAD_DIM), 0.1)
    lam_k1 = nrm((N_ODD, DIFF_HEAD_DIM), 0.1)
    lam_q2 = nrm((N_ODD, DIFF_HEAD_DIM), 0.1)
    lam_k2 = nrm((N_ODD, DIFF_HEAD_DIM), 0.1)
    subln = gain((N_ODD, DIFF_V_DIM))
    w_out_odd = nrm((N_ODD, DIFF_V_WIDTH, D), DIFF_V_WIDTH ** -0.5)
    peer_wq = nrm((DEPTH, D, PEER_HEADS * PEER_QDIM), D ** -0.5)
    peer_keys = nrm((DEPTH, PEER_HEADS, 2, PEER_KEYS, PEER_QDIM // 2), (PEER_QDIM // 2) ** -0.5)
    peer_u = nrm((DEPTH, N_EXPERTS, D), D ** -0.5)
    peer_v = nrm((DEPTH, N_EXPERTS, D), PEER_TOPK ** -0.5)
    final_norm = gain((D,))
    return {'x': x, 'c': c, 'ctx': ctx, 'c_ctx': c_ctx, 'norm1': norm1, 'norm2': norm2,
            'w_ada': w_ada, 'b_ada': b_ada, 'w_in': w_in, 'conv_w': conv_w, 'a_log': a_log,
            'dt_bias': dt_bias, 'gdn_gain': gdn_gain, 'hf_w1': hf_w1, 'hf_b1': hf_b1, 'hf_w2': hf_w2,
            'hf_b2': hf_b2, 'hf_w3': hf_w3, 'hf_b3': hf_b3, 'hf_w4': hf_w4, 'hf_freq': hf_freq,
            'hy_bias': hy_bias, 'w_out_even': w_out_even, 'w_qkv': w_qkv, 'lam_q1': lam_q1,
            'lam_k1': lam_k1, 'lam_q2': lam_q2, 'lam_k2': lam_k2, 'subln': subln,
            'w_out_odd': w_out_odd, 'peer_wq': peer_wq, 'peer_keys': peer_keys, 'peer_u': peer_u,
            'peer_v': peer_v, 'final_norm': final_norm}


def reference(x, c, ctx, c_ctx, norm1, norm2, w_ada, b_ada, w_in, conv_w, a_log, dt_bias, gdn_gain,
              hf_w1, hf_b1, hf_w2, hf_b2, hf_w3, hf_b3, hf_w4, hf_freq, hy_bias, w_out_even,
              w_qkv, lam_q1, lam_k1, lam_q2, lam_k2, subln, w_out_odd,
              peer_wq, peer_keys, peer_u, peer_v, final_norm):
    B, L, D = x.shape
    x_lat, x_ctx = x, ctx
    for layer in range(DEPTH):
        last = layer == DEPTH - 1
        m_l = (jax.nn.silu(c) @ w_ada[layer] + b_ada[layer]).reshape(B, 6, 1, D)
        m_c = (jax.nn.silu(c_ctx) @ w_ada[layer] + b_ada[layer]).reshape(6, D)
        h_l = modulate(rmsnorm(x_lat, norm1[layer]), m_l[:, 0], m_l[:, 1])
        h_c = modulate(rmsnorm(x_ctx, norm1[layer]), m_c[0], m_c[1])
        if layer % 2 == 0:
            e = layer // 2
            y_l, y_c = even_mixer(h_l, h_c, not last, w_in[e], conv_w[e], a_log[e], dt_bias[e], gdn_gain[e],
                                  hf_w1[e], hf_b1[e], hf_w2[e], hf_b2[e], hf_w3[e], hf_b3[e], hf_w4[e],
                                  hf_freq[e], hy_bias[e], w_out_even[e])
        else:
            o = layer // 2
            y_l, y_c = odd_mixer(h_l, h_c, not last, layer, w_qkv[o], lam_q1[o], lam_k1[o], lam_q2[o],
                                 lam_k2[o], subln[o], w_out_odd[o])
        x_lat = x_lat + m_l[:, 2] * y_l
        h_l = modulate(rmsnorm(x_lat, norm2[layer]), m_l[:, 3], m_l[:, 4]).reshape(B * L, D)
        if last:
            f_l = peer_ffn(h_l, peer_wq[layer], peer_keys[layer], peer_u[layer], peer_v[layer])
        else:
            x_ctx = x_ctx + m_c[2] * y_c
            h_c = modulate(rmsnorm(x_ctx, norm2[layer]), m_c[3], m_c[4]).reshape(-1, D)
            f = peer_ffn(jnp.concatenate([h_l, h_c], axis=0), peer_wq[layer], peer_keys[layer],
                         peer_u[layer], peer_v[layer])
            f_l = f[:B * L]
            x_ctx = x_ctx + m_c[5] * f[B * L:].reshape(x_ctx.shape)
        x_lat = x_lat + m_l[:, 5] * f_l.reshape(B, L, D)
    return rmsnorm(x_lat, final_norm)
```

```python
import numpy as np
import concourse.bass as bass
import concourse.mybir as mybir

F32 = mybir.dt.float32
BF16 = mybir.dt.bfloat16
I32 = mybir.dt.int32
U32 = mybir.dt.uint32
AF = mybir.ActivationFunctionType
ALU = mybir.AluOpType
AX = mybir.AxisListType

ARENA_COLS = 52000
N_DMA_SEMS = 6


class V:
    def __init__(self, ap, key):
        self.ap = ap
        self.key = key

    def __getitem__(self, idx):
        return V(self.ap[idx], self.key)

    def bitcast(self, dt):
        return V(self.ap.bitcast(dt), self.key)

    def rearrange(self, pat, **kw):
        return V(self.ap.rearrange(pat, **kw), self.key)

    def bc(self, shape):
        return V(self.ap.to_broadcast(list(shape)), self.key)

    def k(self, sub):
        return V(self.ap, (self.key, sub))

    def unsq(self, axis):
        return V(self.ap.unsqueeze(axis), self.key)

    def pbc(self, n):
        return V(self.ap.partition_broadcast(n), self.key)

    @property
    def shape(self):
        return self.ap.shape


class Op:
    __slots__ = ("eng", "fn", "deps", "signal", "semkey", "semval", "is_dma")

    def __init__(self, eng, fn, is_dma=False):
        self.eng = eng
        self.fn = fn
        self.deps = []
        self.signal = False
        self.semkey = None
        self.semval = None
        self.is_dma = is_dma


def _ap(x):
    return x.ap if isinstance(x, V) else x


class Prog:
    ENGS = ("pe", "dve", "act", "pool", "sp")

    def __init__(self):
        self.nc = bass.Bass("TRN2", target_bir_lowering=False)
        nc = self.nc
        self.ops = {e: [] for e in self.ENGS}
        self.lastw = {}
        self.readers = {}
        self.arena = nc.alloc_sbuf_tensor("arena", [128, ARENA_COLS], F32)
        self.top = 0
        self.psum = []
        for i in range(8):
            t = nc.alloc_psum_tensor(f"psb{i}", [128, 512], F32)
            self.psum.append(V(t.ap(), f"psb{i}"))
        self.dma_last = {}
        self.dma_rr = {q: 0 for q in ("sp", "act", "pool")}
        self.dma_cnt = {}
        self.uid = 0
        self.drams = {}

    def dram(self, name, shape, dt, kind="Internal"):
        t = self.nc.dram_tensor(name, list(shape), dt, kind=kind)
        v = V(t.ap(), name)
        self.drams[name] = v
        return v

    def alloc(self, cols, dt=F32, name=None, parts=128):
        self.uid += 1
        nbytes = cols * mybir.dt.size(dt)
        c32 = (nbytes + 3) // 4
        c32 = (c32 + 7) // 8 * 8
        a = self.top
        self.top += c32
        assert self.top <= ARENA_COLS, f"SBUF arena overflow: {self.top} > {ARENA_COLS}"
        ap = self.arena[0:parts, a:a + c32]
        if dt != F32:
            ap = ap.bitcast(dt)
        ap = ap[:, 0:cols]
        return V(ap, f"{name or 't'}#{self.uid}")

    def mark(self):
        return self.top

    def release(self, m):
        self.top = m

    def _track(self, op, r, w):
        deps = op.deps
        for v in r:
            k = v.key if isinstance(v, V) else v
            lw = self.lastw.get(k)
            if lw is not None:
                deps.append(lw)
            self.readers.setdefault(k, {})
        for v in w:
            k = v.key if isinstance(v, V) else v
            lw = self.lastw.get(k)
            if lw is not None:
                deps.append(lw)
            for rd in self.readers.get(k, {}).values():
                deps.append(rd)
        for v in r:
            k = v.key if isinstance(v, V) else v
            self.readers[k][self._semslot(op)] = op
        for v in w:
            k = v.key if isinstance(v, V) else v
            self.lastw[k] = op
            self.readers[k] = {}

    def _semslot(self, op):
        return op.semkey

    def op(self, eng, fn, r=(), w=()):
        o = Op(eng, fn)
        o.semkey = eng
        self._track(o, r, w)
        self.ops[eng].append(o)
        return o

    def dma(self, q, out, in_, **kw):
        i = self.dma_rr[q]
        self.dma_rr[q] = (i + 1) % N_DMA_SEMS
        o = Op(q, None, is_dma=True)
        o.semkey = ("dma", q, i)
        o.signal = True
        prev = self.dma_last.get((q, i))
        if prev is not None:
            o.deps.append(prev)
        self.dma_last[(q, i)] = o
        oa, ia = _ap(out), _ap(in_)
        eng = self._eng(q)
        o.fn = lambda: eng.dma_start(out=oa, in_=ia, **kw)
        self._track(o, [in_], [out])
        self.ops[q].append(o)
        return o

    def barrier(self):
        lasts = []
        for e in self.ENGS:
            for o in reversed(self.ops[e]):
                if not o.is_dma and o.fn is not None:
                    lasts.append(o)
                    break
        lasts += list(self.dma_last.values())
        for e in self.ENGS:
            o = Op(e, None)
            o.semkey = e
            o.deps = [d for d in lasts]
            self.ops[e].append(o)
        self.lastw = {}
        self.readers = {}

    def _eng(self, e):
        nc = self.nc
        return {"pe": nc.tensor, "dve": nc.vector, "act": nc.scalar, "pool": nc.gpsimd, "sp": nc.sync}[e]

    def mm(self, out, lhsT, rhs, start=True, stop=True, **kw):
        oa, la, ra = _ap(out), _ap(lhsT), _ap(rhs)
        pe = self.nc.tensor
        return self.op("pe", lambda: pe.matmul(oa, la, ra, start=start, stop=stop, **kw), r=[lhsT, rhs], w=[out])

    def tr(self, out, in_, ident):
        oa, ia, da = _ap(out), _ap(in_), _ap(ident)
        pe = self.nc.tensor
        return self.op("pe", lambda: pe.transpose(oa, ia, da), r=[in_, ident], w=[out])

    def act(self, out, in_, func, bias=None, scale=None, accum=None, eng="act"):
        oa, ia = _ap(out), _ap(in_)
        kw = {}
        r = [in_]
        w = [out]
        if bias is not None:
            kw["bias"] = _ap(bias)
            if isinstance(bias, V):
                r.append(bias)
        if scale is not None:
            kw["scale"] = _ap(scale)
            if isinstance(scale, V):
                r.append(scale)
        if accum is not None:
            kw["accum_out"] = _ap(accum)
            w.append(accum)
        sc = self.nc.scalar
        return self.op("act", lambda: sc.activation(oa, ia, func, **kw), r=r, w=w)

    def ts(self, eng, out, in0, s1, s2, op0, op1=None, accum=None):
        oa, ia = _ap(out), _ap(in0)
        r = [in0]
        w = [out]
        for s in (s1, s2):
            if isinstance(s, V):
                r.append(s)
        kw = {}
        if op1 is not None:
            kw["op1"] = op1
        if accum is not None:
            kw["accum_out"] = _ap(accum)
            w.append(accum)
        e = self._eng(eng)
        a1, a2 = _ap(s1), _ap(s2)
        return self.op(eng, lambda: e.tensor_scalar(oa, ia, a1, a2, op0, **kw), r=r, w=w)

    def tt(self, eng, out, in0, in1, op):
        oa, a0, a1 = _ap(out), _ap(in0), _ap(in1)
        e = self._eng(eng)
        return self.op(eng, lambda: e.tensor_tensor(oa, a0, a1, op), r=[in0, in1], w=[out])

    def stt(self, eng, out, in0, scalar, in1, op0, op1):
        oa, a0, a1, sa = _ap(out), _ap(in0), _ap(in1), _ap(scalar)
        r = [in0, in1]
        if isinstance(scalar, V):
            r.append(scalar)
        e = self._eng(eng)
        return self.op(eng, lambda: e.scalar_tensor_tensor(oa, a0, sa, a1, op0, op1), r=r, w=[out])

    def copy(self, eng, out, in_):
        oa, ia = _ap(out), _ap(in_)
        if eng == "act":
            sc = self.nc.scalar
            return self.op("act", lambda: sc.copy(oa, ia), r=[in_], w=[out])
        e = self._eng(eng)
        return self.op(eng, lambda: e.tensor_copy(oa, ia), r=[in_], w=[out])

    def memset(self, eng, out, val):
        oa = _ap(out)
        e = self._eng(eng)
        return self.op(eng, lambda: e.memset(oa, val), r=[], w=[out])

    def reduce(self, eng, out, in_, op, axis=AX.X):
        oa, ia = _ap(out), _ap(in_)
        e = self._eng(eng)
        return self.op(eng, lambda: e.tensor_reduce(oa, ia, axis, op), r=[in_], w=[out])

    def recip(self, out, in_):
        oa, ia = _ap(out), _ap(in_)
        e = self.nc.vector
        return self.op("dve", lambda: e.reciprocal(oa, ia), r=[in_], w=[out])

    def emit(self):
        nc = self.nc
        for e in self.ENGS:
            for o in self.ops[e]:
                for d in o.deps:
                    if d.eng == "pe" and o.eng == "pe" and not d.is_dma and not o.is_dma:
                        continue
                    d.signal = True
        cnt = {}
        n_ins = 0
        for e in self.ENGS:
            for o in self.ops[e]:
                if o.fn is None:
                    continue
                n_ins += 1
                if o.signal:
                    inc = 16 if o.is_dma else 1
                    cnt[o.semkey] = cnt.get(o.semkey, 0) + inc
                    o.semval = cnt[o.semkey]
        self.n_ins = n_ins
        self.sem_final = cnt
        semkeys = list(cnt.keys())
        from contextlib import ExitStack
        with ExitStack() as es:
            sems = {}
            for i, k in enumerate(semkeys):
                sems[k] = es.enter_context(nc.semaphore(f"s{i}"))
            block = es.enter_context(nc.Block())

            def replay(ename):
                def f(eng):
                    waited = {}
                    for o in self.ops[ename]:
                        need = {}
                        for d in o.deps:
                            if d.semval is None:
                                continue
                            if ename == "pe" and d.eng == "pe" and not d.is_dma and not o.is_dma:
                                continue
                            if need.get(d.semkey, 0) < d.semval:
                                need[d.semkey] = d.semval
                        for k, v in need.items():
                            if waited.get(k, 0) < v:
                                eng.wait_ge(sems[k], v)
                                waited[k] = v
                        if o.fn is None:
                            continue
                        ins = o.fn()
                        if o.signal:
                            ins.then_inc(sems[o.semkey], 16 if o.is_dma else 1)
                return f

            block.tensor(replay("pe"))
            block.vector(replay("dve"))
            block.scalar(replay("act"))
            block.gpsimd(replay("pool"))
            block.sync(replay("sp"))
        return nc


from concourse.bass_utils import run_bass_kernel_spmd
IndirectOffsetOnAxis = bass.IndirectOffsetOnAxis

D = 1024
LAT = 4096
CTX = 256
NT_ALL = (LAT + CTX) // 128
EPS = 1e-6
NEG = -1.0e30


class Ring:
    def __init__(self, p, n, cols, dt=F32, name="ring", parts=128):
        self.bufs = [p.alloc(cols, dt, name=f"{name}{i}", parts=parts) for i in range(n)]
        self.i = 0

    def next(self):
        b = self.bufs[self.i % len(self.bufs)]
        self.i += 1
        return b


class Ctx:
    def __init__(self):
        self.p = Prog()
        self.inputs = {}
        self.in_dt = {}

    def inp(self, name, shape, dt=F32):
        if name not in self.p.drams:
            self.p.dram(name, shape, dt, kind="ExternalInput")
            self.inputs[name] = tuple(shape)
            self.in_dt[name] = dt
        return self.p.drams[name]


def load_row_bc(p, q, dst, src_row):
    return p.dma(q, dst, V(src_row.ap.to_broadcast([dst.shape[0], src_row.shape[1]]), src_row.key))


def phase_mod(cx, layer, m_scr):
    p = cx.p
    mk = p.mark()
    cc = cx.inp("cc", [128, 16])
    w_ada = cx.inp("w_ada", [4, 1024, 6144])
    b_ada = cx.inp("b_ada", [4, 6144])
    cct = p.alloc(16)
    sil = p.alloc(16)
    p.dma("sp", cct, cc)
    p.act(sil, cct, AF.Silu)
    silv = sil.rearrange("p (a k) -> p a k", a=2)
    wr = Ring(p, 2, 8 * 512, name="wada")
    br = Ring(p, 2, 512, name="bada")
    mr = Ring(p, 2, 512, name="mrow")
    wsrc = w_ada[layer].rearrange("(k p) c -> p k c", p=128)
    for cb in range(12):
        wt = wr.next()
        p.dma("sp", wt.rearrange("p (k c) -> p k c", k=8), wsrc[:, :, cb * 512:(cb + 1) * 512])
        bt = br.next()
        load_row_bc(p, "act", bt[0:2, :], b_ada[layer:layer + 1, cb * 512:(cb + 1) * 512])
        ps = p.psum[cb % 2]
        for k in range(8):
            p.mm(ps[0:2, :], silv[:, :, k], wt[:, k * 512:(k + 1) * 512], start=(k == 0), stop=(k == 7))
        mt = mr.next()
        p.tt("dve", mt[0:2, :], ps[0:2, :], bt[0:2, :], ALU.add)
        p.dma("sp", m_scr[layer, :, cb * 512:(cb + 1) * 512], mt[0:2, :])
    p.barrier()
    p.release(mk)


class NormMod:
    def __init__(self, cx, layer, xs, m_scr, norm_name, i_shift, i_scale, nbuf=2):
        p = cx.p
        self.p = p
        self.xs = xs
        nrm = cx.inp(norm_name, [4, 1024])
        self.A = {}
        self.B = {}
        g = p.alloc(1024, name="g_row")
        load_row_bc(p, "act", g, nrm[layer:layer + 1, :])
        for v in (0, 1):
            a = p.alloc(1024, name="A_row")
            b = p.alloc(1024, name="B_row")
            load_row_bc(p, "act", a, m_scr[layer, v:v + 1, i_scale * 1024:(i_scale + 1) * 1024])
            load_row_bc(p, "act", b, m_scr[layer, v:v + 1, i_shift * 1024:(i_shift + 1) * 1024])
            p.stt("dve", a, a, 1.0, g, ALU.add, ALU.mult)
            self.A[v] = a
            self.B[v] = b
        self.xr = Ring(p, nbuf, 1024, name="xt")
        self.hr = Ring(p, nbuf, 1024, name="ht")
        self.junk = p.alloc(1024, name="junk")
        self.st = Ring(p, nbuf, 4, name="stat")

    def tile(self, ti):
        p = self.p
        v = 1 if ti < 2 else 0
        xt = self.xr.next()
        p.dma("sp", xt, self.xs[ti * 128:(ti + 1) * 128, :])
        st = self.st.next()
        p.act(self.junk, xt, AF.Square, accum=st[:, 0:1])
        p.act(st[:, 1:2], st[:, 0:1], AF.Sqrt, scale=1.0 / D, bias=self.eps_ap())
        p.recip(st[:, 2:3], st[:, 1:2])
        ht = self.hr.next()
        p.stt("dve", ht, xt, st[:, 2:3], self.A[v], ALU.mult, ALU.mult)
        p.tt("pool", ht, ht, self.B[v], ALU.add)
        return xt, ht

    def eps_ap(self):
        if not hasattr(self, "_eps"):
            self._eps = self.p.alloc(1, name="eps")
            self.p.memset("dve", self._eps, EPS)
        return self._eps


def transpose_tile(p, ident, src, dst_fn, banks, evac_eng="act"):
    for j in range(2):
        ps = p.psum[banks[j]]
        for c in range(4):
            k = 4 * j + c
            p.tr(ps[:, c * 128:(c + 1) * 128], src[:, k * 128:(k + 1) * 128], ident)
        dst = dst_fn(j)
        p.copy(evac_eng, dst, ps.rearrange("p (c t) -> p c t", c=4))


def phase_peer(cx, layer, xs, m_scr, tiles):
    p = cx.p
    mk = p.mark()
    ident_d = cx.inp("ident", [128, 128])
    iota_d = cx.inp("iota16", [128, 16])
    wq_d = cx.inp("peer_wq", [4, 1024, 2048])
    keysT_d = cx.inp("peer_keysT", [4, 128, 2048])
    pu = cx.inp("peer_u", [4, 16384, 1024])
    pv = cx.inp("peer_v", [4, 16384, 1024])
    pu_flat = pu.rearrange("l e d -> (l e) d")
    pv_flat = pv.rearrange("l e d -> (l e) d")

    ident = p.alloc(128, name="ident")
    p.dma("act", ident, ident_d)
    iota = p.alloc(16, name="iota")
    p.dma("act", iota, iota_d)
    wq = p.alloc(8 * 2048, name="wq")
    p.dma("sp", wq.rearrange("p (k c) -> p k c", k=8), wq_d[layer].rearrange("(k p) c -> p k c", p=128))
    keysT = p.alloc(2048, name="keysT")
    p.dma("act", keysT, keysT_d[layer])
    G2 = {}
    for v in (0, 1):
        if v == 1 and tiles[0] >= 2:
            continue
        G2[v] = p.alloc(1024, name="G2")
        load_row_bc(p, "act", G2[v], m_scr[layer, v:v + 1, 5 * 1024:6 * 1024])
    nm = NormMod(cx, layer, xs, m_scr, "norm2", 3, 4, nbuf=1)

    h2T = p.alloc(1024, name="h2T")
    qTt = p.alloc(2048, name="qTt")
    sc = p.alloc(2048, name="sc")
    sc2 = p.alloc(2048, name="sc2")
    sv = p.alloc(256, name="sv")
    si = p.alloc(256, U32, name="si")
    sif = p.alloc(256, name="sif")
    cand = p.alloc(2048, name="cand")
    cand2 = p.alloc(2048, name="cand2")
    ts_ = p.alloc(128, name="ts")
    pos = p.alloc(128, U32, name="pos")
    ipos = p.alloc(128, U32, name="ipos")
    jpos = p.alloc(128, U32, name="jpos")
    iposf = p.alloc(128, name="iposf")
    jposf = p.alloc(128, name="jposf")
    eq = p.alloc(2048, name="eq")
    asel = p.alloc(128, name="asel")
    bsel = p.alloc(128, name="bsel")
    eidf = p.alloc(128, name="eidf")
    eid = p.alloc(128, U32, name="eid")
    gate = p.alloc(128, name="gate")
    gz = p.alloc(16, name="gz")
    actv = p.alloc(128, name="actv")
    coef = p.alloc(128, name="coef")
    acc = p.alloc(1024, name="acc")
    junk = p.alloc(1024, name="pjunk")
    ug = Ring(p, 4, 1024, name="ug")
    vg = ug
    outr = Ring(p, 1, 1024, name="xo")

    sv4 = sv.rearrange("p (h a i) -> p h a i", h=8, a=2)
    sif4 = sif.rearrange("p (h a i) -> p h a i", h=8, a=2)
    cand4 = cand.rearrange("p (h i j) -> p h i j", h=8, i=16)
    ts3 = ts_.rearrange("p (h k) -> p h k", h=8)
    eq4 = eq.rearrange("p (h k i) -> p h k i", h=8, k=16)
    iota_b = iota.unsq(1).unsq(1).bc([128, 8, 16, 16])

    for ti in tiles:
        v = 1 if ti < 2 else 0
        xt, h2 = nm.tile(ti)
        transpose_tile(p, ident, h2, lambda j: h2T.rearrange("p (k t) -> p k t", k=8)[:, 4 * j:4 * j + 4, :], (0, 1))
        for g4 in range(4):
            ps = p.psum[2 + g4]
            for c in range(4):
                hp = 4 * g4 + c
                for k in range(8):
                    p.mm(ps[:, c * 128:(c + 1) * 128], wq[:, k * 2048 + hp * 128:k * 2048 + (hp + 1) * 128],
                         h2T[:, k * 128:(k + 1) * 128], start=(k == 0), stop=(k == 7))
            p.copy("act", qTt[:, g4 * 512:(g4 + 1) * 512], ps)
        for g4 in range(4):
            ps = p.psum[(6 + g4) % 8]
            for c in range(4):
                hp = 4 * g4 + c
                p.mm(ps[:, c * 128:(c + 1) * 128], qTt[:, hp * 128:(hp + 1) * 128], keysT[:, hp * 128:(hp + 1) * 128])
            p.copy("act", sc[:, g4 * 512:(g4 + 1) * 512], ps)
        for hp in range(16):
            s_hp = sc[:, hp * 128:(hp + 1) * 128]
            s2_hp = sc2[:, hp * 128:(hp + 1) * 128]
            o8a = sv[:, hp * 16:hp * 16 + 8]
            o8b = sv[:, hp * 16 + 8:hp * 16 + 16]
            _max8(p, o8a, s_hp)
            _match_replace(p, s2_hp, o8a, s_hp)
            _max8(p, o8b, s2_hp)
            _max_index(p, si[:, hp * 16:hp * 16 + 8], o8a, s_hp)
            _max_index(p, si[:, hp * 16 + 8:hp * 16 + 16], o8b, s_hp)
        p.copy("dve", sif, si)
        p.tt("dve", cand4, sv4[:, :, 0, :].unsq(3).bc([128, 8, 16, 16]), sv4[:, :, 1, :].unsq(2).bc([128, 8, 16, 16]), ALU.add)
        for h in range(8):
            c_h = cand[:, h * 256:(h + 1) * 256]
            c2_h = cand2[:, h * 256:(h + 1) * 256]
            o8a = ts_[:, h * 16:h * 16 + 8]
            o8b = ts_[:, h * 16 + 8:h * 16 + 16]
            _max8(p, o8a, c_h)
            _match_replace(p, c2_h, o8a, c_h)
            _max8(p, o8b, c2_h)
            _max_index(p, pos[:, h * 16:h * 16 + 8], o8a, c_h)
            _max_index(p, pos[:, h * 16 + 8:h * 16 + 16], o8b, c_h)
        p.ts("dve", ipos, pos, 4, None, ALU.logical_shift_right)
        p.ts("dve", jpos, pos, 15, None, ALU.bitwise_and)
        p.copy("dve", iposf, ipos)
        p.copy("dve", jposf, jpos)
        for (dst, posf, a) in ((asel, iposf, 0), (bsel, jposf, 1)):
            pf3 = posf.rearrange("p (h k) -> p h k", h=8)
            p.tt("dve", eq4, iota_b, pf3.unsq(3).bc([128, 8, 16, 16]), ALU.is_equal)
            p.tt("dve", eq4, eq4, sif4[:, :, a, :].unsq(2).bc([128, 8, 16, 16]), ALU.mult)
            p.reduce("dve", dst.rearrange("p (h k) -> p h k", h=8), eq4, ALU.add, AX.X)
        p.stt("dve", eidf, asel, 128.0, bsel, ALU.mult, ALU.add)
        if layer > 0:
            p.ts("dve", eidf, eidf, float(layer * 16384), None, ALU.add)
        p.copy("dve", eid, eidf)
        p.tt("dve", gate.rearrange("p (h k) -> p h k", h=8), ts3, ts3[:, :, 0:1].bc([128, 8, 16]), ALU.subtract)
        p.act(gate, gate, AF.Exp)
        p.reduce("dve", gz[:, 0:8], gate.rearrange("p (h k) -> p h k", h=8), ALU.add, AX.X)
        p.recip(gz[:, 8:16], gz[:, 0:8])
        p.tt("dve", gate.rearrange("p (h k) -> p h k", h=8), gate.rearrange("p (h k) -> p h k", h=8),
             gz[:, 8:16].unsq(2).bc([128, 8, 16]), ALU.mult)
        for s in range(128):
            ub = ug.next()
            _gather(p, ub, pu_flat, eid[:, s:s + 1])
            _ttr(p, junk, ub, h2, actv[:, s:s + 1])
        p.act(coef, actv, AF.Gelu)
        p.tt("dve", coef, coef, gate, ALU.mult)
        for s in range(128):
            vb = vg.next()
            _gather(p, vb, pv_flat, eid[:, s:s + 1])
            if s == 0:
                p.ts("dve", acc, vb, coef[:, 0:1], None, ALU.mult)
            else:
                p.stt("dve", acc, vb, coef[:, s:s + 1], acc, ALU.mult, ALU.add)
        xo = outr.next()
        p.tt("dve", xo, acc, G2[v], ALU.mult)
        p.tt("dve", xo, xo, xt, ALU.add)
        p.dma("sp", xs[ti * 128:(ti + 1) * 128, :], xo)
    p.barrier()
    p.release(mk)


def _max8(p, out, in_):
    oa, ia = out.ap, in_.ap
    e = p.nc.vector
    return p.op("dve", lambda: e.max(oa, ia), r=[in_], w=[out])


def _max_index(p, out, in_max, in_values):
    oa, ma, va = out.ap, in_max.ap, in_values.ap
    e = p.nc.vector
    return p.op("dve", lambda: e.max_index(oa, ma, va), r=[in_max, in_values], w=[out])


def _match_replace(p, out, in_to_replace, in_values):
    oa, ra, va = out.ap, in_to_replace.ap, in_values.ap
    e = p.nc.vector
    return p.op("dve", lambda: e.match_replace(oa, ra, va, NEG), r=[in_to_replace, in_values], w=[out])


def _ttr(p, out, in0, in1, accum):
    oa, a0, a1, ac = out.ap, in0.ap, in1.ap, accum.ap
    e = p.nc.vector
    return p.op("dve", lambda: e.scalar_tensor_tensor(oa, a0, 1.0, a1, ALU.mult, ALU.mult, accum_out=ac),
                r=[in0, in1], w=[out, accum])


def _gather(p, out, table, idx):
    q = "pool"
    i = p.dma_rr[q]
    p.dma_rr[q] = (i + 1) % N_DMA_SEMS
    o = Op(q, None, is_dma=True)
    o.semkey = ("dma", q, i)
    o.signal = True
    prev = p.dma_last.get((q, i))
    if prev is not None:
        o.deps.append(prev)
    p.dma_last[(q, i)] = o
    oa, ta, ia = out.ap, table.ap, idx.ap
    g = p.nc.gpsimd
    o.fn = lambda: g.indirect_dma_start(oa, None, ta, IndirectOffsetOnAxis(ia, 0))
    p._track(o, [table, idx], [out])
    p.ops[q].append(o)
    return o


def dram_copy(p, dst, src, rows, q="sp", chunk=512):
    for r0 in range(0, rows, chunk):
        r1 = min(rows, r0 + chunk)
        p.dma(q, dst[r0:r1, :], src[r0:r1, :])


def build(cfg):
    cx = Ctx()
    p = cx.p
    xs = p.dram("xs", [LAT + CTX, D], F32)
    m_scr = p.dram("m_scr", [4, 2, 6144], F32)
    x_in = cx.inp("x", [LAT, D])
    ctx_in = cx.inp("ctx", [CTX, D])
    out = p.dram("out", [LAT, D], F32, kind="ExternalOutput")
    dram_copy(p, xs[CTX:, :], x_in, LAT)
    dram_copy(p, xs[0:CTX, :], ctx_in, CTX, q="act")
    p.barrier()
    test = cfg.get("test")
    if test is None:
        build_full(cx, xs, m_scr, out)
    if test == "layers":
        build_full(cx, xs, m_scr, out, layers=cfg["layers"])
        dbg = p.dram("dbg_xs", [LAT + CTX, D], F32, kind="ExternalOutput")
        dram_copy(p, dbg, xs, LAT + CTX)
    if test == "odd":
        layer = cfg["layer"]
        phase_mod(cx, layer, m_scr)
        phase_odd(cx, layer, xs, m_scr, layer != 3)
        dbg = p.dram("dbg_xs", [LAT + CTX, D], F32, kind="ExternalOutput")
        dram_copy(p, dbg, xs, LAT + CTX)
    if test == "even":
        layer = cfg["layer"]
        S = even_scratch(p, layer)
        phase_mod(cx, layer, m_scr)
        for ph in cfg["phases"]:
            {"proj": phase_even_proj, "gdn": phase_gdn, "gdn_out": phase_gdn_out, "hy": phase_hyena}[ph](*((cx, layer, xs, m_scr, S) if ph == "proj" else (cx, layer, S)))
        for nm in cfg.get("dump", []):
            src = S[nm]
            shp = list(src.shape)
            dd = p.dram("dbg_" + nm, shp, src.ap.dtype, kind="ExternalOutput")
            if len(shp) == 3:
                for g in range(shp[0]):
                    p.dma("sp", dd[g], src[g])
            else:
                dram_copy(p, dd, src, shp[0], chunk=1024)
    if test == "peer":
        layer = cfg["layer"]
        phase_mod(cx, layer, m_scr)
        phase_peer(cx, layer, xs, m_scr, cfg["tiles"])
        dbg = p.dram("dbg_xs", [LAT + CTX, D], F32, kind="ExternalOutput")
        dram_copy(p, dbg, xs, LAT + CTX)
        dbm = p.dram("dbg_m", [2, 6144], F32, kind="ExternalOutput")
        p.dma("sp", dbm, m_scr[layer])
    p.barrier()
    nc = p.emit()
    return cx, nc


def host_inputs(cx, inputs, b):
    m = {}
    f32 = np.float32
    for name in cx.inputs:
        if name == "x":
            a = inputs["x"][b]
        elif name == "ctx":
            a = inputs["ctx"][b]
        elif name == "cc":
            a = np.concatenate([inputs["c"][b].reshape(8, 128).T, inputs["c_ctx"].reshape(8, 128).T], axis=1)
        elif name == "ident":
            a = np.eye(128, dtype=f32)
        elif name == "iota16":
            a = np.tile(np.arange(16, dtype=f32)[None, :], (128, 1))
        elif name == "rope_cs":
            a = rope_table()
        elif name == "conv_wT":
            a = inputs["conv_w"].reshape(2, 3, 24, 128).transpose(0, 3, 2, 1).reshape(2, 128, 72)
        elif name == "gdn_masks":
            i = np.arange(64)[:, None]; j = np.arange(64)[None, :]
            a = np.concatenate([(i <= j), (i >= j), (i < j), (i > j)], axis=1).astype(f32)
        elif name == "final_norm":
            a = inputs["final_norm"].reshape(1, 1024)
        elif name == "peer_keysT":
            a = inputs["peer_keys"].transpose(0, 4, 1, 2, 3).reshape(4, 128, 2048)
        elif name.startswith("dft"):
            L = int(name[3:].split("_")[0])
            a = dft_tables(L)[name.split("_")[1]]
        elif name.startswith("hy_zT"):
            a = hyena_consts(int(name[5:]))[0]
        elif name.startswith("hy_win"):
            a = hyena_consts(int(name[6:]))[1]
        else:
            a = inputs[name]
        if cx.in_dt[name] == BF16:
            m[name] = np.ascontiguousarray(a)
            assert m[name].dtype == _mld.bfloat16
        else:
            m[name] = np.ascontiguousarray(a, dtype=f32)
        assert m[name].shape == cx.inputs[name], (name, m[name].shape, cx.inputs[name])
    return m


def make_hT(cx, layer, xs, m_scr, ident, hT):
    p = cx.p
    mk = p.mark()
    nm = NormMod(cx, layer, xs, m_scr, "norm1", 0, 1, nbuf=2)
    hT3 = hT.rearrange("p (k t) -> p k t", k=8)
    for ti in range(NT_ALL):
        xt, h = nm.tile(ti)
        b0 = (ti % 2) * 2
        transpose_tile(p, ident, h, lambda j: hT3[:, 4 * j:4 * j + 4, ti * 128:(ti + 1) * 128], (b0, b0 + 1),
                       evac_eng=("act" if ti % 2 == 0 else "dve"))
    p.barrier()
    p.release(mk)


def cast_weight(p, dst3, src3, ncols, stage_ring, blk=512):
    K = dst3.shape[1]
    i = 0
    for c0 in range(0, ncols, blk):
        c1 = min(ncols, c0 + blk)
        st = stage_ring.next()
        sv = st.rearrange("p (k c) -> p k c", k=K)[:, :, 0:c1 - c0]
        p.dma("sp" if i % 2 == 0 else "act", sv, src3[:, :, c0:c1])
        p.copy("pool" if i % 2 == 0 else "dve", dst3[:, :, c0:c1], sv)
        i += 1


def phase_odd(cx, layer, xs, m_scr, need_ctx):
    import math
    p = cx.p
    o = layer // 2
    lam_init = 0.8 - 0.6 * math.exp(-0.3 * layer)
    mk0 = p.mark()
    ident_d = cx.inp("ident", [128, 128])
    w_qkv = cx.inp("w_qkv", [2, 1024, 3072])
    w_out = cx.inp("w_out_odd", [2, 1024, 1024])
    rope_d = cx.inp("rope_cs", [LAT, 64])
    subln_d = cx.inp("subln", [2, 128])
    lam_d = {n: cx.inp(n, [2, 64]) for n in ("lam_q1", "lam_k1", "lam_q2", "lam_k2")}
    NTOK = LAT + CTX
    qkT_d = p.dram(f"qkT_d{layer}", [16, 128, NTOK], BF16)
    v_d = p.dram(f"v_d{layer}", [NTOK, 1024], BF16)

    ident = p.alloc(128, name="ident")
    p.dma("act", ident, ident_d)
    hT = p.alloc(8 * NTOK, BF16, name="hT")
    make_hT(cx, layer, xs, m_scr, ident, hT)
    hT3 = hT.rearrange("p (k t) -> p k t", k=8)

    mk = p.mark()
    wb = p.alloc(8 * 3072, BF16, name="wqkv_b")
    wb3 = wb.rearrange("p (k c) -> p k c", k=8)
    stg = Ring(p, 2, 8 * 512, name="wstage")
    cast_weight(p, wb3, w_qkv[o].rearrange("(k p) c -> p k c", p=128), 3072, stg)
    qk_r = Ring(p, 2, 2048, name="qk_t")
    rot_r = Ring(p, 2, 2048, name="qk_rot")
    tmp_r = Ring(p, 2, 1024, name="rtmp")
    cs_r = Ring(p, 2, 64, name="cs")
    qkTs_r = Ring(p, 2, 16 * 128, BF16, name="qkTs")
    vs_r = Ring(p, 2, 1024, BF16, name="vs")
    for ti in range(NT_ALL):
        qk = qk_r.next()
        for cb in range(6):
            ps = p.psum[(ti * 6 + cb) % 4]
            for k in range(8):
                p.mm(ps, hT3[:, k, ti * 128:(ti + 1) * 128], wb3[:, k, cb * 512:(cb + 1) * 512], start=(k == 0), stop=(k == 7))
            if cb < 4:
                p.copy("act", qk[:, cb * 512:(cb + 1) * 512], ps)
            else:
                if cb == 4:
                    vs = vs_r.next()
                p.copy("act", vs[:, (cb - 4) * 512:(cb - 3) * 512], ps)
        p.dma("act", v_d[ti * 128:(ti + 1) * 128, :], vs)
        if ti >= 2:
            cs = cs_r.next()
            p.dma("sp", cs, rope_d[(ti - 2) * 128:(ti - 1) * 128, :])
            rot = rot_r.next()
            x5 = qk.rearrange("p (g a h f) -> p g a h f", g=32, a=2, h=2)
            r5 = rot.rearrange("p (g a h f) -> p g a h f", g=32, a=2, h=2)
            cosb = cs[:, 0:32].rearrange("p (a f) -> p a f", a=2).unsq(1).bc([128, 32, 2, 16])
            sinb = cs[:, 32:64].rearrange("p (a f) -> p a f", a=2).unsq(1).bc([128, 32, 2, 16])
            x1, x2 = x5[:, :, :, 0, :], x5[:, :, :, 1, :]
            t = tmp_r.next().rearrange("p (g a f) -> p g a f", g=32, a=2)
            t2 = tmp_r.next().rearrange("p (g a f) -> p g a f", g=32, a=2)
            p.tt("dve", t, x2, sinb, ALU.mult)
            p.tt("pool", t2, x1, sinb, ALU.mult)
            p.tt("dve", r5[:, :, :, 0, :], x1, cosb, ALU.mult)
            p.tt("pool", r5[:, :, :, 1, :], x2, cosb, ALU.mult)
            p.tt("dve", r5[:, :, :, 0, :], r5[:, :, :, 0, :], t, ALU.subtract)
            p.tt("pool", r5[:, :, :, 1, :], r5[:, :, :, 1, :], t2, ALU.add)
            src = rot
        else:
            src = qk
        qkTs = qkTs_r.next()
        for g4 in range(4):
            ps = p.psum[4 + g4]
            for c in range(4):
                blk = 4 * g4 + c
                p.tr(ps[:, c * 128:(c + 1) * 128], src[:, blk * 128:(blk + 1) * 128], ident)
            p.copy("dve" if g4 % 2 == 0 else "act", qkTs[:, g4 * 512:(g4 + 1) * 512], ps)
        p.dma("sp", qkT_d[:, :, ti * 128:(ti + 1) * 128].rearrange("g p t -> p g t"),
              qkTs.rearrange("p (g t) -> p g t", g=16))
    p.barrier()
    p.release(mk)
    p.release(mk0)

    mk = p.mark()
    onT = p.alloc(8 * NTOK, BF16, name="onT")
    onT3 = onT.rearrange("p (h t) -> p h t", h=8)
    ones_b = p.alloc(128, BF16, name="ones_b")
    p.memset("dve", ones_b, 1.0)
    eps_t = p.alloc(1, name="eps")
    p.memset("dve", eps_t, EPS)
    lt = p.alloc(4 * 64, name="lamv")
    for i, n in enumerate(("lam_q1", "lam_k1", "lam_q2", "lam_k2")):
        load_row_bc(p, "act", lt[:, i * 64:(i + 1) * 64], lam_d[n][o:o + 1, :])
    ls = p.alloc(8, name="lams")
    lj = p.alloc(64, name="lamj")
    _ttr(p, lj, lt[:, 0:64], lt[:, 64:128], ls[:, 0:1])
    _ttr(p, lj, lt[:, 128:192], lt[:, 192:256], ls[:, 1:2])
    p.act(ls[:, 2:4], ls[:, 0:2], AF.Exp)
    p.stt("dve", ls[:, 4:5], ls[:, 3:4], -lam_init, ls[:, 2:3], ALU.add, ALU.subtract)
    neg_lam = ls[:, 4:5]
    sg = p.alloc(2, name="sg")
    p.dma("act", sg[:, 0:1], subln_d[o].rearrange("(p o) -> p o", o=1))
    p.ts("dve", sg[:, 1:2], sg[:, 0:1], 1.0 - lam_init, None, ALU.mult)
    sgs = sg[:, 1:2]

    mk_att = p.mark()
    kT_r = Ring(p, 2, NTOK, BF16, name="kT_h")
    qT_r = Ring(p, 2, NTOK, BF16, name="qT_h")
    v_r = Ring(p, 2, NT_ALL * 128, BF16, name="v_h")
    pt_r = Ring(p, 4, 512, BF16, name="PT")
    rz_r = Ring(p, 2, 512, name="rz")
    o0_r = Ring(p, 2, 512, name="o0")
    o1_r = Ring(p, 2, 512, name="o1")
    sq_r = Ring(p, 2, 512, BF16, name="sq")
    rs_r = Ring(p, 2, 512, name="rs")
    scale = 0.125
    it = 0
    for h in range(8):
        kT = kT_r.next()
        qT = qT_r.next()
        vh = v_r.next()
        p.dma("sp", kT, qkT_d[8 + h])
        p.dma("act", qT, qkT_d[h])
        p.dma("sp", vh.rearrange("p (t d) -> p t d", d=128),
              v_d[:, h * 128:(h + 1) * 128].rearrange("(t p) d -> p t d", p=128))
        blocks = [(CTX + qb * 512, 512, list(range(NT_ALL))) for qb in range(LAT // 512)]
        if need_ctx:
            blocks.append((0, CTX, [0, 1]))
        for (q0, nq, kts) in blocks:
            o0 = o0_r.next()
            o1 = o1_r.next()
            for m in range(2):
                psO = p.psum[2 + (it % 2)]
                psZ = p.psum[4 + (it % 2)]
                it += 1
                for i, kt in enumerate(kts):
                    psS = p.psum[i % 2]
                    p.mm(psS[:, 0:nq], kT[m * 64:(m + 1) * 64, kt * 128:(kt + 1) * 128], qT[m * 64:(m + 1) * 64, q0:q0 + nq])
                    pt = pt_r.next()
                    p.act(pt[:, 0:nq], psS[:, 0:nq], AF.Exp, scale=scale)
                    p.mm(psO[:, 0:nq], vh[:, kt * 128:(kt + 1) * 128], pt[:, 0:nq], start=(i == 0), stop=(i == len(kts) - 1))
                    p.mm(psZ[:, 0:nq], ones_b, pt[:, 0:nq], start=(i == 0), stop=(i == len(kts) - 1))
                rz = rz_r.next()
                p.recip(rz[:, 0:nq], psZ[:, 0:nq])
                if m == 0:
                    p.tt("dve", o0[:, 0:nq], psO[:, 0:nq], rz[:, 0:nq], ALU.mult)
                else:
                    p.tt("dve", o1[:, 0:nq], psO[:, 0:nq], rz[:, 0:nq], ALU.mult)
                    p.stt("dve", o0[:, 0:nq], o1[:, 0:nq], neg_lam, o0[:, 0:nq], ALU.mult, ALU.add)
            sq = sq_r.next()
            p.tt("pool", sq[:, 0:nq], o0[:, 0:nq], o0[:, 0:nq], ALU.mult)
            psR = p.psum[6]
            p.mm(psR[:, 0:nq], ones_b, sq[:, 0:nq])
            rs = rs_r.next()
            p.act(rs[:, 0:nq], psR[:, 0:nq], AF.Sqrt, scale=1.0 / 128, bias=eps_t)
            p.recip(rs[:, 0:nq], rs[:, 0:nq])
            p.stt("dve", onT3[:, h, q0:q0 + nq], o0[:, 0:nq], sgs, rs[:, 0:nq], ALU.mult, ALU.mult)

    p.barrier()
    p.release(mk_att)
    wo = p.alloc(8 * 1024, BF16, name="wo_b")
    wo3 = wo.rearrange("p (h c) -> p h c", h=8)
    stg = Ring(p, 2, 8 * 512, name="wstage2")
    cast_weight(p, wo3, w_out[o].rearrange("(h p) c -> p h c", p=128), 1024, stg)
    G1 = {}
    for v in ((0, 1) if need_ctx else (0,)):
        G1[v] = p.alloc(1024, name="G1")
        load_row_bc(p, "act", G1[v], m_scr[layer, v:v + 1, 2 * 1024:3 * 1024])
    xr = Ring(p, 2, 1024, name="xres")
    yr = Ring(p, 2, 1024, name="yres")
    for ti in range(0 if need_ctx else 2, NT_ALL):
        v = 1 if ti < 2 else 0
        xt = xr.next()
        p.dma("sp", xt, xs[ti * 128:(ti + 1) * 128, :])
        yt = yr.next()
        for cb in range(2):
            ps = p.psum[(ti * 2 + cb) % 4]
            for h in range(8):
                p.mm(ps, onT3[:, h, ti * 128:(ti + 1) * 128], wo3[:, h, cb * 512:(cb + 1) * 512], start=(h == 0), stop=(h == 7))
            p.tt("dve", yt[:, cb * 512:(cb + 1) * 512], ps, G1[v][:, cb * 512:(cb + 1) * 512], ALU.mult)
        p.tt("pool", yt, yt, xt, ALU.add)
        p.dma("act", xs[ti * 128:(ti + 1) * 128, :], yt)
    p.barrier()
    p.release(mk)
    p.release(mk0)


def rope_table():
    rows = LAT // 64
    r, c = np.meshgrid(np.arange(rows), np.arange(64), indexing="ij")
    pos = np.stack([r.reshape(-1), c.reshape(-1)], axis=-1).astype(np.float32)
    inv = (10000.0 ** (-np.arange(16, dtype=np.float32) / 16)).astype(np.float32)
    ang = pos[:, :, None] * inv
    return np.concatenate([np.cos(ang).reshape(LAT, 32), np.sin(ang).reshape(LAT, 32)], axis=1).astype(np.float32)


NTOK = LAT + CTX
NCH = NTOK // 64
SEQS = ((0, CTX), (CTX, LAT))


def even_scratch(p, layer):
    s = {}
    s["qT"] = p.dram(f"e{layer}_qT", [4, 128, NTOK], F32)
    s["kT"] = p.dram(f"e{layer}_kT", [4, 128, NTOK], F32)
    s["k_tok"] = p.dram(f"e{layer}_ktok", [NTOK, 512], F32)
    s["v_tok"] = p.dram(f"e{layer}_vtok", [NTOK, 512], F32)
    s["zs"] = p.dram(f"e{layer}_zs", [NTOK, 512], F32)
    s["ba"] = p.dram(f"e{layer}_ba", [64, NCH * 16], F32)
    s["hyx"] = p.dram(f"e{layer}_hyx", [8, 128, NTOK], F32)
    s["hyv"] = p.dram(f"e{layer}_hyv", [NTOK, 512], BF16)
    s["o_f"] = p.dram(f"e{layer}_of", [NTOK, 512], F32)
    s["o_b"] = p.dram(f"e{layer}_ob", [NTOK, 512], F32)
    s["actT"] = p.dram(f"e{layer}_actT", [8, 128, NTOK], BF16)
    return s


def phase_even_proj(cx, layer, xs, m_scr, S):
    p = cx.p
    e = layer // 2
    mk0 = p.mark()
    ident_d = cx.inp("ident", [128, 128])
    w_in = cx.inp("w_in", [2, 1024, 3600])
    convT_d = cx.inp("conv_wT", [2, 128, 72])
    ident = p.alloc(128, name="ident")
    p.dma("act", ident, ident_d)
    hT = p.alloc(8 * NTOK, BF16, name="hT")
    make_hT(cx, layer, xs, m_scr, ident, hT)
    hT3 = hT.rearrange("p (k t) -> p k t", k=8)
    wb = p.alloc(8 * 3600, BF16, name="win_b")
    wb3 = wb.rearrange("p (k c) -> p k c", k=8)
    mks = p.mark()
    stg = Ring(p, 2, 8 * 512, name="wstage")
    cast_weight(p, wb3, w_in[e].rearrange("(k p) c -> p k c", p=128), 3600, stg)
    p.barrier()
    p.release(mks)
    cw = p.alloc(72, name="convw")
    p.dma("act", cw, convT_d[e])
    ones_f = p.alloc(128, name="ones_f")
    p.memset("dve", ones_f, 1.0)
    eps_t = p.alloc(1, name="eps")
    p.memset("dve", eps_t, EPS)

    PB = NTOK + 4
    pb_r = Ring(p, 1, PB, name="pbuf")
    cv_r = Ring(p, 2, NTOK, name="cv")
    sq_r = Ring(p, 2, 512, name="sq")
    rn_r = Ring(p, 2, 512, name="rn")
    tk_r = Ring(p, 2, 512, name="tok_stage")
    tkb_r = Ring(p, 2, 512, BF16, name="tokb_stage")
    blocks = [(0, CTX, 1)] + [(CTX + 512 * b, 512, CTX + 512 * b + 3) for b in range(LAT // 512)]
    for pb in pb_r.bufs:
        p.memset("pool", pb, 0.0)
    ib = 0
    for cg in range(24):
        pbuf = pb_r.next()
        for (t0, n, c0) in blocks:
            ps = p.psum[ib % 4]
            ib += 1
            for k in range(8):
                p.mm(ps[:, 0:n], wb3[:, k, cg * 128:(cg + 1) * 128], hT3[:, k, t0:t0 + n], start=(k == 0), stop=(k == 7))
            p.copy("act" if ib % 2 == 0 else "dve", pbuf[:, c0:c0 + n], ps[:, 0:n])
        cv = cv_r.next()
        for (t0, n) in SEQS:
            c0 = t0 + 1 if t0 == 0 else t0 + 3
            p.act(cv[:, t0:t0 + n], pbuf[:, c0:c0 + n], AF.Copy, scale=cw[:, cg * 3 + 1:cg * 3 + 2])
            p.stt("dve", cv[:, t0:t0 + n], pbuf[:, c0 - 1:c0 - 1 + n], cw[:, cg * 3:cg * 3 + 1], cv[:, t0:t0 + n], ALU.mult, ALU.add)
            p.stt("dve", cv[:, t0:t0 + n], pbuf[:, c0 + 1:c0 + 1 + n], cw[:, cg * 3 + 2:cg * 3 + 3], cv[:, t0:t0 + n], ALU.mult, ALU.add)
        if cg < 12:
            p.act(cv, cv, AF.Silu)
        if cg < 8:
            for (t0, n, _) in blocks:
                sq = sq_r.next()
                p.tt("pool", sq[:, 0:n], cv[:, t0:t0 + n], cv[:, t0:t0 + n], ALU.mult)
                ps = p.psum[4 + (ib % 2)]
                ib += 1
                p.mm(ps[:, 0:n], ones_f, sq[:, 0:n])
                rn = rn_r.next()
                p.act(rn[:, 0:n], ps[:, 0:n], AF.Sqrt, bias=eps_t)
                p.recip(rn[:, 0:n], rn[:, 0:n])
                if cg < 4:
                    p.stt("dve", cv[:, t0:t0 + n], cv[:, t0:t0 + n], 128.0 ** -0.5, rn[:, 0:n], ALU.mult, ALU.mult)
                else:
                    p.tt("dve", cv[:, t0:t0 + n], cv[:, t0:t0 + n], rn[:, 0:n], ALU.mult)
        if cg < 4:
            p.dma("sp", S["qT"][cg], cv)
        elif cg < 8:
            p.dma("sp", S["kT"][cg - 4], cv)
        elif 12 <= cg < 20:
            p.dma("sp", S["hyx"][cg - 12], cv)
        if 4 <= cg < 12 or cg >= 20:
            if cg < 8:
                dst, col, bf = S["k_tok"], (cg - 4) * 128, False
            elif cg < 12:
                dst, col, bf = S["v_tok"], (cg - 8) * 128, False
            else:
                dst, col, bf = S["hyv"], (cg - 20) * 128, True
            for t4 in range(0, NT_ALL, 4):
                nt = min(4, NT_ALL - t4)
                ps = p.psum[6 + (ib % 2)]
                ib += 1
                for c in range(nt):
                    p.tr(ps[:, c * 128:(c + 1) * 128], cv[:, (t4 + c) * 128:(t4 + c + 1) * 128], ident)
                st = (tkb_r if bf else tk_r).next()
                p.copy("act", st[:, 0:nt * 128], ps[:, 0:nt * 128])
                p.dma("act" if bf else "sp", dst[t4 * 128:(t4 + nt) * 128, col:col + 128].rearrange("(c p) d -> p c d", p=128),
                      st[:, 0:nt * 128].rearrange("p (c d) -> p c d", d=128))
    z_r = Ring(p, 2, 512, name="zt")
    for ti in range(NT_ALL):
        ps = p.psum[ti % 4]
        for k in range(8):
            p.mm(ps, hT3[:, k, ti * 128:(ti + 1) * 128], wb3[:, k, 3072:3584], start=(k == 0), stop=(k == 7))
        zt = z_r.next()
        p.act(zt, ps, AF.Silu)
        p.dma("sp", S["zs"][ti * 128:(ti + 1) * 128, :], zt)
    bas = p.alloc(NCH * 16, name="ba_s", parts=64)
    for c0 in range(0, NCH, 32):
        nc_ = min(32, NCH - c0)
        ps = p.psum[4 + (c0 // 32) % 2]
        for c in range(nc_):
            ch = c0 + c
            for k in range(8):
                p.mm(ps[0:64, c * 16:(c + 1) * 16], hT3[:, k, ch * 64:(ch + 1) * 64], wb3[:, k, 3584:3600], start=(k == 0), stop=(k == 7))
        p.copy("dve", bas[:, c0 * 16:(c0 + nc_) * 16], ps[0:64, 0:nc_ * 16])
    p.dma("sp", S["ba"], bas)
    p.barrier()
    p.release(mk0)


class BankRR:
    def __init__(self, p, banks):
        self.p = p
        self.banks = list(banks)
        self.i = 0

    def next(self):
        b = self.p.psum[self.banks[self.i % len(self.banks)]]
        self.i += 1
        return b


class GdnDir:
    def __init__(self, p, d):
        self.d = d
        a = lambda cols, nm, parts=64: p.alloc(cols, name=f"g{d}_{nm}", parts=parts)
        self.ktok = a(8 * 128, "ktok")
        self.vtok = a(8 * 128, "vtok")
        self.gm1 = a(512, "gm1")
        self.gm2 = a(512, "gm2")
        self.dec = a(512, "dec")
        self.decT = a(512, "decT")
        self.A = a(512, "A")
        self.P = [a(512, "Pa"), a(512, "Pb")]
        self.Q = [a(512, "Qa"), a(512, "Qb")]
        self.RT = [a(512, "RTa"), a(512, "RTb")]
        self.IP = a(512, "IP")
        self.qkdT = a(512, "qkdT")
        self.bv = a(8 * 128, "bv")
        self.bk = a(8 * 128, "bk")
        self.kd = a(8 * 128, "kd")
        self.wi = a(8 * 128, "wi")
        self.kcT = a(512, "kcT", parts=128)
        self.sc = a(64, "scal")
        self.cdb = a(8, "cdb", parts=128)
        self.S = a(128, "S", parts=128)
        self.ws = [a(128, "w_s0"), a(128, "w_s1")]
        self.tmp = [a(128, "tmp0"), a(128, "tmp1")]
        self.ob = a(8 * 128, "ob")


def phase_gdn(cx, layer, S):
    p = cx.p
    e = layer // 2
    mk0 = p.mark()
    masks_d = cx.inp("gdn_masks", [64, 256])
    ident_d = cx.inp("ident", [128, 128])
    alog_d = cx.inp("a_log", [2, 2, 4])
    dtb_d = cx.inp("dt_bias", [2, 2, 4])
    ident = p.alloc(128, name="ident")
    p.dma("act", ident, ident_d)
    id64 = ident[0:64, 0:64]
    masks = p.alloc(256, name="masks", parts=64)
    p.dma("act", masks, masks_d)
    Um, Lom, SUm, SLm = (masks[:, i * 64:(i + 1) * 64] for i in range(4))
    ones_f = p.alloc(128, name="ones_f")
    p.memset("dve", ones_f, 1.0)
    one64 = ones_f[0:64, 0:1]
    bas = p.alloc(NCH * 16, name="bas", parts=64)
    p.dma("sp", bas, S["ba"])
    bas4 = bas.rearrange("p (c t x) -> p c t x", c=NCH, t=2)
    beta = p.alloc(NCH * 8, name="beta", parts=64)
    gt = p.alloc(NCH * 8, name="gt", parts=64)
    beta3 = beta.rearrange("p (c x) -> p c x", c=NCH)
    gt3 = gt.rearrange("p (c x) -> p c x", c=NCH)
    p.act(beta3, bas4[:, :, 0, :], AF.Exp, scale=-1.0)
    p.ts("dve", beta, beta, 1.0, None, ALU.add)
    p.recip(beta, beta)
    rows = p.alloc(16, name="rows", parts=64)
    load_row_bc(p, "act", rows[:, 0:8], alog_d[e:e + 1].rearrange("a d h -> a (d h)"))
    load_row_bc(p, "act", rows[:, 8:16], dtb_d[e:e + 1].rearrange("a d h -> a (d h)"))
    p.act(rows[:, 0:8], rows[:, 0:8], AF.Exp)
    p.ts("dve", rows[:, 0:8], rows[:, 0:8], -1.0, None, ALU.mult)
    p.tt("dve", gt3, bas4[:, :, 1, :], rows[:, 8:16].unsq(1).bc([64, NCH, 8]), ALU.add)
    p.act(gt, gt, AF.Exp)
    p.act(gt, gt, AF.Ln, bias=one64)
    p.tt("dve", gt3, gt3, rows[:, 0:8].unsq(1).bc([64, NCH, 8]), ALU.mult)

    qk_r = Ring(p, 2, NTOK, name="qT_h")
    kk_r = Ring(p, 2, NTOK, name="kT_h")
    dirs = [GdnDir(p, 0), GdnDir(p, 1)]
    brr = BankRR(p, range(8))
    lat_groups = [list(range(4 + 8 * g, 12 + 8 * g)) for g in range(8)]
    groups = {0: [[0, 1, 2, 3]] + lat_groups, 1: [[0, 1, 2, 3]] + lat_groups[::-1]}
    MS = {0: (Um, SLm, SLm, SLm, Um, Um), 1: (Lom, SUm, SUm, SUm, Lom, Lom)}

    def prep(dc, h, chunks, qT, kT):
        d = dc.d
        L, R, MSk, L2, R2, MB = MS[d]
        n = len(chunks)
        c0 = chunks[0]
        W = n * 64
        x = d * 4 + h
        gcols = gt3[:, c0:c0 + n, x]
        bcols = beta3[:, c0:c0 + n, x]
        v3 = lambda t, w=64: t[:, 0:n * w].rearrange("p (c j) -> p c j", c=n)
        p.dma("sp", v3(dc.ktok, 128), S["k_tok"][c0 * 64:(c0 + n) * 64, h * 128:(h + 1) * 128].rearrange("(c p) d -> p c d", p=64))
        p.dma("act", v3(dc.vtok, 128), S["v_tok"][c0 * 64:(c0 + n) * 64, h * 128:(h + 1) * 128].rearrange("(c p) d -> p c d", p=64))
        ps_kk = brr.next()
        ps_qk = brr.next()
        for c, ch in enumerate(chunks):
            p.mm(ps_kk[0:64, c * 64:(c + 1) * 64], kT[:, ch * 64:(ch + 1) * 64], kT[:, ch * 64:(ch + 1) * 64])
            p.mm(ps_qk[0:64, c * 64:(c + 1) * 64], kT[:, ch * 64:(ch + 1) * 64], qT[:, ch * 64:(ch + 1) * 64])
        p.tt("dve", v3(dc.gm1), R.unsq(1).bc([64, n, 64]), gcols.unsq(2).bc([64, n, 64]), ALU.mult)
        p.tt("pool", v3(dc.gm2), R2.unsq(1).bc([64, n, 64]), gcols.unsq(2).bc([64, n, 64]), ALU.mult)
        ps3 = brr.next()
        ps4 = brr.next()
        p.mm(ps3[0:64, 0:W], L, dc.gm1[:, 0:W])
        p.mm(ps4[0:64, 0:W], L2, dc.gm2[:, 0:W])
        p.act(dc.dec[:, 0:W], ps3[0:64, 0:W], AF.Exp)
        p.act(dc.decT[:, 0:W], ps4[0:64, 0:W], AF.Exp)
        p.tt("dve", v3(dc.dec), v3(dc.dec), MSk.unsq(1).bc([64, n, 64]), ALU.mult)
        p.tt("pool", v3(dc.decT), v3(dc.decT), MB.unsq(1).bc([64, n, 64]), ALU.mult)
        p.tt("dve", dc.A[:, 0:W], ps_kk[0:64, 0:W], dc.dec[:, 0:W], ALU.mult)
        p.tt("dve", v3(dc.A), v3(dc.A), bcols.unsq(2).bc([64, n, 64]), ALU.mult)
        p.tt("dve", dc.qkdT[:, 0:W], ps_qk[0:64, 0:W], dc.decT[:, 0:W], ALU.mult)
        ps5 = brr.next()
        for c in range(n):
            p.tr(ps5[0:64, c * 64:(c + 1) * 64], dc.A[:, c * 64:(c + 1) * 64], id64)
        Pc, Qc, RTc = dc.A, dc.Q[0], dc.RT[0]
        p.copy("act", Qc[:, 0:W], ps5[0:64, 0:W])
        p.tt("pool", v3(RTc), id64.unsq(1).bc([64, n, 64]), v3(Qc), ALU.subtract)
        for r in range(5):
            Pn, Qn, RTn = dc.P[r % 2], dc.Q[(r + 1) % 2], dc.RT[(r + 1) % 2]
            psP = brr.next()
            for c in range(n):
                p.mm(psP[0:64, c * 64:(c + 1) * 64], Qc[:, c * 64:(c + 1) * 64], Pc[:, c * 64:(c + 1) * 64])
            if r < 4:
                psQ = brr.next()
                for c in range(n):
                    p.mm(psQ[0:64, c * 64:(c + 1) * 64], Pc[:, c * 64:(c + 1) * 64], Qc[:, c * 64:(c + 1) * 64])
            p.copy("act", Pn[:, 0:W], psP[0:64, 0:W])
            if r < 4:
                p.copy("dve", Qn[:, 0:W], psQ[0:64, 0:W])
            p.tt("pool", v3(dc.IP), v3(Pn), id64.unsq(1).bc([64, n, 64]), ALU.add)
            psR = brr.next()
            for c in range(n):
                p.mm(psR[0:64, c * 64:(c + 1) * 64], dc.IP[:, c * 64:(c + 1) * 64], RTc[:, c * 64:(c + 1) * 64])
            p.copy("dve", RTn[:, 0:W], psR[0:64, 0:W])
            Pc, Qc, RTc = Pn, Qn, RTn
        psG = brr.next()
        p.mm(psG[0:64, 0:n], L, gcols)
        p.mm(psG[0:128, 8:8 + n], ones_f[0:64, :], gcols)
        Gs, eG, kds, bke = (dc.sc[:, i * 8:i * 8 + n] for i in range(4))
        p.copy("dve", Gs, psG[0:64, 0:n])
        p.act(eG, psG[0:64, 0:n], AF.Exp)
        p.act(dc.cdb[:, 0:n], psG[0:128, 8:8 + n], AF.Exp)
        p.tt("dve", kds, psG[0:64, 8:8 + n], Gs, ALU.subtract)
        p.act(kds, kds, AF.Exp)
        p.tt("dve", bke, bcols, eG, ALU.mult)
        p.tt("pool", v3(dc.bv, 128), v3(dc.vtok, 128), bcols.unsq(2).bc([64, n, 128]), ALU.mult)
        p.tt("dve", v3(dc.bk, 128), v3(dc.ktok, 128), bke.unsq(2).bc([64, n, 128]), ALU.mult)
        p.tt("pool", v3(dc.kd, 128), v3(dc.ktok, 128), kds.unsq(2).bc([64, n, 128]), ALU.mult)
        for half in range(0, n, 4):
            psW = brr.next()
            m = min(4, n - half)
            for c in range(half, half + m):
                p.mm(psW[0:64, (c - half) * 128:(c - half + 1) * 128], RTc[:, c * 64:(c + 1) * 64], dc.bv[:, c * 128:(c + 1) * 128])
            p.copy("act", dc.wi[:, half * 128:(half + m) * 128], psW[0:64, 0:m * 128])
        psK = brr.next()
        for c in range(n):
            p.mm(psK[0:128, c * 64:(c + 1) * 64], dc.bk[:, c * 128:(c + 1) * 128], RTc[:, c * 64:(c + 1) * 64])
        p.copy("dve", dc.kcT[:, 0:W], psK[0:128, 0:W])

    def scan_step(dc, ci, ch, qT, it):
        psA = brr.next()
        p.mm(psA[0:64, 0:128], dc.kcT[:, ci * 64:(ci + 1) * 64], dc.S)
        ws = dc.ws[it % 2]
        p.tt("dve", ws, dc.wi[:, ci * 128:(ci + 1) * 128], psA[0:64, 0:128], ALU.subtract)
        psO = brr.next()
        p.mm(psO[0:64, 0:128], qT[:, ch * 64:(ch + 1) * 64], dc.S)
        p.mm(psO[0:64, 128:256], dc.qkdT[:, ci * 64:(ci + 1) * 64], ws)
        tmp = dc.tmp[it % 2]
        p.act(tmp, psO[0:64, 0:128], AF.Copy, scale=dc.sc[:, 8 + ci:8 + ci + 1])
        p.tt("dve", dc.ob[:, ci * 128:(ci + 1) * 128], tmp, psO[0:64, 128:256], ALU.add)
        psS = brr.next()
        p.mm(psS[0:128, 0:128], dc.kd[:, ci * 128:(ci + 1) * 128], ws)
        p.stt("dve", dc.S, dc.S, dc.cdb[:, ci:ci + 1], psS[0:128, 0:128], ALU.mult, ALU.add)

    it = 0
    for h in range(4):
        qT = qk_r.next()
        kT = kk_r.next()
        p.dma("sp", qT, S["qT"][h])
        p.dma("act", kT, S["kT"][h])
        for dc in dirs:
            p.memset("pool", dc.S, 0.0)
        for gi in range(9):
            for dc in dirs:
                prep(dc, h, groups[dc.d][gi], qT, kT)
            n = len(groups[0][gi])
            for s in range(n):
                for dc in dirs:
                    chunks = groups[dc.d][gi]
                    ci = s if dc.d == 0 else n - 1 - s
                    scan_step(dc, ci, chunks[ci], qT, it)
                it += 1
            for dc in dirs:
                chunks = groups[dc.d][gi]
                c0 = chunks[0]
                dst = S["o_f"] if dc.d == 0 else S["o_b"]
                p.dma("sp" if dc.d == 0 else "act",
                      dst[c0 * 64:(c0 + n) * 64, h * 128:(h + 1) * 128].rearrange("(c p) d -> p c d", p=64),
                      dc.ob[:, 0:n * 128].rearrange("p (c d) -> p c d", c=n))
    p.barrier()
    p.release(mk0)


def phase_gdn_out(cx, layer, S):
    p = cx.p
    e = layer // 2
    mk0 = p.mark()
    ident_d = cx.inp("ident", [128, 128])
    gain_d = cx.inp("gdn_gain", [2, 128])
    ident = p.alloc(128, name="ident")
    p.dma("act", ident, ident_d)
    gain = p.alloc(128, name="gain")
    load_row_bc(p, "act", gain, gain_d[e:e + 1, :])
    eps_t = p.alloc(1, name="eps")
    p.memset("dve", eps_t, EPS)
    of_r = Ring(p, 2, 512, name="of")
    ob_r = Ring(p, 2, 512, name="ob")
    z_r = Ring(p, 2, 512, name="zz")
    sq_r = Ring(p, 2, 512, name="sq")
    st_r = Ring(p, 2, 8, name="st")
    tb_r = Ring(p, 2, 512, BF16, name="tb")
    for ti in range(NT_ALL):
        of, ob, zz = of_r.next(), ob_r.next(), z_r.next()
        rows = slice(ti * 128, (ti + 1) * 128)
        p.dma("sp", of, S["o_f"][rows, :])
        p.dma("act", ob, S["o_b"][rows, :])
        p.dma("sp", zz, S["zs"][rows, :])
        p.tt("dve", of, of, ob, ALU.add)
        sq = sq_r.next()
        p.tt("pool", sq, of, of, ALU.mult)
        st = st_r.next()
        p.reduce("dve", st[:, 0:4], sq.rearrange("p (h d) -> p h d", h=4), ALU.add, AX.X)
        p.act(st[:, 4:8], st[:, 0:4], AF.Sqrt, scale=1.0 / 128, bias=eps_t)
        p.recip(st[:, 4:8], st[:, 4:8])
        o3 = of.rearrange("p (h d) -> p h d", h=4)
        p.tt("dve", o3, o3, st[:, 4:8].unsq(2).bc([128, 4, 128]), ALU.mult)
        p.tt("pool", o3, o3, gain.unsq(1).bc([128, 4, 128]), ALU.mult)
        p.tt("dve", of, of, zz, ALU.mult)
        ps = p.psum[ti % 4]
        for c in range(4):
            p.tr(ps[:, c * 128:(c + 1) * 128], of[:, c * 128:(c + 1) * 128], ident)
        tb = tb_r.next()
        p.copy("act", tb, ps)
        p.dma("act", S["actT"][0:4, :, ti * 128:(ti + 1) * 128].rearrange("g p t -> p g t"), tb.rearrange("p (g t) -> p g t", g=4))
    p.barrier()
    p.release(mk0)


import math as _math
import ml_dtypes as _mld

_TABLE_CACHE = {}


def dft_tables(L):
    if L in _TABLE_CACHE:
        return _TABLE_CACHE[L]
    N = 2 * L
    TB = min(512, L)
    n_t = L // 128
    t = np.arange(L, dtype=np.float64)[:, None]
    f = np.arange(L, dtype=np.float64)[None, :]
    ang = (2.0 * np.pi / N) * ((f + 0.5) * t)
    out = {}
    for nm, M in (("C", np.cos(ang)), ("S", np.sin(ang))):
        M = M.astype(np.float32)
        fw = M.reshape(n_t, 128, n_t, 128).transpose(2, 1, 0, 3).reshape(n_t, 128, n_t * 128)
        inv = M.reshape(L // TB, TB, n_t, 128).transpose(0, 3, 2, 1).reshape(L // TB, 128, n_t * TB)
        out[nm + "fw"] = np.ascontiguousarray(fw).astype(_mld.bfloat16)
        out[nm + "inv"] = np.ascontiguousarray(inv).astype(_mld.bfloat16)
    _TABLE_CACHE[L] = out
    return out


def hyena_consts(L):
    f32 = np.float32
    t = np.linspace(0.0, 1.0, L, dtype=f32)
    bands = 16
    wpos = (2.0 * _math.pi * np.arange(L, dtype=f32) / L).astype(f32)
    fb = np.linspace(1e-4, bands - 1, bands, dtype=f32)
    z = np.concatenate([t[:, None], np.cos(wpos[:, None] * fb), -np.sin(wpos[:, None] * fb)], axis=-1).astype(f32)
    mn = _math.log(1e-2) / 1.5
    mx = _math.log(1e-2) / 0.3
    deltas = np.linspace(mn, mx, 512, dtype=f32)
    win = np.exp(-t[:, None] * np.abs(deltas)).astype(f32)
    return np.ascontiguousarray(z.T), win


def phase_hyena(cx, layer, S):
    p = cx.p
    e = layer // 2
    mk0 = p.mark()
    ident_d = cx.inp("ident", [128, 128])
    ident = p.alloc(128, name="ident")
    p.dma("act", ident, ident_d)
    w1_d = cx.inp("hf_w1", [2, 33, 64])
    w2_d = cx.inp("hf_w2", [2, 64, 64])
    w3_d = cx.inp("hf_w3", [2, 64, 64])
    w4_d = cx.inp("hf_w4", [2, 64, 2048])
    vec_d = {n: cx.inp(n, [2, 64]) for n in ("hf_b1", "hf_b2", "hf_b3", "hf_freq")}
    hyb_d = cx.inp("hy_bias", [2, 2, 512])
    PI = _math.pi

    w1 = p.alloc(64, name="w1", parts=33)
    w2 = p.alloc(64, name="w2", parts=64)
    w3 = p.alloc(64, name="w3", parts=64)
    w4 = p.alloc(2048, name="w4", parts=64)
    p.dma("sp", w1, w1_d[e])
    p.dma("sp", w2, w2_d[e])
    p.dma("sp", w3, w3_d[e])
    p.dma("sp", w4, w4_d[e])
    vec = p.alloc(8, name="hvec", parts=64)
    for i, n in enumerate(("hf_b1", "hf_b2", "hf_b3", "hf_freq")):
        p.dma("act", vec[:, i:i + 1], vec_d[n][e].rearrange("(p o) -> p o", o=1))
    for i in range(3):
        p.tt("dve", vec[:, 4 + i:5 + i], vec[:, i:i + 1], vec[:, 3:4], ALU.mult)
    freq = vec[:, 3:4]
    brow = p.alloc(1024, name="hybias", parts=1)
    p.dma("act", brow, hyb_d[e:e + 1].rearrange("a o c -> a (o c)"))

    for (t0, L) in SEQS:
        N = 2 * L
        TB = min(512, L)
        n_t = L // 128
        n_tb = L // TB
        tabs = {k: cx.inp(f"dft{L}_{k}", list(shp), BF16) for k, shp in
                (("Cfw", (n_t, 128, n_t * 128)), ("Sfw", (n_t, 128, n_t * 128)),
                 ("Cinv", (n_tb, 128, n_t * TB)), ("Sinv", (n_tb, 128, n_t * TB)))}
        zT_d = cx.inp(f"hy_zT{L}", [33, L])
        win_d = cx.inp(f"hy_win{L}", [L, 512])
        fa_d = p.dram(f"e{layer}_fa{L}", [4, L, 512], BF16)
        spec_d = p.dram(f"e{layer}_spec{L}", [4, L, 512], F32)
        S[f"fa{L}"] = fa_d
        S[f"spec{L}"] = spec_d
        mk = p.mark()
        h3 = p.alloc(L, name="h3", parts=64)
        zr = Ring(p, 2, TB, name="zTt", parts=33)
        hr = Ring(p, 4, TB, name="hh", parts=64)
        tr_ = Ring(p, 2, TB, name="wrapt", parts=64)

        def sin_layer(dst, ps, fbcol, n):
            y = hr.next()
            p.ts("dve", y[:, 0:n], ps[0:64, 0:n], freq, fbcol, ALU.mult, ALU.add)
            for _ in range(2):
                t = tr_.next()
                p.ts("dve", t[:, 0:n], y[:, 0:n], PI, -2.0 * PI, ALU.is_gt, ALU.mult)
                p.tt("dve", y[:, 0:n], y[:, 0:n], t[:, 0:n], ALU.add)
                t = tr_.next()
                p.ts("dve", t[:, 0:n], y[:, 0:n], -PI, 2.0 * PI, ALU.is_lt, ALU.mult)
                p.tt("dve", y[:, 0:n], y[:, 0:n], t[:, 0:n], ALU.add)
            p.act(dst, y[:, 0:n], AF.Sin)

        for b in range(n_tb):
            zt = zr.next()
            p.dma("sp", zt, zT_d[:, b * TB:(b + 1) * TB])
            ps = p.psum[b % 2]
            p.mm(ps[0:64, 0:TB], w1, zt)
            h1 = hr.next()
            sin_layer(h1, ps, vec[:, 4:5], TB)
            ps = p.psum[2 + b % 2]
            p.mm(ps[0:64, 0:TB], w2, h1)
            h2 = hr.next()
            sin_layer(h2, ps, vec[:, 5:6], TB)
            ps = p.psum[4 + b % 2]
            p.mm(ps[0:64, 0:TB], w3, h2)
            sin_layer(h3[:, b * TB:(b + 1) * TB], ps, vec[:, 6:7], TB)
        win_r = Ring(p, 2, 512, name="win")
        hf_r = Ring(p, 2, 512, name="hf")
        hb_r = Ring(p, 2, 512, name="hb")
        ad_r = Ring(p, 4, 512, BF16, name="ad")
        for tt_ in range(n_t):
            wt = win_r.next()
            p.dma("sp", wt, win_d[tt_ * 128:(tt_ + 1) * 128, :])
            for o in range(2):
                psf = p.psum[(2 * o) % 4]
                psb = p.psum[(2 * o + 1) % 4]
                p.mm(psf, h3[:, tt_ * 128:(tt_ + 1) * 128], w4[:, (2 * o) * 512:(2 * o + 1) * 512])
                p.mm(psb, h3[:, tt_ * 128:(tt_ + 1) * 128], w4[:, (2 * o + 1) * 512:(2 * o + 2) * 512])
                hf, hb = hf_r.next(), hb_r.next()
                p.tt("dve", hf, psf, wt, ALU.mult)
                p.tt("dve", hb, psb, wt, ALU.mult)
                if tt_ == 0:
                    p.tt("dve", hf[0:1, :], hf[0:1, :], brow[0:1, o * 512:(o + 1) * 512], ALU.add)
                    p.memset("dve", hb[0:1, :], 0.0)
                a, d = ad_r.next(), ad_r.next()
                p.tt("pool", a, hf, hb, ALU.add)
                p.tt("pool", d, hb, hf, ALU.subtract)
                p.dma("sp", fa_d[2 * o, tt_ * 128:(tt_ + 1) * 128, :], a)
                p.dma("act", fa_d[2 * o + 1, tt_ * 128:(tt_ + 1) * 128, :], d)
        p.barrier()
        p.release(mk)

        U = p.alloc(n_t * 512, BF16, name="U")
        U3 = U.rearrange("p (t c) -> p t c", c=512)
        Yr = p.alloc(n_t * 512, BF16, name="Yr")
        Yi = p.alloc(n_t * 512, BF16, name="Yi")
        Yr3 = Yr.rearrange("p (t c) -> p t c", c=512)
        Yi3 = Yi.rearrange("p (t c) -> p t c", c=512)
        FQ = min(8, n_t)
        tab_r = Ring(p, 4, max(n_t * 128, FQ * TB), BF16, name="tab")
        sp_r = Ring(p, 4, 512, name="spec")
        x_r = Ring(p, 4, 512, name="xcs")
        t_r = Ring(p, 4, 512, name="ytmp")
        xt_r = Ring(p, 2, TB, name="xT")
        zo_r = Ring(p, 2, TB, name="zo")
        zb_r = Ring(p, 2, TB, BF16, name="zob")

        def load_U(src_rows):
            p.dma("sp", U3, src_rows.rearrange("(t p) c -> p t c", p=128))

        def fwd_pass(which, consumer):
            for ft in range(n_t):
                pss = {}
                for i, w in enumerate(which):
                    tb_ = tab_r.next()
                    p.dma("sp" if i == 0 else "act", tb_[:, 0:n_t * 128], tabs[w + "fw"][ft])
                    ps = p.psum[(2 * ft + i) % 4]
                    for tc in range(n_t):
                        p.mm(ps, tb_[:, tc * 128:(tc + 1) * 128], U3[:, tc, :], start=(tc == 0), stop=(tc == n_t - 1))
                    pss[w] = ps
                consumer(ft, pss)

        for o in range(2):
            for j, w in enumerate(("C", "S")):
                load_U(fa_d[2 * o + j])

                def store_spec(ft, pss, j=j, w=w, o=o):
                    st = sp_r.next()
                    p.act(st, pss[w], AF.Copy, scale=2.0 / N)
                    p.dma("act", spec_d[2 * o + j, ft * 128:(ft + 1) * 128, :], st)
                fwd_pass((w,), store_spec)
        for o in range(2):
            if o == 0:
                load_U(S["hyv"][t0:t0 + L, :])

            def make_Y(ft, pss, o=o):
                ac, ds = sp_r.next(), sp_r.next()
                p.dma("sp", ac, spec_d[2 * o, ft * 128:(ft + 1) * 128, :])
                p.dma("act", ds, spec_d[2 * o + 1, ft * 128:(ft + 1) * 128, :])
                xc, xs_ = x_r.next(), x_r.next()
                p.copy("act", xc, pss["C"])
                p.copy("act", xs_, pss["S"])
                t1, t2 = t_r.next(), t_r.next()
                p.tt("pool", t1, xc, ac, ALU.mult)
                p.tt("dve", t2, xs_, ds, ALU.mult)
                p.tt("dve", Yr3[:, ft, :], t1, t2, ALU.add)
                t3, t4 = t_r.next(), t_r.next()
                p.tt("pool", t3, xs_, ac, ALU.mult)
                p.tt("dve", t4, xc, ds, ALU.mult)
                p.tt("pool", Yi3[:, ft, :], t3, t4, ALU.subtract)
            fwd_pass(("C", "S"), make_Y)
            for tb in range(n_tb):
                for fq in range(n_t // FQ):
                    ct, st_ = tab_r.next(), tab_r.next()
                    p.dma("sp", ct[:, 0:FQ * TB], tabs["Cinv"][tb][:, fq * FQ * TB:(fq + 1) * FQ * TB])
                    p.dma("act", st_[:, 0:FQ * TB], tabs["Sinv"][tb][:, fq * FQ * TB:(fq + 1) * FQ * TB])
                    for cgp in range(4):
                        ps = p.psum[4 + cgp]
                        for j in range(FQ):
                            fc = fq * FQ + j
                            p.mm(ps[:, 0:TB], Yr3[:, fc, cgp * 128:(cgp + 1) * 128], ct[:, j * TB:(j + 1) * TB],
                                 start=(fc == 0), stop=False)
                            p.mm(ps[:, 0:TB], Yi3[:, fc, cgp * 128:(cgp + 1) * 128], st_[:, j * TB:(j + 1) * TB],
                                 start=False, stop=(fc == n_t - 1))
                for cgp in range(4):
                    ps = p.psum[4 + cgp]
                    xT = xt_r.next()
                    p.dma("sp", xT, S["hyx"][o * 4 + cgp, :, t0 + tb * TB:t0 + (tb + 1) * TB])
                    if o == 0:
                        zo = zo_r.next()
                        p.tt("dve", zo, ps[:, 0:TB], xT, ALU.mult)
                        pst = p.psum[cgp % 2]
                        nb = TB // 128
                        for b in range(nb):
                            p.tr(pst[:, b * 128:(b + 1) * 128], zo[:, b * 128:(b + 1) * 128], ident)
                        p.copy("act", U3[:, tb * nb:(tb + 1) * nb, cgp * 128:(cgp + 1) * 128],
                               pst[:, 0:nb * 128].rearrange("p (b c) -> p b c", b=nb))
                    else:
                        zb = zb_r.next()
                        p.tt("dve", zb, ps[:, 0:TB], xT, ALU.mult)
                        p.dma("act", S["actT"][4 + cgp, :, t0 + tb * TB:t0 + (tb + 1) * TB], zb)
        p.barrier()
        p.release(mk)
    p.release(mk0)


def phase_even_out(cx, layer, xs, m_scr, S):
    p = cx.p
    e = layer // 2
    mk0 = p.mark()
    w_out = cx.inp("w_out_even", [2, 1024, 1024])
    actT = p.alloc(8 * NTOK, BF16, name="actT")
    aT3 = actT.rearrange("p (g t) -> p g t", g=8)
    for g in range(8):
        p.dma("sp" if g % 2 == 0 else "act", aT3[:, g, :], S["actT"][g])
    wo = p.alloc(8 * 1024, BF16, name="wo_b")
    wo3 = wo.rearrange("p (h c) -> p h c", h=8)
    stg = Ring(p, 2, 8 * 512, name="wstage2")
    cast_weight(p, wo3, w_out[e].rearrange("(h p) c -> p h c", p=128), 1024, stg)
    G1 = {}
    for v in (0, 1):
        G1[v] = p.alloc(1024, name="G1")
        load_row_bc(p, "act", G1[v], m_scr[layer, v:v + 1, 2 * 1024:3 * 1024])
    xr = Ring(p, 2, 1024, name="xres")
    yr = Ring(p, 2, 1024, name="yres")
    for ti in range(NT_ALL):
        v = 1 if ti < 2 else 0
        xt = xr.next()
        p.dma("sp", xt, xs[ti * 128:(ti + 1) * 128, :])
        yt = yr.next()
        for cb in range(2):
            ps = p.psum[(ti * 2 + cb) % 4]
            for h in range(8):
                p.mm(ps, aT3[:, h, ti * 128:(ti + 1) * 128], wo3[:, h, cb * 512:(cb + 1) * 512], start=(h == 0), stop=(h == 7))
            p.tt("dve", yt[:, cb * 512:(cb + 1) * 512], ps, G1[v][:, cb * 512:(cb + 1) * 512], ALU.mult)
        p.tt("pool", yt, yt, xt, ALU.add)
        p.dma("act", xs[ti * 128:(ti + 1) * 128, :], yt)
    p.barrier()
    p.release(mk0)


def phase_final(cx, xs, out):
    p = cx.p
    mk0 = p.mark()
    fn_d = cx.inp("final_norm", [1, 1024])
    g = p.alloc(1024, name="fn_row")
    load_row_bc(p, "act", g, fn_d[0:1, :])
    eps_t = p.alloc(1, name="eps")
    p.memset("dve", eps_t, EPS)
    xr = Ring(p, 2, 1024, name="xt")
    yr = Ring(p, 2, 1024, name="yt")
    junk = p.alloc(1024, name="junk")
    st_r = Ring(p, 2, 4, name="st")
    for ti in range(2, NT_ALL):
        xt = xr.next()
        p.dma("sp", xt, xs[ti * 128:(ti + 1) * 128, :])
        st = st_r.next()
        p.act(junk, xt, AF.Square, accum=st[:, 0:1])
        p.act(st[:, 1:2], st[:, 0:1], AF.Sqrt, scale=1.0 / D, bias=eps_t)
        p.recip(st[:, 2:3], st[:, 1:2])
        yt = yr.next()
        p.stt("dve", yt, xt, st[:, 2:3], g, ALU.mult, ALU.mult)
        p.dma("act", out[(ti - 2) * 128:(ti - 1) * 128, :], yt)
    p.barrier()
    p.release(mk0)


def build_full(cx, xs, m_scr, out, layers=(0, 1, 2, 3)):
    p = cx.p
    for layer in layers:
        last = layer == 3
        phase_mod(cx, layer, m_scr)
        if layer % 2 == 0:
            S = even_scratch(p, layer)
            phase_even_proj(cx, layer, xs, m_scr, S)
            phase_gdn(cx, layer, S)
            phase_gdn_out(cx, layer, S)
            phase_hyena(cx, layer, S)
            phase_even_out(cx, layer, xs, m_scr, S)
        else:
            phase_odd(cx, layer, xs, m_scr, not last)
        phase_peer(cx, layer, xs, m_scr, list(range(2 if last else 0, NT_ALL)))
    phase_final(cx, xs, out)


_BUILD_CACHE = {}


def kernel(**inputs):
    inputs = {k: np.asarray(v) for k, v in inputs.items()}
    if "full" not in _BUILD_CACHE:
        _BUILD_CACHE["full"] = build({})
    cx, nc = _BUILD_CACHE["full"]
    n = 8
    in_maps = [host_inputs(cx, inputs, b) for b in range(n)]
    res = run_bass_kernel_spmd(nc, in_maps, core_ids=list(range(n)))
    return np.stack([np.asarray(res.results[b]["out"]) for b in range(n)], axis=0).astype(np.float32)
```

```python
import numpy as np
import concourse.bass as bass
import concourse.mybir as mybir

F32 = mybir.dt.float32
BF16 = mybir.dt.bfloat16
I32 = mybir.dt.int32
U32 = mybir.dt.uint32
AF = mybir.ActivationFunctionType
ALU = mybir.AluOpType
AX = mybir.AxisListType

ARENA_COLS = 52000
N_DMA_SEMS = 8


class V:
    def __init__(self, ap, key):
        self.ap = ap
        self.key = key

    def __getitem__(self, idx):
        return V(self.ap[idx], self.key)

    def bitcast(self, dt):
        return V(self.ap.bitcast(dt), self.key)

    def rearrange(self, pat, **kw):
        return V(self.ap.rearrange(pat, **kw), self.key)

    def bc(self, shape):
        return V(self.ap.to_broadcast(list(shape)), self.key)

    def k(self, sub):
        return V(self.ap, (self.key, sub))

    def unsq(self, axis):
        return V(self.ap.unsqueeze(axis), self.key)

    def pbc(self, n):
        return V(self.ap.partition_broadcast(n), self.key)

    @property
    def shape(self):
        return self.ap.shape


class Op:
    __slots__ = ("eng", "fn", "deps", "signal", "semkey", "semval", "is_dma")

    def __init__(self, eng, fn, is_dma=False):
        self.eng = eng
        self.fn = fn
        self.deps = []
        self.signal = False
        self.semkey = None
        self.semval = None
        self.is_dma = is_dma


def _ap(x):
    return x.ap if isinstance(x, V) else x


class Prog:
    ENGS = ("pe", "dve", "act", "pool", "sp")

    def __init__(self):
        self.nc = bass.Bass("TRN2", target_bir_lowering=False)
        nc = self.nc
        self.ops = {e: [] for e in self.ENGS}
        self.lastw = {}
        self.readers = {}
        self.arena = nc.alloc_sbuf_tensor("arena", [128, ARENA_COLS], F32)
        self.top = 0
        self.psum = []
        for i in range(8):
            t = nc.alloc_psum_tensor(f"psb{i}", [128, 512], F32)
            self.psum.append(V(t.ap(), f"psb{i}"))
        self.dma_last = {}
        self.dma_rr = {q: 0 for q in ("sp", "act", "pool")}
        self.dma_cnt = {}
        self.uid = 0
        self.drams = {}

    def dram(self, name, shape, dt, kind="Internal"):
        t = self.nc.dram_tensor(name, list(shape), dt, kind=kind)
        v = V(t.ap(), name)
        self.drams[name] = v
        return v

    def alloc(self, cols, dt=F32, name=None, parts=128):
        self.uid += 1
        nbytes = cols * mybir.dt.size(dt)
        c32 = (nbytes + 3) // 4
        c32 = (c32 + 7) // 8 * 8
        a = self.top
        self.top += c32
        assert self.top <= ARENA_COLS, f"SBUF arena overflow: {self.top} > {ARENA_COLS}"
        ap = self.arena[0:parts, a:a + c32]
        if dt != F32:
            ap = ap.bitcast(dt)
        ap = ap[:, 0:cols]
        return V(ap, f"{name or 't'}#{self.uid}")

    def mark(self):
        return self.top

    def release(self, m):
        self.top = m

    def _track(self, op, r, w):
        deps = op.deps
        for v in r:
            k = v.key if isinstance(v, V) else v
            lw = self.lastw.get(k)
            if lw is not None:
                deps.append(lw)
            self.readers.setdefault(k, {})
        for v in w:
            k = v.key if isinstance(v, V) else v
            lw = self.lastw.get(k)
            if lw is not None:
                deps.append(lw)
            for rd in self.readers.get(k, {}).values():
                deps.append(rd)
        for v in r:
            k = v.key if isinstance(v, V) else v
            self.readers[k][self._semslot(op)] = op
        for v in w:
            k = v.key if isinstance(v, V) else v
            self.lastw[k] = op
            self.readers[k] = {}

    def _semslot(self, op):
        return op.semkey

    def op(self, eng, fn, r=(), w=()):
        o = Op(eng, fn)
        o.semkey = eng
        self._track(o, r, w)
        self.ops[eng].append(o)
        return o

    def dma(self, q, out, in_, **kw):
        i = self.dma_rr[q]
        self.dma_rr[q] = (i + 1) % N_DMA_SEMS
        o = Op(q, None, is_dma=True)
        o.semkey = ("dma", q, i)
        o.signal = True
        prev = self.dma_last.get((q, i))
        if prev is not None:
            o.deps.append(prev)
        self.dma_last[(q, i)] = o
        oa, ia = _ap(out), _ap(in_)
        eng = self._eng(q)
        o.fn = lambda: eng.dma_start(out=oa, in_=ia, **kw)
        self._track(o, [in_], [out])
        self.ops[q].append(o)
        return o

    def barrier(self):
        lasts = []
        for e in self.ENGS:
            for o in reversed(self.ops[e]):
                if not o.is_dma and o.fn is not None:
                    lasts.append(o)
                    break
        lasts += list(self.dma_last.values())
        for e in self.ENGS:
            o = Op(e, None)
            o.semkey = e
            o.deps = [d for d in lasts]
            self.ops[e].append(o)
        self.lastw = {}
        self.readers = {}

    def _eng(self, e):
        nc = self.nc
        return {"pe": nc.tensor, "dve": nc.vector, "act": nc.scalar, "pool": nc.gpsimd, "sp": nc.sync}[e]

    def mm(self, out, lhsT, rhs, start=True, stop=True, **kw):
        oa, la, ra = _ap(out), _ap(lhsT), _ap(rhs)
        pe = self.nc.tensor
        return self.op("pe", lambda: pe.matmul(oa, la, ra, start=start, stop=stop, **kw), r=[lhsT, rhs], w=[out])

    def tr(self, out, in_, ident):
        oa, ia, da = _ap(out), _ap(in_), _ap(ident)
        pe = self.nc.tensor
        return self.op("pe", lambda: pe.transpose(oa, ia, da), r=[in_, ident], w=[out])

    def act(self, out, in_, func, bias=None, scale=None, accum=None, eng="act"):
        oa, ia = _ap(out), _ap(in_)
        kw = {}
        r = [in_]
        w = [out]
        if bias is not None:
            kw["bias"] = _ap(bias)
            if isinstance(bias, V):
                r.append(bias)
        if scale is not None:
            kw["scale"] = _ap(scale)
            if isinstance(scale, V):
                r.append(scale)
        if accum is not None:
            kw["accum_out"] = _ap(accum)
            w.append(accum)
        sc = self.nc.scalar
        return self.op("act", lambda: sc.activation(oa, ia, func, **kw), r=r, w=w)

    def ts(self, eng, out, in0, s1, s2, op0, op1=None, accum=None):
        oa, ia = _ap(out), _ap(in0)
        r = [in0]
        w = [out]
        for s in (s1, s2):
            if isinstance(s, V):
                r.append(s)
        kw = {}
        if op1 is not None:
            kw["op1"] = op1
        if accum is not None:
            kw["accum_out"] = _ap(accum)
            w.append(accum)
        e = self._eng(eng)
        a1, a2 = _ap(s1), _ap(s2)
        return self.op(eng, lambda: e.tensor_scalar(oa, ia, a1, a2, op0, **kw), r=r, w=w)

    def tt(self, eng, out, in0, in1, op):
        oa, a0, a1 = _ap(out), _ap(in0), _ap(in1)
        e = self._eng(eng)
        return self.op(eng, lambda: e.tensor_tensor(oa, a0, a1, op), r=[in0, in1], w=[out])

    def stt(self, eng, out, in0, scalar, in1, op0, op1):
        oa, a0, a1, sa = _ap(out), _ap(in0), _ap(in1), _ap(scalar)
        r = [in0, in1]
        if isinstance(scalar, V):
            r.append(scalar)
        e = self._eng(eng)
        return self.op(eng, lambda: e.scalar_tensor_tensor(oa, a0, sa, a1, op0, op1), r=r, w=[out])

    def copy(self, eng, out, in_):
        oa, ia = _ap(out), _ap(in_)
        if eng == "act":
            sc = self.nc.scalar
            return self.op("act", lambda: sc.copy(oa, ia), r=[in_], w=[out])
        e = self._eng(eng)
        return self.op(eng, lambda: e.tensor_copy(oa, ia), r=[in_], w=[out])

    def memset(self, eng, out, val):
        oa = _ap(out)
        e = self._eng(eng)
        return self.op(eng, lambda: e.memset(oa, val), r=[], w=[out])

    def reduce(self, eng, out, in_, op, axis=AX.X):
        oa, ia = _ap(out), _ap(in_)
        e = self._eng(eng)
        return self.op(eng, lambda: e.tensor_reduce(oa, ia, axis, op), r=[in_], w=[out])

    def recip(self, out, in_):
        oa, ia = _ap(out), _ap(in_)
        e = self.nc.vector
        return self.op("dve", lambda: e.reciprocal(oa, ia), r=[in_], w=[out])

    def emit(self):
        nc = self.nc
        for e in self.ENGS:
            for o in self.ops[e]:
                for d in o.deps:
                    if d.eng == "pe" and o.eng == "pe" and not d.is_dma and not o.is_dma:
                        continue
                    d.signal = True
        cnt = {}
        n_ins = 0
        for e in self.ENGS:
            for o in self.ops[e]:
                if o.fn is None:
                    continue
                n_ins += 1
                if o.signal:
                    inc = 16 if o.is_dma else 1
                    cnt[o.semkey] = cnt.get(o.semkey, 0) + inc
                    o.semval = cnt[o.semkey]
        self.n_ins = n_ins
        self.sem_final = cnt
        semkeys = list(cnt.keys())
        from contextlib import ExitStack
        with ExitStack() as es:
            sems = {}
            for i, k in enumerate(semkeys):
                sems[k] = es.enter_context(nc.semaphore(f"s{i}"))
            block = es.enter_context(nc.Block())

            def replay(ename):
                def f(eng):
                    waited = {}
                    for o in self.ops[ename]:
                        need = {}
                        for d in o.deps:
                            if d.semval is None:
                                continue
                            if ename == "pe" and d.eng == "pe" and not d.is_dma and not o.is_dma:
                                continue
                            if need.get(d.semkey, 0) < d.semval:
                                need[d.semkey] = d.semval
                        for k, v in need.items():
                            if waited.get(k, 0) < v:
                                eng.wait_ge(sems[k], v)
                                waited[k] = v
                        if o.fn is None:
                            continue
                        ins = o.fn()
                        if o.signal:
                            ins.then_inc(sems[o.semkey], 16 if o.is_dma else 1)
                return f

            block.tensor(replay("pe"))
            block.vector(replay("dve"))
            block.scalar(replay("act"))
            block.gpsimd(replay("pool"))
            block.sync(replay("sp"))
        return nc


from concourse.bass_utils import run_bass_kernel_spmd
IndirectOffsetOnAxis = bass.IndirectOffsetOnAxis

D = 1024
LAT = 4096
CTX = 256
NT_ALL = (LAT + CTX) // 128
EPS = 1e-6
NEG = -1.0e30


class Ring:
    def __init__(self, p, n, cols, dt=F32, name="ring", parts=128):
        self.bufs = [p.alloc(cols, dt, name=f"{name}{i}", parts=parts) for i in range(n)]
        self.i = 0

    def next(self):
        b = self.bufs[self.i % len(self.bufs)]
        self.i += 1
        return b


class Ctx:
    def __init__(self):
        self.p = Prog()
        self.inputs = {}
        self.in_dt = {}

    def inp(self, name, shape, dt=F32):
        if name not in self.p.drams:
            self.p.dram(name, shape, dt, kind="ExternalInput")
            self.inputs[name] = tuple(shape)
            self.in_dt[name] = dt
        return self.p.drams[name]


def load_row_bc(p, q, dst, src_row):
    return p.dma(q, dst, V(src_row.ap.to_broadcast([dst.shape[0], src_row.shape[1]]), src_row.key))


def phase_mod(cx, layer, m_scr):
    p = cx.p
    mk = p.mark()
    cc = cx.inp("cc", [128, 16])
    w_ada = cx.inp("w_ada", [4, 1024, 6144])
    b_ada = cx.inp("b_ada", [4, 6144])
    cct = p.alloc(16)
    sil = p.alloc(16)
    p.dma("sp", cct, cc)
    p.act(sil, cct, AF.Silu)
    silv = sil.rearrange("p (a k) -> p a k", a=2)
    wr = Ring(p, 2, 8 * 512, name="wada")
    br = Ring(p, 2, 512, name="bada")
    mr = Ring(p, 2, 512, name="mrow")
    wsrc = w_ada[layer].rearrange("(k p) c -> p k c", p=128)
    for cb in range(12):
        wt = wr.next()
        p.dma("sp", wt.rearrange("p (k c) -> p k c", k=8), wsrc[:, :, cb * 512:(cb + 1) * 512])
        bt = br.next()
        load_row_bc(p, "act", bt[0:2, :], b_ada[layer:layer + 1, cb * 512:(cb + 1) * 512])
        ps = p.psum[cb % 2]
        for k in range(8):
            p.mm(ps[0:2, :], silv[:, :, k], wt[:, k * 512:(k + 1) * 512], start=(k == 0), stop=(k == 7))
        mt = mr.next()
        p.tt("dve", mt[0:2, :], ps[0:2, :], bt[0:2, :], ALU.add)
        p.dma("sp", m_scr[layer, :, cb * 512:(cb + 1) * 512], mt[0:2, :])
    p.barrier()
    p.release(mk)


class NormMod:
    def __init__(self, cx, layer, xs, m_scr, norm_name, i_shift, i_scale, nbuf=2, shared_ab=False, junk=None):
        p = cx.p
        self.p = p
        self.xs = xs
        nrm = cx.inp(norm_name, [4, 1024])
        self.A = {}
        self.B = {}
        self.shared_ab = shared_ab
        self.layer, self.m_scr, self.i_shift, self.i_scale = layer, m_scr, i_shift, i_scale
        g = p.alloc(1024, name="g_row")
        self.g = g
        load_row_bc(p, "act", g, nrm[layer:layer + 1, :])
        if shared_ab:
            self.Ash = p.alloc(1024, name="A_row")
            self.Bsh = p.alloc(1024, name="B_row")
            self.cur_v = None
        else:
            for v in (0, 1):
                a = p.alloc(1024, name="A_row")
                b = p.alloc(1024, name="B_row")
                self._load_ab(a, b, v)
                self.A[v] = a
                self.B[v] = b
        self.xr = Ring(p, nbuf, 1024, name="xt")
        self.hr = Ring(p, nbuf, 1024, name="ht")
        self.junk = junk if junk is not None else p.alloc(1024, name="junk")
        self.st = Ring(p, nbuf, 4, name="stat")

    def _load_ab(self, a, b, v):
        p = self.p
        load_row_bc(p, "act", a, self.m_scr[self.layer, v:v + 1, self.i_scale * 1024:(self.i_scale + 1) * 1024])
        load_row_bc(p, "act", b, self.m_scr[self.layer, v:v + 1, self.i_shift * 1024:(self.i_shift + 1) * 1024])
        p.stt("dve", a, a, 1.0, self.g, ALU.add, ALU.mult)

    def tile(self, ti):
        p = self.p
        v = 1 if ti < 2 else 0
        if self.shared_ab:
            if self.cur_v != v:
                self._load_ab(self.Ash, self.Bsh, v)
                self.cur_v = v
            self.A[v] = self.Ash
            self.B[v] = self.Bsh
        xt = self.xr.next()
        p.dma("sp", xt, self.xs[ti * 128:(ti + 1) * 128, :])
        st = self.st.next()
        p.act(self.junk, xt, AF.Square, accum=st[:, 0:1])
        p.act(st[:, 1:2], st[:, 0:1], AF.Sqrt, scale=1.0 / D, bias=self.eps_ap())
        p.recip(st[:, 2:3], st[:, 1:2])
        ht = self.hr.next()
        p.stt("dve", ht, xt, st[:, 2:3], self.A[v], ALU.mult, ALU.mult)
        p.tt("pool", ht, ht, self.B[v], ALU.add)
        return xt, ht

    def eps_ap(self):
        if not hasattr(self, "_eps"):
            self._eps = self.p.alloc(1, name="eps")
            self.p.memset("dve", self._eps, EPS)
        return self._eps


def transpose_tile(p, ident, src, dst_fn, banks, evac_eng="act"):
    for j in range(2):
        ps = p.psum[banks[j]]
        for c in range(4):
            k = 4 * j + c
            p.tr(ps[:, c * 128:(c + 1) * 128], src[:, k * 128:(k + 1) * 128], ident)
        dst = dst_fn(j)
        p.copy(evac_eng, dst, ps.rearrange("p (c t) -> p c t", c=4))


def phase_peer(cx, layer, xs, m_scr, tiles):
    p = cx.p
    mk = p.mark()
    ident_d = cx.inp("ident", [128, 128])
    iota_d = cx.inp("iota16", [128, 16])
    wq_d = cx.inp("peer_wq", [4, 1024, 2048])
    keysT_d = cx.inp("peer_keysT", [4, 128, 2048])
    pu = cx.inp("peer_u", [4, 16384, 1024])
    pv = cx.inp("peer_v", [4, 16384, 1024])
    pu_flat = pu.rearrange("l e d -> (l e) d")
    pv_flat = pv.rearrange("l e d -> (l e) d")

    uvb_d = p.dram(f"peer_uvb{layer}", [16384, 2048], BF16)
    mkc = p.mark()
    RB = 8
    cin = Ring(p, 2, RB * 1024, name="cast_in")
    cout = Ring(p, 2, RB * 1024, BF16, name="cast_out")
    ci = 0
    for (src, dst) in ((pu[layer], uvb_d[:, 0:1024]), (pv[layer], uvb_d[:, 1024:2048])):
        for r0 in range(0, 16384, 128 * RB):
            a = cin.next()
            b = cout.next()
            p.dma("sp", a, src[r0:r0 + 128 * RB, :].rearrange("(p r) d -> p (r d)", r=RB))
            eng = ("dve", "act", "pool")[ci % 3]
            p.copy(eng, b, a)
            p.dma("act", dst[r0:r0 + 128 * RB, :].rearrange("(p r) d -> p r d", r=RB), b.rearrange("p (r d) -> p r d", r=RB))
            ci += 1
    p.barrier()
    p.release(mkc)
    ident = p.alloc(128, name="ident")
    p.dma("act", ident, ident_d)
    iota = p.alloc(16, name="iota")
    p.dma("act", iota, iota_d)
    wq = p.alloc(8 * 2048, name="wq")
    p.dma("sp", wq.rearrange("p (k c) -> p k c", k=8), wq_d[layer].rearrange("(k p) c -> p k c", p=128))
    keysT = p.alloc(2048, name="keysT")
    p.dma("act", keysT, keysT_d[layer])
    G2 = {}
    for v in (0, 1):
        if v == 1 and tiles[0] >= 2:
            continue
        G2[v] = p.alloc(1024, name="G2")
        load_row_bc(p, "act", G2[v], m_scr[layer, v:v + 1, 5 * 1024:6 * 1024])
    junk = p.alloc(1024, name="pjunk")
    nm = NormMod(cx, layer, xs, m_scr, "norm2", 3, 4, nbuf=2, shared_ab=True, junk=junk)

    h2T = p.alloc(1024, name="h2T")
    qTt = p.alloc(2048, name="qTt")
    sc = p.alloc(2048, name="sc")
    sc2 = Ring(p, 2, 128, name="sc2")
    sv = p.alloc(256, name="sv")
    si = p.alloc(256, U32, name="si")
    sif = p.alloc(256, name="sif")
    cand = sc
    cand2 = Ring(p, 2, 256, name="cand2")
    ts_ = p.alloc(128, name="ts")
    pos = p.alloc(128, U32, name="pos")
    ipos = p.alloc(128, U32, name="ipos")
    jpos = p.alloc(128, U32, name="jpos")
    iposf = p.alloc(128, name="iposf")
    jposf = p.alloc(128, name="jposf")
    eq = qTt
    asel = p.alloc(128, name="asel")
    bsel = p.alloc(128, name="bsel")
    eidf = p.alloc(128, name="eidf")
    eid_r = Ring(p, 2, 128, U32, name="eid")
    gate_r = Ring(p, 2, 128, name="gate")
    gz = p.alloc(16, name="gz")
    actv = p.alloc(128, name="actv")
    coef = p.alloc(128, name="coef")
    acc = p.alloc(1024, name="acc")
    ug = Ring(p, 12, 2048, BF16, name="uvg")
    gl = p.alloc(128, name="gelu_a")

    sv4 = sv.rearrange("p (h a i) -> p h a i", h=8, a=2)
    sif4 = sif.rearrange("p (h a i) -> p h a i", h=8, a=2)
    cand4 = cand.rearrange("p (h i j) -> p h i j", h=8, i=16)
    ts3 = ts_.rearrange("p (h k) -> p h k", h=8)
    eq4 = eq.rearrange("p (h k i) -> p h k i", h=8, k=16)
    iota_b = iota.unsq(1).unsq(1).bc([128, 8, 16, 16])

    def routing(ti, R):
        v = 1 if ti < 2 else 0
        xt, h2 = nm.tile(ti)
        eid = eid_r.next()
        gate = gate_r.next()
        R.update(ti=ti, v=v, xt=xt, h2=h2, eid=eid, gate=gate)
        yield
        transpose_tile(p, ident, h2, lambda j: h2T.rearrange("p (k t) -> p k t", k=8)[:, 4 * j:4 * j + 4, :], (0, 1))
        yield
        for g4 in range(4):
            ps = p.psum[2 + g4]
            for c in range(4):
                hp = 4 * g4 + c
                for k in range(8):
                    p.mm(ps[:, c * 128:(c + 1) * 128], wq[:, k * 2048 + hp * 128:k * 2048 + (hp + 1) * 128],
                         h2T[:, k * 128:(k + 1) * 128], start=(k == 0), stop=(k == 7))
                yield
            p.copy("act", qTt[:, g4 * 512:(g4 + 1) * 512], ps)
        for g4 in range(4):
            ps = p.psum[g4]
            for c in range(4):
                hp = 4 * g4 + c
                p.mm(ps[:, c * 128:(c + 1) * 128], qTt[:, hp * 128:(hp + 1) * 128], keysT[:, hp * 128:(hp + 1) * 128])
            p.copy("act", sc[:, g4 * 512:(g4 + 1) * 512], ps)
            yield
        for hp in range(16):
            s_hp = sc[:, hp * 128:(hp + 1) * 128]
            s2_hp = sc2.next()
            o8a = sv[:, hp * 16:hp * 16 + 8]
            o8b = sv[:, hp * 16 + 8:hp * 16 + 16]
            _max8(p, o8a, s_hp)
            _match_replace(p, s2_hp, o8a, s_hp)
            _max8(p, o8b, s2_hp)
            _max_index(p, si[:, hp * 16:hp * 16 + 8], o8a, s_hp)
            _max_index(p, si[:, hp * 16 + 8:hp * 16 + 16], o8b, s_hp)
            yield
        p.copy("dve", sif, si)
        p.tt("dve", cand4, sv4[:, :, 0, :].unsq(3).bc([128, 8, 16, 16]), sv4[:, :, 1, :].unsq(2).bc([128, 8, 16, 16]), ALU.add)
        for h in range(8):
            c_h = cand[:, h * 256:(h + 1) * 256]
            c2_h = cand2.next()
            o8a = ts_[:, h * 16:h * 16 + 8]
            o8b = ts_[:, h * 16 + 8:h * 16 + 16]
            _max8(p, o8a, c_h)
            _match_replace(p, c2_h, o8a, c_h)
            _max8(p, o8b, c2_h)
            _max_index(p, pos[:, h * 16:h * 16 + 8], o8a, c_h)
            _max_index(p, pos[:, h * 16 + 8:h * 16 + 16], o8b, c_h)
            yield
        p.ts("dve", ipos, pos, 4, None, ALU.logical_shift_right)
        p.ts("dve", jpos, pos, 15, None, ALU.bitwise_and)
        p.copy("dve", iposf, ipos)
        p.copy("dve", jposf, jpos)
        for (dst, posf, a) in ((asel, iposf, 0), (bsel, jposf, 1)):
            pf3 = posf.rearrange("p (h k) -> p h k", h=8)
            p.tt("dve", eq4, iota_b, pf3.unsq(3).bc([128, 8, 16, 16]), ALU.is_equal)
            p.tt("dve", eq4, eq4, sif4[:, :, a, :].unsq(2).bc([128, 8, 16, 16]), ALU.mult)
            p.reduce("dve", dst.rearrange("p (h k) -> p h k", h=8), eq4, ALU.add, AX.X)
            yield
        p.stt("dve", eidf, asel, 128.0, bsel, ALU.mult, ALU.add)
        p.copy("dve", eid, eidf)
        p.tt("dve", gate.rearrange("p (h k) -> p h k", h=8), ts3, ts3[:, :, 0:1].bc([128, 8, 16]), ALU.subtract)
        p.act(gate, gate, AF.Exp)
        p.reduce("dve", gz[:, 0:8], gate.rearrange("p (h k) -> p h k", h=8), ALU.add, AX.X)
        p.recip(gz[:, 8:16], gz[:, 0:8])
        p.tt("dve", gate.rearrange("p (h k) -> p h k", h=8), gate.rearrange("p (h k) -> p h k", h=8),
             gz[:, 8:16].unsq(2).bc([128, 8, 16]), ALU.mult)
        yield

    def slots(R, gen):
        ti, v, xt, h2, eid, gate = R["ti"], R["v"], R["xt"], R["h2"], R["eid"], R["gate"]
        accA, accB = p.psum[6], p.psum[7]
        GS = 4
        pend = None

        def finish(g):
            s0_, bs_ = g
            p.tt("dve", coef[:, s0_:s0_ + GS], gl[:, s0_:s0_ + GS], gate[:, s0_:s0_ + GS], ALU.mult)
            dg = dg_r.next()
            dg3 = dg.rearrange("p (g c) -> p g c", g=GS)
            p.tt("dve", dg3, ident_b.unsq(1).bc([128, GS, 128]), coef[:, s0_:s0_ + GS].unsq(2).bc([128, GS, 128]), ALU.mult)
            for j, s in enumerate(range(s0_, s0_ + GS)):
                p.mm(accA, dg[:, j * 128:(j + 1) * 128], bs_[j][:, 1024:1536], start=(s == 0), stop=(s == 127))
                p.mm(accB, dg[:, j * 128:(j + 1) * 128], bs_[j][:, 1536:2048], start=(s == 0), stop=(s == 127))
                if gen is not None:
                    next(gen, None)

        for s0 in range(0, 128, GS):
            bs = []
            for s in range(s0, s0 + GS):
                b = ug.next()
                bs.append(b)
                _gather(p, b, uvb_d, eid[:, s:s + 1])
                _ttr(p, junk, b[:, 0:1024], h2, actv[:, s:s + 1])
            p.act(gl[:, s0:s0 + GS], actv[:, s0:s0 + GS], AF.Gelu)
            if pend is not None:
                finish(pend)
            pend = (s0, bs)
        finish(pend)
        p.tt("dve", acc[:, 0:512], accA, G2[v][:, 0:512], ALU.mult)
        p.tt("dve", acc[:, 512:1024], accB, G2[v][:, 512:1024], ALU.mult)
        p.tt("dve", acc, acc, xt, ALU.add)
        p.dma("sp", xs[ti * 128:(ti + 1) * 128, :], acc)

    ident_b = p.alloc(128, BF16, name="ident_b")
    p.copy("dve", ident_b, ident)
    dg_r = Ring(p, 3, 4 * 128, BF16, name="diag")
    cur = {}
    for _ in routing(tiles[0], cur):
        pass
    for i in range(len(tiles)):
        nxt = {}
        gen = routing(tiles[i + 1], nxt) if i + 1 < len(tiles) else None
        slots(cur, gen)
        if gen is not None:
            for _ in gen:
                pass
        cur = nxt
    p.barrier()
    p.release(mk)


def _max8(p, out, in_):
    oa, ia = out.ap, in_.ap
    e = p.nc.vector
    return p.op("dve", lambda: e.max(oa, ia), r=[in_], w=[out])


def _max_index(p, out, in_max, in_values):
    oa, ma, va = out.ap, in_max.ap, in_values.ap
    e = p.nc.vector
    return p.op("dve", lambda: e.max_index(oa, ma, va), r=[in_max, in_values], w=[out])


def _match_replace(p, out, in_to_replace, in_values):
    oa, ra, va = out.ap, in_to_replace.ap, in_values.ap
    e = p.nc.vector
    return p.op("dve", lambda: e.match_replace(oa, ra, va, NEG), r=[in_to_replace, in_values], w=[out])


def _ttr(p, out, in0, in1, accum):
    oa, a0, a1, ac = out.ap, in0.ap, in1.ap, accum.ap
    e = p.nc.vector
    return p.op("dve", lambda: e.scalar_tensor_tensor(oa, a0, 1.0, a1, ALU.mult, ALU.mult, accum_out=ac),
                r=[in0, in1], w=[out, accum])


def _gather(p, out, table, idx):
    q = "pool"
    i = p.dma_rr[q]
    p.dma_rr[q] = (i + 1) % N_DMA_SEMS
    o = Op(q, None, is_dma=True)
    o.semkey = ("dma", q, i)
    o.signal = True
    prev = p.dma_last.get((q, i))
    if prev is not None:
        o.deps.append(prev)
    p.dma_last[(q, i)] = o
    oa, ta, ia = out.ap, table.ap, idx.ap
    g = p.nc.gpsimd
    o.fn = lambda: g.indirect_dma_start(oa, None, ta, IndirectOffsetOnAxis(ia, 0))
    p._track(o, [table, idx], [out])
    p.ops[q].append(o)
    return o


def dram_copy(p, dst, src, rows, q="sp", chunk=512):
    for r0 in range(0, rows, chunk):
        r1 = min(rows, r0 + chunk)
        p.dma(q, dst[r0:r1, :], src[r0:r1, :])


def build(cfg):
    cx = Ctx()
    p = cx.p
    xs = p.dram("xs", [LAT + CTX, D], F32)
    m_scr = p.dram("m_scr", [4, 2, 6144], F32)
    x_in = cx.inp("x", [LAT, D])
    ctx_in = cx.inp("ctx", [CTX, D])
    out = p.dram("out", [LAT, D], F32, kind="ExternalOutput")
    dram_copy(p, xs[CTX:, :], x_in, LAT)
    dram_copy(p, xs[0:CTX, :], ctx_in, CTX, q="act")
    p.barrier()
    test = cfg.get("test")
    if test is None:
        build_full(cx, xs, m_scr, out)
    if test == "layers":
        build_full(cx, xs, m_scr, out, layers=cfg["layers"])
        dbg = p.dram("dbg_xs", [LAT + CTX, D], F32, kind="ExternalOutput")
        dram_copy(p, dbg, xs, LAT + CTX)
    if test == "odd":
        layer = cfg["layer"]
        phase_mod(cx, layer, m_scr)
        phase_odd(cx, layer, xs, m_scr, layer != 3)
        dbg = p.dram("dbg_xs", [LAT + CTX, D], F32, kind="ExternalOutput")
        dram_copy(p, dbg, xs, LAT + CTX)
    if test == "even":
        layer = cfg["layer"]
        S = even_scratch(p, layer)
        phase_mod(cx, layer, m_scr)
        for ph in cfg["phases"]:
            {"proj": phase_even_proj, "gdn": phase_gdn, "gdn_out": phase_gdn_out, "hy": phase_hyena}[ph](*((cx, layer, xs, m_scr, S) if ph == "proj" else (cx, layer, S)))
        for nm in cfg.get("dump", []):
            src = S[nm]
            shp = list(src.shape)
            dd = p.dram("dbg_" + nm, shp, src.ap.dtype, kind="ExternalOutput")
            if len(shp) == 3:
                for g in range(shp[0]):
                    p.dma("sp", dd[g], src[g])
            else:
                dram_copy(p, dd, src, shp[0], chunk=1024)
    if test == "peer":
        layer = cfg["layer"]
        phase_mod(cx, layer, m_scr)
        phase_peer(cx, layer, xs, m_scr, cfg["tiles"])
        dbg = p.dram("dbg_xs", [LAT + CTX, D], F32, kind="ExternalOutput")
        dram_copy(p, dbg, xs, LAT + CTX)
        dbm = p.dram("dbg_m", [2, 6144], F32, kind="ExternalOutput")
        p.dma("sp", dbm, m_scr[layer])
    p.barrier()
    nc = p.emit()
    return cx, nc


def host_inputs(cx, inputs, b):
    m = {}
    f32 = np.float32
    for name in cx.inputs:
        if name == "x":
            a = inputs["x"][b]
        elif name == "ctx":
            a = inputs["ctx"][b]
        elif name == "cc":
            a = np.concatenate([inputs["c"][b].reshape(8, 128).T, inputs["c_ctx"].reshape(8, 128).T], axis=1)
        elif name == "ident":
            a = np.eye(128, dtype=f32)
        elif name == "iota16":
            a = np.tile(np.arange(16, dtype=f32)[None, :], (128, 1))
        elif name == "rope_cs":
            a = rope_table()
        elif name == "conv_wT":
            a = inputs["conv_w"].reshape(2, 3, 24, 128).transpose(0, 3, 2, 1).reshape(2, 128, 72)
        elif name == "gdn_masks":
            i = np.arange(64)[:, None]; j = np.arange(64)[None, :]
            a = np.concatenate([(i <= j), (i >= j), (i < j), (i > j)], axis=1).astype(f32)
        elif name == "final_norm":
            a = inputs["final_norm"].reshape(1, 1024)
        elif name == "peer_keysT":
            a = inputs["peer_keys"].transpose(0, 4, 1, 2, 3).reshape(4, 128, 2048)
        elif name.startswith("dft"):
            L = int(name[3:].split("_")[0])
            a = dft_tables(L)[name.split("_")[1]]
        elif name.startswith("hy_zT"):
            a = hyena_consts(int(name[5:]))[0]
        elif name.startswith("hy_win"):
            a = hyena_consts(int(name[6:]))[1]
        else:
            a = inputs[name]
        if cx.in_dt[name] == BF16:
            m[name] = np.ascontiguousarray(a)
            assert m[name].dtype == _mld.bfloat16
        else:
            m[name] = np.ascontiguousarray(a, dtype=f32)
        assert m[name].shape == cx.inputs[name], (name, m[name].shape, cx.inputs[name])
    return m


def make_hT(cx, layer, xs, m_scr, ident, hT):
    p = cx.p
    mk = p.mark()
    nm = NormMod(cx, layer, xs, m_scr, "norm1", 0, 1, nbuf=2)
    hT3 = hT.rearrange("p (k t) -> p k t", k=8)
    for ti in range(NT_ALL):
        xt, h = nm.tile(ti)
        b0 = (ti % 2) * 2
        transpose_tile(p, ident, h, lambda j: hT3[:, 4 * j:4 * j + 4, ti * 128:(ti + 1) * 128], (b0, b0 + 1),
                       evac_eng=("act" if ti % 2 == 0 else "dve"))
    p.barrier()
    p.release(mk)


def cast_weight(p, dst3, src3, ncols, stage_ring, blk=512):
    K = dst3.shape[1]
    i = 0
    for c0 in range(0, ncols, blk):
        c1 = min(ncols, c0 + blk)
        st = stage_ring.next()
        sv = st.rearrange("p (k c) -> p k c", k=K)[:, :, 0:c1 - c0]
        p.dma("sp" if i % 2 == 0 else "act", sv, src3[:, :, c0:c1])
        p.copy("pool" if i % 2 == 0 else "dve", dst3[:, :, c0:c1], sv)
        i += 1


def phase_odd(cx, layer, xs, m_scr, need_ctx):
    import math
    p = cx.p
    o = layer // 2
    lam_init = 0.8 - 0.6 * math.exp(-0.3 * layer)
    mk0 = p.mark()
    ident_d = cx.inp("ident", [128, 128])
    w_qkv = cx.inp("w_qkv", [2, 1024, 3072])
    w_out = cx.inp("w_out_odd", [2, 1024, 1024])
    rope_d = cx.inp("rope_cs", [LAT, 64])
    subln_d = cx.inp("subln", [2, 128])
    lam_d = {n: cx.inp(n, [2, 64]) for n in ("lam_q1", "lam_k1", "lam_q2", "lam_k2")}
    NTOK = LAT + CTX
    qkT_d = p.dram(f"qkT_d{layer}", [16, 128, NTOK], BF16)
    v_d = p.dram(f"v_d{layer}", [NTOK, 1024], BF16)

    ident = p.alloc(128, name="ident")
    p.dma("act", ident, ident_d)
    hT = p.alloc(8 * NTOK, BF16, name="hT")
    make_hT(cx, layer, xs, m_scr, ident, hT)
    hT3 = hT.rearrange("p (k t) -> p k t", k=8)

    mk = p.mark()
    wb = p.alloc(8 * 3072, BF16, name="wqkv_b")
    wb3 = wb.rearrange("p (k c) -> p k c", k=8)
    stg = Ring(p, 2, 8 * 512, name="wstage")
    cast_weight(p, wb3, w_qkv[o].rearrange("(k p) c -> p k c", p=128), 3072, stg)
    qk_r = Ring(p, 2, 2048, name="qk_t")
    rot_r = Ring(p, 2, 2048, name="qk_rot")
    tmp_r = Ring(p, 2, 1024, name="rtmp")
    cs_r = Ring(p, 2, 64, name="cs")
    qkTs_r = Ring(p, 2, 16 * 128, BF16, name="qkTs")
    vs_r = Ring(p, 2, 1024, BF16, name="vs")
    for ti in range(NT_ALL):
        qk = qk_r.next()
        for cb in range(6):
            ps = p.psum[(ti * 6 + cb) % 4]
            for k in range(8):
                p.mm(ps, hT3[:, k, ti * 128:(ti + 1) * 128], wb3[:, k, cb * 512:(cb + 1) * 512], start=(k == 0), stop=(k == 7))
            if cb < 4:
                p.copy("act", qk[:, cb * 512:(cb + 1) * 512], ps)
            else:
                if cb == 4:
                    vs = vs_r.next()
                p.copy("act", vs[:, (cb - 4) * 512:(cb - 3) * 512], ps)
        p.dma("act", v_d[ti * 128:(ti + 1) * 128, :], vs)
        if ti >= 2:
            cs = cs_r.next()
            p.dma("sp", cs, rope_d[(ti - 2) * 128:(ti - 1) * 128, :])
            rot = rot_r.next()
            x5 = qk.rearrange("p (g a h f) -> p g a h f", g=32, a=2, h=2)
            r5 = rot.rearrange("p (g a h f) -> p g a h f", g=32, a=2, h=2)
            cosb = cs[:, 0:32].rearrange("p (a f) -> p a f", a=2).unsq(1).bc([128, 32, 2, 16])
            sinb = cs[:, 32:64].rearrange("p (a f) -> p a f", a=2).unsq(1).bc([128, 32, 2, 16])
            x1, x2 = x5[:, :, :, 0, :], x5[:, :, :, 1, :]
            t = tmp_r.next().rearrange("p (g a f) -> p g a f", g=32, a=2)
            t2 = tmp_r.next().rearrange("p (g a f) -> p g a f", g=32, a=2)
            p.tt("dve", t, x2, sinb, ALU.mult)
            p.tt("pool", t2, x1, sinb, ALU.mult)
            p.tt("dve", r5[:, :, :, 0, :], x1, cosb, ALU.mult)
            p.tt("pool", r5[:, :, :, 1, :], x2, cosb, ALU.mult)
            p.tt("dve", r5[:, :, :, 0, :], r5[:, :, :, 0, :], t, ALU.subtract)
            p.tt("pool", r5[:, :, :, 1, :], r5[:, :, :, 1, :], t2, ALU.add)
            src = rot
        else:
            src = qk
        qkTs = qkTs_r.next()
        for g4 in range(4):
            ps = p.psum[4 + g4]
            for c in range(4):
                blk = 4 * g4 + c
                p.tr(ps[:, c * 128:(c + 1) * 128], src[:, blk * 128:(blk + 1) * 128], ident)
            p.copy("dve" if g4 % 2 == 0 else "act", qkTs[:, g4 * 512:(g4 + 1) * 512], ps)
        p.dma("sp", qkT_d[:, :, ti * 128:(ti + 1) * 128].rearrange("g p t -> p g t"),
              qkTs.rearrange("p (g t) -> p g t", g=16))
    p.barrier()
    p.release(mk)
    p.release(mk0)

    mk = p.mark()
    onT = p.alloc(8 * NTOK, BF16, name="onT")
    onT3 = onT.rearrange("p (h t) -> p h t", h=8)
    ones_b = p.alloc(128, BF16, name="ones_b")
    p.memset("dve", ones_b, 1.0)
    eps_t = p.alloc(1, name="eps")
    p.memset("dve", eps_t, EPS)
    lt = p.alloc(4 * 64, name="lamv")
    for i, n in enumerate(("lam_q1", "lam_k1", "lam_q2", "lam_k2")):
        load_row_bc(p, "act", lt[:, i * 64:(i + 1) * 64], lam_d[n][o:o + 1, :])
    ls = p.alloc(8, name="lams")
    lj = p.alloc(64, name="lamj")
    _ttr(p, lj, lt[:, 0:64], lt[:, 64:128], ls[:, 0:1])
    _ttr(p, lj, lt[:, 128:192], lt[:, 192:256], ls[:, 1:2])
    p.act(ls[:, 2:4], ls[:, 0:2], AF.Exp)
    p.stt("dve", ls[:, 4:5], ls[:, 3:4], -lam_init, ls[:, 2:3], ALU.add, ALU.subtract)
    neg_lam = ls[:, 4:5]
    sg = p.alloc(2, name="sg")
    p.dma("act", sg[:, 0:1], subln_d[o].rearrange("(p o) -> p o", o=1))
    p.ts("dve", sg[:, 1:2], sg[:, 0:1], 1.0 - lam_init, None, ALU.mult)
    sgs = sg[:, 1:2]

    mk_att = p.mark()
    kT_r = Ring(p, 2, NTOK, BF16, name="kT_h")
    qT_r = Ring(p, 2, NTOK, BF16, name="qT_h")
    v_r = Ring(p, 2, NT_ALL * 128, BF16, name="v_h")
    pt_r = Ring(p, 4, 512, BF16, name="PT")
    rz_r = Ring(p, 2, 512, name="rz")
    o0_r = Ring(p, 2, 512, name="o0")
    o1_r = Ring(p, 2, 512, name="o1")
    sq_r = Ring(p, 2, 512, BF16, name="sq")
    rs_r = Ring(p, 2, 512, name="rs")
    scale = 0.125
    it = 0
    for h in range(8):
        kT = kT_r.next()
        qT = qT_r.next()
        vh = v_r.next()
        p.dma("sp", kT, qkT_d[8 + h])
        p.dma("act", qT, qkT_d[h])
        p.dma("sp", vh.rearrange("p (t d) -> p t d", d=128),
              v_d[:, h * 128:(h + 1) * 128].rearrange("(t p) d -> p t d", p=128))
        blocks = [(CTX + qb * 512, 512, list(range(NT_ALL))) for qb in range(LAT // 512)]
        if need_ctx:
            blocks.append((0, CTX, [0, 1]))
        for (q0, nq, kts) in blocks:
            o0 = o0_r.next()
            o1 = o1_r.next()
            for m in range(2):
                psO = p.psum[2 + (it % 2)]
                psZ = p.psum[4 + (it % 2)]
                it += 1
                def s_mm(i):
                    kt_ = kts[i]
                    p.mm(p.psum[i % 2][:, 0:nq], kT[m * 64:(m + 1) * 64, kt_ * 128:(kt_ + 1) * 128], qT[m * 64:(m + 1) * 64, q0:q0 + nq])
                s_mm(0)
                for i, kt in enumerate(kts):
                    psS = p.psum[i % 2]
                    if i + 1 < len(kts):
                        s_mm(i + 1)
                    pt = pt_r.next()
                    p.act(pt[:, 0:nq], psS[:, 0:nq], AF.Exp, scale=scale)
                    p.mm(psO[:, 0:nq], vh[:, kt * 128:(kt + 1) * 128], pt[:, 0:nq], start=(i == 0), stop=(i == len(kts) - 1))
                    p.mm(psZ[:, 0:nq], ones_b, pt[:, 0:nq], start=(i == 0), stop=(i == len(kts) - 1))
                rz = rz_r.next()
                p.recip(rz[:, 0:nq], psZ[:, 0:nq])
                if m == 0:
                    p.tt("dve", o0[:, 0:nq], psO[:, 0:nq], rz[:, 0:nq], ALU.mult)
                else:
                    p.tt("dve", o1[:, 0:nq], psO[:, 0:nq], rz[:, 0:nq], ALU.mult)
                    p.stt("dve", o0[:, 0:nq], o1[:, 0:nq], neg_lam, o0[:, 0:nq], ALU.mult, ALU.add)
            sq = sq_r.next()
            p.tt("pool", sq[:, 0:nq], o0[:, 0:nq], o0[:, 0:nq], ALU.mult)
            psR = p.psum[6]
            p.mm(psR[:, 0:nq], ones_b, sq[:, 0:nq])
            rs = rs_r.next()
            p.act(rs[:, 0:nq], psR[:, 0:nq], AF.Sqrt, scale=1.0 / 128, bias=eps_t)
            p.recip(rs[:, 0:nq], rs[:, 0:nq])
            p.stt("dve", onT3[:, h, q0:q0 + nq], o0[:, 0:nq], sgs, rs[:, 0:nq], ALU.mult, ALU.mult)

    p.barrier()
    p.release(mk_att)
    wo = p.alloc(8 * 1024, BF16, name="wo_b")
    wo3 = wo.rearrange("p (h c) -> p h c", h=8)
    stg = Ring(p, 2, 8 * 512, name="wstage2")
    cast_weight(p, wo3, w_out[o].rearrange("(h p) c -> p h c", p=128), 1024, stg)
    G1 = {}
    for v in ((0, 1) if need_ctx else (0,)):
        G1[v] = p.alloc(1024, name="G1")
        load_row_bc(p, "act", G1[v], m_scr[layer, v:v + 1, 2 * 1024:3 * 1024])
    xr = Ring(p, 2, 1024, name="xres")
    yr = Ring(p, 2, 1024, name="yres")
    for ti in range(0 if need_ctx else 2, NT_ALL):
        v = 1 if ti < 2 else 0
        xt = xr.next()
        p.dma("sp", xt, xs[ti * 128:(ti + 1) * 128, :])
        yt = yr.next()
        for cb in range(2):
            ps = p.psum[(ti * 2 + cb) % 4]
            for h in range(8):
                p.mm(ps, onT3[:, h, ti * 128:(ti + 1) * 128], wo3[:, h, cb * 512:(cb + 1) * 512], start=(h == 0), stop=(h == 7))
            p.tt("dve", yt[:, cb * 512:(cb + 1) * 512], ps, G1[v][:, cb * 512:(cb + 1) * 512], ALU.mult)
        p.tt("pool", yt, yt, xt, ALU.add)
        p.dma("act", xs[ti * 128:(ti + 1) * 128, :], yt)
    p.barrier()
    p.release(mk)
    p.release(mk0)


def rope_table():
    rows = LAT // 64
    r, c = np.meshgrid(np.arange(rows), np.arange(64), indexing="ij")
    pos = np.stack([r.reshape(-1), c.reshape(-1)], axis=-1).astype(np.float32)
    inv = (10000.0 ** (-np.arange(16, dtype=np.float32) / 16)).astype(np.float32)
    ang = pos[:, :, None] * inv
    return np.concatenate([np.cos(ang).reshape(LAT, 32), np.sin(ang).reshape(LAT, 32)], axis=1).astype(np.float32)


NTOK = LAT + CTX
NCH = NTOK // 64
SEQS = ((0, CTX), (CTX, LAT))


def even_scratch(p, layer):
    s = {}
    s["qT"] = p.dram(f"e{layer}_qT", [4, 128, NTOK], F32)
    s["kT"] = p.dram(f"e{layer}_kT", [4, 128, NTOK], F32)
    s["k_tok"] = p.dram(f"e{layer}_ktok", [NTOK, 512], F32)
    s["v_tok"] = p.dram(f"e{layer}_vtok", [NTOK, 512], F32)
    s["zs"] = p.dram(f"e{layer}_zs", [NTOK, 512], F32)
    s["ba"] = p.dram(f"e{layer}_ba", [64, NCH * 16], F32)
    s["hyx"] = p.dram(f"e{layer}_hyx", [8, 128, NTOK], F32)
    s["hyv"] = p.dram(f"e{layer}_hyv", [NTOK, 512], BF16)
    s["o_f"] = p.dram(f"e{layer}_of", [NTOK, 512], F32)
    s["o_b"] = p.dram(f"e{layer}_ob", [NTOK, 512], F32)
    s["actT"] = p.dram(f"e{layer}_actT", [8, 128, NTOK], BF16)
    return s


def phase_even_proj(cx, layer, xs, m_scr, S):
    p = cx.p
    e = layer // 2
    mk0 = p.mark()
    ident_d = cx.inp("ident", [128, 128])
    w_in = cx.inp("w_in", [2, 1024, 3600])
    convT_d = cx.inp("conv_wT", [2, 128, 72])
    ident = p.alloc(128, name="ident")
    p.dma("act", ident, ident_d)
    hT = p.alloc(8 * NTOK, BF16, name="hT")
    make_hT(cx, layer, xs, m_scr, ident, hT)
    hT3 = hT.rearrange("p (k t) -> p k t", k=8)
    wb = p.alloc(8 * 3600, BF16, name="win_b")
    wb3 = wb.rearrange("p (k c) -> p k c", k=8)
    mks = p.mark()
    stg = Ring(p, 2, 8 * 512, name="wstage")
    cast_weight(p, wb3, w_in[e].rearrange("(k p) c -> p k c", p=128), 3600, stg)
    p.barrier()
    p.release(mks)
    cw = p.alloc(72, name="convw")
    p.dma("act", cw, convT_d[e])
    ones_f = p.alloc(128, name="ones_f")
    p.memset("dve", ones_f, 1.0)
    eps_t = p.alloc(1, name="eps")
    p.memset("dve", eps_t, EPS)

    PB = NTOK + 4
    pb_r = Ring(p, 1, PB, name="pbuf")
    cv_r = Ring(p, 2, NTOK, name="cv")
    sq_r = Ring(p, 2, 512, name="sq")
    rn_r = Ring(p, 2, 512, name="rn")
    tk_r = Ring(p, 2, 512, name="tok_stage")
    tkb_r = Ring(p, 2, 512, BF16, name="tokb_stage")
    blocks = [(0, CTX, 1)] + [(CTX + 512 * b, 512, CTX + 512 * b + 3) for b in range(LAT // 512)]
    for pb in pb_r.bufs:
        p.memset("pool", pb, 0.0)
    ib = 0
    for cg in range(24):
        pbuf = pb_r.next()
        for (t0, n, c0) in blocks:
            ps = p.psum[ib % 4]
            ib += 1
            for k in range(8):
                p.mm(ps[:, 0:n], wb3[:, k, cg * 128:(cg + 1) * 128], hT3[:, k, t0:t0 + n], start=(k == 0), stop=(k == 7))
            p.copy("act" if ib % 2 == 0 else "dve", pbuf[:, c0:c0 + n], ps[:, 0:n])
        cv = cv_r.next()
        for (t0, n) in SEQS:
            c0 = t0 + 1 if t0 == 0 else t0 + 3
            p.act(cv[:, t0:t0 + n], pbuf[:, c0:c0 + n], AF.Copy, scale=cw[:, cg * 3 + 1:cg * 3 + 2])
            p.stt("dve", cv[:, t0:t0 + n], pbuf[:, c0 - 1:c0 - 1 + n], cw[:, cg * 3:cg * 3 + 1], cv[:, t0:t0 + n], ALU.mult, ALU.add)
            p.stt("dve", cv[:, t0:t0 + n], pbuf[:, c0 + 1:c0 + 1 + n], cw[:, cg * 3 + 2:cg * 3 + 3], cv[:, t0:t0 + n], ALU.mult, ALU.add)
        if cg < 12:
            p.act(cv, cv, AF.Silu)
        if cg < 8:
            for (t0, n, _) in blocks:
                sq = sq_r.next()
                p.tt("pool", sq[:, 0:n], cv[:, t0:t0 + n], cv[:, t0:t0 + n], ALU.mult)
                ps = p.psum[4 + (ib % 2)]
                ib += 1
                p.mm(ps[:, 0:n], ones_f, sq[:, 0:n])
                rn = rn_r.next()
                p.act(rn[:, 0:n], ps[:, 0:n], AF.Sqrt, bias=eps_t)
                p.recip(rn[:, 0:n], rn[:, 0:n])
                if cg < 4:
                    p.stt("dve", cv[:, t0:t0 + n], cv[:, t0:t0 + n], 128.0 ** -0.5, rn[:, 0:n], ALU.mult, ALU.mult)
                else:
                    p.tt("dve", cv[:, t0:t0 + n], cv[:, t0:t0 + n], rn[:, 0:n], ALU.mult)
        if cg < 4:
            p.dma("sp", S["qT"][cg], cv)
        elif cg < 8:
            p.dma("sp", S["kT"][cg - 4], cv)
        elif 12 <= cg < 20:
            p.dma("sp", S["hyx"][cg - 12], cv)
        if 4 <= cg < 12 or cg >= 20:
            if cg < 8:
                dst, col, bf = S["k_tok"], (cg - 4) * 128, False
            elif cg < 12:
                dst, col, bf = S["v_tok"], (cg - 8) * 128, False
            else:
                dst, col, bf = S["hyv"], (cg - 20) * 128, True
            for t4 in range(0, NT_ALL, 4):
                nt = min(4, NT_ALL - t4)
                ps = p.psum[6 + (ib % 2)]
                ib += 1
                for c in range(nt):
                    p.tr(ps[:, c * 128:(c + 1) * 128], cv[:, (t4 + c) * 128:(t4 + c + 1) * 128], ident)
                st = (tkb_r if bf else tk_r).next()
                p.copy("act", st[:, 0:nt * 128], ps[:, 0:nt * 128])
                p.dma("act" if bf else "sp", dst[t4 * 128:(t4 + nt) * 128, col:col + 128].rearrange("(c p) d -> p c d", p=128),
                      st[:, 0:nt * 128].rearrange("p (c d) -> p c d", d=128))
    z_r = Ring(p, 2, 512, name="zt")
    for ti in range(NT_ALL):
        ps = p.psum[ti % 4]
        for k in range(8):
            p.mm(ps, hT3[:, k, ti * 128:(ti + 1) * 128], wb3[:, k, 3072:3584], start=(k == 0), stop=(k == 7))
        zt = z_r.next()
        p.act(zt, ps, AF.Silu)
        p.dma("sp", S["zs"][ti * 128:(ti + 1) * 128, :], zt)
    bas = p.alloc(NCH * 16, name="ba_s", parts=64)
    for c0 in range(0, NCH, 32):
        nc_ = min(32, NCH - c0)
        ps = p.psum[4 + (c0 // 32) % 2]
        for c in range(nc_):
            ch = c0 + c
            for k in range(8):
                p.mm(ps[0:64, c * 16:(c + 1) * 16], hT3[:, k, ch * 64:(ch + 1) * 64], wb3[:, k, 3584:3600], start=(k == 0), stop=(k == 7))
        p.copy("dve", bas[:, c0 * 16:(c0 + nc_) * 16], ps[0:64, 0:nc_ * 16])
    p.dma("sp", S["ba"], bas)
    p.barrier()
    p.release(mk0)


class BankRR:
    def __init__(self, p, banks):
        self.p = p
        self.banks = list(banks)
        self.i = 0

    def next(self):
        b = self.p.psum[self.banks[self.i % len(self.banks)]]
        self.i += 1
        return b


class GdnDir:
    def __init__(self, p, d):
        self.d = d
        a = lambda cols, nm, parts=64: p.alloc(cols, name=f"g{d}_{nm}", parts=parts)
        self.ktok = a(8 * 128, "ktok")
        self.vtok = a(8 * 128, "vtok")
        self.gm1 = a(512, "gm1")
        self.gm2 = a(512, "gm2")
        self.dec = a(512, "dec")
        self.decT = a(512, "decT")
        self.A = a(512, "A")
        self.P = [a(512, "Pa"), a(512, "Pb")]
        self.Q = [a(512, "Qa"), a(512, "Qb")]
        self.RT = [a(512, "RTa"), a(512, "RTb")]
        self.IP = a(512, "IP")
        self.qkdT = a(512, "qkdT")
        self.bv = a(8 * 128, "bv")
        self.bk = a(8 * 128, "bk")
        self.kd = a(8 * 128, "kd")
        self.wi = a(8 * 128, "wi")
        self.kcT = a(512, "kcT", parts=128)
        self.sc = a(64, "scal")
        self.cdb = a(8, "cdb", parts=128)
        self.S = a(128, "S", parts=128)
        self.ws = [a(128, "w_s0"), a(128, "w_s1")]
        self.tmp = [a(128, "tmp0"), a(128, "tmp1")]
        self.ob = a(8 * 128, "ob")


def phase_gdn(cx, layer, S):
    p = cx.p
    e = layer // 2
    mk0 = p.mark()
    masks_d = cx.inp("gdn_masks", [64, 256])
    ident_d = cx.inp("ident", [128, 128])
    alog_d = cx.inp("a_log", [2, 2, 4])
    dtb_d = cx.inp("dt_bias", [2, 2, 4])
    ident = p.alloc(128, name="ident")
    p.dma("act", ident, ident_d)
    id64 = ident[0:64, 0:64]
    masks = p.alloc(256, name="masks", parts=64)
    p.dma("act", masks, masks_d)
    Um, Lom, SUm, SLm = (masks[:, i * 64:(i + 1) * 64] for i in range(4))
    ones_f = p.alloc(128, name="ones_f")
    p.memset("dve", ones_f, 1.0)
    one64 = ones_f[0:64, 0:1]
    bas = p.alloc(NCH * 16, name="bas", parts=64)
    p.dma("sp", bas, S["ba"])
    bas4 = bas.rearrange("p (c t x) -> p c t x", c=NCH, t=2)
    beta = p.alloc(NCH * 8, name="beta", parts=64)
    gt = p.alloc(NCH * 8, name="gt", parts=64)
    beta3 = beta.rearrange("p (c x) -> p c x", c=NCH)
    gt3 = gt.rearrange("p (c x) -> p c x", c=NCH)
    p.act(beta3, bas4[:, :, 0, :], AF.Exp, scale=-1.0)
    p.ts("dve", beta, beta, 1.0, None, ALU.add)
    p.recip(beta, beta)
    rows = p.alloc(16, name="rows", parts=64)
    load_row_bc(p, "act", rows[:, 0:8], alog_d[e:e + 1].rearrange("a d h -> a (d h)"))
    load_row_bc(p, "act", rows[:, 8:16], dtb_d[e:e + 1].rearrange("a d h -> a (d h)"))
    p.act(rows[:, 0:8], rows[:, 0:8], AF.Exp)
    p.ts("dve", rows[:, 0:8], rows[:, 0:8], -1.0, None, ALU.mult)
    p.tt("dve", gt3, bas4[:, :, 1, :], rows[:, 8:16].unsq(1).bc([64, NCH, 8]), ALU.add)
    p.act(gt, gt, AF.Exp)
    p.act(gt, gt, AF.Ln, bias=one64)
    p.tt("dve", gt3, gt3, rows[:, 0:8].unsq(1).bc([64, NCH, 8]), ALU.mult)

    qk_r = Ring(p, 2, NTOK, name="qT_h")
    kk_r = Ring(p, 2, NTOK, name="kT_h")
    dirs = [GdnDir(p, 0), GdnDir(p, 1)]
    brr = BankRR(p, range(8))
    lat_groups = [list(range(4 + 8 * g, 12 + 8 * g)) for g in range(8)]
    groups = {0: [[0, 1, 2, 3]] + lat_groups, 1: [[0, 1, 2, 3]] + lat_groups[::-1]}
    MS = {0: (Um, SLm, SLm, SLm, Um, Um), 1: (Lom, SUm, SUm, SUm, Lom, Lom)}

    def prep(dc, h, chunks, qT, kT):
        d = dc.d
        L, R, MSk, L2, R2, MB = MS[d]
        n = len(chunks)
        c0 = chunks[0]
        W = n * 64
        x = d * 4 + h
        gcols = gt3[:, c0:c0 + n, x]
        bcols = beta3[:, c0:c0 + n, x]
        v3 = lambda t, w=64: t[:, 0:n * w].rearrange("p (c j) -> p c j", c=n)
        p.dma("sp", v3(dc.ktok, 128), S["k_tok"][c0 * 64:(c0 + n) * 64, h * 128:(h + 1) * 128].rearrange("(c p) d -> p c d", p=64))
        p.dma("act", v3(dc.vtok, 128), S["v_tok"][c0 * 64:(c0 + n) * 64, h * 128:(h + 1) * 128].rearrange("(c p) d -> p c d", p=64))
        ps_kk = brr.next()
        ps_qk = brr.next()
        for c, ch in enumerate(chunks):
            p.mm(ps_kk[0:64, c * 64:(c + 1) * 64], kT[:, ch * 64:(ch + 1) * 64], kT[:, ch * 64:(ch + 1) * 64])
            p.mm(ps_qk[0:64, c * 64:(c + 1) * 64], kT[:, ch * 64:(ch + 1) * 64], qT[:, ch * 64:(ch + 1) * 64])
        p.tt("dve", v3(dc.gm1), R.unsq(1).bc([64, n, 64]), gcols.unsq(2).bc([64, n, 64]), ALU.mult)
        p.tt("pool", v3(dc.gm2), R2.unsq(1).bc([64, n, 64]), gcols.unsq(2).bc([64, n, 64]), ALU.mult)
        ps3 = brr.next()
        ps4 = brr.next()
        p.mm(ps3[0:64, 0:W], L, dc.gm1[:, 0:W])
        p.mm(ps4[0:64, 0:W], L2, dc.gm2[:, 0:W])
        p.act(dc.dec[:, 0:W], ps3[0:64, 0:W], AF.Exp)
        p.act(dc.decT[:, 0:W], ps4[0:64, 0:W], AF.Exp)
        p.tt("dve", v3(dc.dec), v3(dc.dec), MSk.unsq(1).bc([64, n, 64]), ALU.mult)
        p.tt("pool", v3(dc.decT), v3(dc.decT), MB.unsq(1).bc([64, n, 64]), ALU.mult)
        p.tt("dve", dc.A[:, 0:W], ps_kk[0:64, 0:W], dc.dec[:, 0:W], ALU.mult)
        p.tt("dve", v3(dc.A), v3(dc.A), bcols.unsq(2).bc([64, n, 64]), ALU.mult)
        p.tt("dve", dc.qkdT[:, 0:W], ps_qk[0:64, 0:W], dc.decT[:, 0:W], ALU.mult)
        ps5 = brr.next()
        for c in range(n):
            p.tr(ps5[0:64, c * 64:(c + 1) * 64], dc.A[:, c * 64:(c + 1) * 64], id64)
        Pc, Qc, RTc = dc.A, dc.Q[0], dc.RT[0]
        p.copy("act", Qc[:, 0:W], ps5[0:64, 0:W])
        p.tt("pool", v3(RTc), id64.unsq(1).bc([64, n, 64]), v3(Qc), ALU.subtract)
        for r in range(5):
            Pn, Qn, RTn = dc.P[r % 2], dc.Q[(r + 1) % 2], dc.RT[(r + 1) % 2]
            psP = brr.next()
            for c in range(n):
                p.mm(psP[0:64, c * 64:(c + 1) * 64], Qc[:, c * 64:(c + 1) * 64], Pc[:, c * 64:(c + 1) * 64])
            if r < 4:
                psQ = brr.next()
                for c in range(n):
                    p.mm(psQ[0:64, c * 64:(c + 1) * 64], Pc[:, c * 64:(c + 1) * 64], Qc[:, c * 64:(c + 1) * 64])
            p.copy("act", Pn[:, 0:W], psP[0:64, 0:W])
            if r < 4:
                p.copy("dve", Qn[:, 0:W], psQ[0:64, 0:W])
            p.tt("pool", v3(dc.IP), v3(Pn), id64.unsq(1).bc([64, n, 64]), ALU.add)
            psR = brr.next()
            for c in range(n):
                p.mm(psR[0:64, c * 64:(c + 1) * 64], dc.IP[:, c * 64:(c + 1) * 64], RTc[:, c * 64:(c + 1) * 64])
            p.copy("dve", RTn[:, 0:W], psR[0:64, 0:W])
            Pc, Qc, RTc = Pn, Qn, RTn
        psG = brr.next()
        p.mm(psG[0:64, 0:n], L, gcols)
        p.mm(psG[0:128, 8:8 + n], ones_f[0:64, :], gcols)
        Gs, eG, kds, bke = (dc.sc[:, i * 8:i * 8 + n] for i in range(4))
        p.copy("dve", Gs, psG[0:64, 0:n])
        p.act(eG, psG[0:64, 0:n], AF.Exp)
        p.act(dc.cdb[:, 0:n], psG[0:128, 8:8 + n], AF.Exp)
        p.tt("dve", kds, psG[0:64, 8:8 + n], Gs, ALU.subtract)
        p.act(kds, kds, AF.Exp)
        p.tt("dve", bke, bcols, eG, ALU.mult)
        p.tt("pool", v3(dc.bv, 128), v3(dc.vtok, 128), bcols.unsq(2).bc([64, n, 128]), ALU.mult)
        p.tt("dve", v3(dc.bk, 128), v3(dc.ktok, 128), bke.unsq(2).bc([64, n, 128]), ALU.mult)
        p.tt("pool", v3(dc.kd, 128), v3(dc.ktok, 128), kds.unsq(2).bc([64, n, 128]), ALU.mult)
        for half in range(0, n, 4):
            psW = brr.next()
            m = min(4, n - half)
            for c in range(half, half + m):
                p.mm(psW[0:64, (c - half) * 128:(c - half + 1) * 128], RTc[:, c * 64:(c + 1) * 64], dc.bv[:, c * 128:(c + 1) * 128])
            p.copy("act", dc.wi[:, half * 128:(half + m) * 128], psW[0:64, 0:m * 128])
        psK = brr.next()
        for c in range(n):
            p.mm(psK[0:128, c * 64:(c + 1) * 64], dc.bk[:, c * 128:(c + 1) * 128], RTc[:, c * 64:(c + 1) * 64])
        p.copy("dve", dc.kcT[:, 0:W], psK[0:128, 0:W])

    def scan_step(dc, ci, ch, qT, it):
        psA = brr.next()
        p.mm(psA[0:64, 0:128], dc.kcT[:, ci * 64:(ci + 1) * 64], dc.S)
        ws = dc.ws[it % 2]
        p.tt("dve", ws, dc.wi[:, ci * 128:(ci + 1) * 128], psA[0:64, 0:128], ALU.subtract)
        psO = brr.next()
        p.mm(psO[0:64, 0:128], qT[:, ch * 64:(ch + 1) * 64], dc.S)
        p.mm(psO[0:64, 128:256], dc.qkdT[:, ci * 64:(ci + 1) * 64], ws)
        tmp = dc.tmp[it % 2]
        p.act(tmp, psO[0:64, 0:128], AF.Copy, scale=dc.sc[:, 8 + ci:8 + ci + 1])
        p.tt("dve", dc.ob[:, ci * 128:(ci + 1) * 128], tmp, psO[0:64, 128:256], ALU.add)
        psS = brr.next()
        p.mm(psS[0:128, 0:128], dc.kd[:, ci * 128:(ci + 1) * 128], ws)
        p.stt("dve", dc.S, dc.S, dc.cdb[:, ci:ci + 1], psS[0:128, 0:128], ALU.mult, ALU.add)

    it = 0
    for h in range(4):
        qT = qk_r.next()
        kT = kk_r.next()
        p.dma("sp", qT, S["qT"][h])
        p.dma("act", kT, S["kT"][h])
        for dc in dirs:
            p.memset("pool", dc.S, 0.0)
        for gi in range(9):
            for dc in dirs:
                prep(dc, h, groups[dc.d][gi], qT, kT)
            n = len(groups[0][gi])
            for s in range(n):
                for dc in dirs:
                    chunks = groups[dc.d][gi]
                    ci = s if dc.d == 0 else n - 1 - s
                    scan_step(dc, ci, chunks[ci], qT, it)
                it += 1
            for dc in dirs:
                chunks = groups[dc.d][gi]
                c0 = chunks[0]
                dst = S["o_f"] if dc.d == 0 else S["o_b"]
                p.dma("sp" if dc.d == 0 else "act",
                      dst[c0 * 64:(c0 + n) * 64, h * 128:(h + 1) * 128].rearrange("(c p) d -> p c d", p=64),
                      dc.ob[:, 0:n * 128].rearrange("p (c d) -> p c d", c=n))
    p.barrier()
    p.release(mk0)


def phase_gdn_out(cx, layer, S):
    p = cx.p
    e = layer // 2
    mk0 = p.mark()
    ident_d = cx.inp("ident", [128, 128])
    gain_d = cx.inp("gdn_gain", [2, 128])
    ident = p.alloc(128, name="ident")
    p.dma("act", ident, ident_d)
    gain = p.alloc(128, name="gain")
    load_row_bc(p, "act", gain, gain_d[e:e + 1, :])
    eps_t = p.alloc(1, name="eps")
    p.memset("dve", eps_t, EPS)
    of_r = Ring(p, 2, 512, name="of")
    ob_r = Ring(p, 2, 512, name="ob")
    z_r = Ring(p, 2, 512, name="zz")
    sq_r = Ring(p, 2, 512, name="sq")
    st_r = Ring(p, 2, 8, name="st")
    tb_r = Ring(p, 2, 512, BF16, name="tb")
    for ti in range(NT_ALL):
        of, ob, zz = of_r.next(), ob_r.next(), z_r.next()
        rows = slice(ti * 128, (ti + 1) * 128)
        p.dma("sp", of, S["o_f"][rows, :])
        p.dma("act", ob, S["o_b"][rows, :])
        p.dma("sp", zz, S["zs"][rows, :])
        p.tt("dve", of, of, ob, ALU.add)
        sq = sq_r.next()
        p.tt("pool", sq, of, of, ALU.mult)
        st = st_r.next()
        p.reduce("dve", st[:, 0:4], sq.rearrange("p (h d) -> p h d", h=4), ALU.add, AX.X)
        p.act(st[:, 4:8], st[:, 0:4], AF.Sqrt, scale=1.0 / 128, bias=eps_t)
        p.recip(st[:, 4:8], st[:, 4:8])
        o3 = of.rearrange("p (h d) -> p h d", h=4)
        p.tt("dve", o3, o3, st[:, 4:8].unsq(2).bc([128, 4, 128]), ALU.mult)
        p.tt("pool", o3, o3, gain.unsq(1).bc([128, 4, 128]), ALU.mult)
        p.tt("dve", of, of, zz, ALU.mult)
        ps = p.psum[ti % 4]
        for c in range(4):
            p.tr(ps[:, c * 128:(c + 1) * 128], of[:, c * 128:(c + 1) * 128], ident)
        tb = tb_r.next()
        p.copy("act", tb, ps)
        p.dma("act", S["actT"][0:4, :, ti * 128:(ti + 1) * 128].rearrange("g p t -> p g t"), tb.rearrange("p (g t) -> p g t", g=4))
    p.barrier()
    p.release(mk0)


import math as _math
import ml_dtypes as _mld

_TABLE_CACHE = {}


def dft_tables(L):
    if L in _TABLE_CACHE:
        return _TABLE_CACHE[L]
    N = 2 * L
    TB = min(512, L)
    n_t = L // 128
    t = np.arange(L, dtype=np.float64)[:, None]
    f = np.arange(L, dtype=np.float64)[None, :]
    ang = (2.0 * np.pi / N) * ((f + 0.5) * t)
    out = {}
    for nm, M in (("C", np.cos(ang)), ("S", np.sin(ang))):
        M = M.astype(np.float32)
        fw = M.reshape(n_t, 128, n_t, 128).transpose(2, 1, 0, 3).reshape(n_t, 128, n_t * 128)
        inv = M.reshape(L // TB, TB, n_t, 128).transpose(0, 3, 2, 1).reshape(L // TB, 128, n_t * TB)
        out[nm + "fw"] = np.ascontiguousarray(fw).astype(_mld.bfloat16)
        out[nm + "inv"] = np.ascontiguousarray(inv).astype(_mld.bfloat16)
    _TABLE_CACHE[L] = out
    return out


def hyena_consts(L):
    f32 = np.float32
    t = np.linspace(0.0, 1.0, L, dtype=f32)
    bands = 16
    wpos = (2.0 * _math.pi * np.arange(L, dtype=f32) / L).astype(f32)
    fb = np.linspace(1e-4, bands - 1, bands, dtype=f32)
    z = np.concatenate([t[:, None], np.cos(wpos[:, None] * fb), -np.sin(wpos[:, None] * fb)], axis=-1).astype(f32)
    mn = _math.log(1e-2) / 1.5
    mx = _math.log(1e-2) / 0.3
    deltas = np.linspace(mn, mx, 512, dtype=f32)
    win = np.exp(-t[:, None] * np.abs(deltas)).astype(f32)
    return np.ascontiguousarray(z.T), win


def phase_hyena(cx, layer, S):
    p = cx.p
    e = layer // 2
    mk0 = p.mark()
    ident_d = cx.inp("ident", [128, 128])
    ident = p.alloc(128, name="ident")
    p.dma("act", ident, ident_d)
    w1_d = cx.inp("hf_w1", [2, 33, 64])
    w2_d = cx.inp("hf_w2", [2, 64, 64])
    w3_d = cx.inp("hf_w3", [2, 64, 64])
    w4_d = cx.inp("hf_w4", [2, 64, 2048])
    vec_d = {n: cx.inp(n, [2, 64]) for n in ("hf_b1", "hf_b2", "hf_b3", "hf_freq")}
    hyb_d = cx.inp("hy_bias", [2, 2, 512])
    PI = _math.pi

    w1 = p.alloc(64, name="w1", parts=33)
    w2 = p.alloc(64, name="w2", parts=64)
    w3 = p.alloc(64, name="w3", parts=64)
    w4 = p.alloc(2048, name="w4", parts=64)
    p.dma("sp", w1, w1_d[e])
    p.dma("sp", w2, w2_d[e])
    p.dma("sp", w3, w3_d[e])
    p.dma("sp", w4, w4_d[e])
    vec = p.alloc(8, name="hvec", parts=64)
    for i, n in enumerate(("hf_b1", "hf_b2", "hf_b3", "hf_freq")):
        p.dma("act", vec[:, i:i + 1], vec_d[n][e].rearrange("(p o) -> p o", o=1))
    for i in range(3):
        p.tt("dve", vec[:, 4 + i:5 + i], vec[:, i:i + 1], vec[:, 3:4], ALU.mult)
    freq = vec[:, 3:4]
    brow = p.alloc(1024, name="hybias", parts=1)
    p.dma("act", brow, hyb_d[e:e + 1].rearrange("a o c -> a (o c)"))

    for (t0, L) in SEQS:
        N = 2 * L
        TB = min(512, L)
        n_t = L // 128
        n_tb = L // TB
        tabs = {k: cx.inp(f"dft{L}_{k}", list(shp), BF16) for k, shp in
                (("Cfw", (n_t, 128, n_t * 128)), ("Sfw", (n_t, 128, n_t * 128)),
                 ("Cinv", (n_tb, 128, n_t * TB)), ("Sinv", (n_tb, 128, n_t * TB)))}
        zT_d = cx.inp(f"hy_zT{L}", [33, L])
        win_d = cx.inp(f"hy_win{L}", [L, 512])
        fa_d = p.dram(f"e{layer}_fa{L}", [4, L, 512], BF16)
        spec_d = p.dram(f"e{layer}_spec{L}", [4, L, 512], F32)
        S[f"fa{L}"] = fa_d
        S[f"spec{L}"] = spec_d
        mk = p.mark()
        h3 = p.alloc(L, name="h3", parts=64)
        zr = Ring(p, 2, TB, name="zTt", parts=33)
        hr = Ring(p, 4, TB, name="hh", parts=64)
        tr_ = Ring(p, 2, TB, name="wrapt", parts=64)

        def sin_layer(dst, ps, fbcol, n):
            y = hr.next()
            p.ts("dve", y[:, 0:n], ps[0:64, 0:n], freq, fbcol, ALU.mult, ALU.add)
            for _ in range(2):
                t = tr_.next()
                p.ts("dve", t[:, 0:n], y[:, 0:n], PI, -2.0 * PI, ALU.is_gt, ALU.mult)
                p.tt("dve", y[:, 0:n], y[:, 0:n], t[:, 0:n], ALU.add)
                t = tr_.next()
                p.ts("dve", t[:, 0:n], y[:, 0:n], -PI, 2.0 * PI, ALU.is_lt, ALU.mult)
                p.tt("dve", y[:, 0:n], y[:, 0:n], t[:, 0:n], ALU.add)
            p.act(dst, y[:, 0:n], AF.Sin)

        for b in range(n_tb):
            zt = zr.next()
            p.dma("sp", zt, zT_d[:, b * TB:(b + 1) * TB])
            ps = p.psum[b % 2]
            p.mm(ps[0:64, 0:TB], w1, zt)
            h1 = hr.next()
            sin_layer(h1, ps, vec[:, 4:5], TB)
            ps = p.psum[2 + b % 2]
            p.mm(ps[0:64, 0:TB], w2, h1)
            h2 = hr.next()
            sin_layer(h2, ps, vec[:, 5:6], TB)
            ps = p.psum[4 + b % 2]
            p.mm(ps[0:64, 0:TB], w3, h2)
            sin_layer(h3[:, b * TB:(b + 1) * TB], ps, vec[:, 6:7], TB)
        win_r = Ring(p, 2, 512, name="win")
        hf_r = Ring(p, 2, 512, name="hf")
        hb_r = Ring(p, 2, 512, name="hb")
        ad_r = Ring(p, 4, 512, BF16, name="ad")
        for tt_ in range(n_t):
            wt = win_r.next()
            p.dma("sp", wt, win_d[tt_ * 128:(tt_ + 1) * 128, :])
            for o in range(2):
                psf = p.psum[(2 * o) % 4]
                psb = p.psum[(2 * o + 1) % 4]
                p.mm(psf, h3[:, tt_ * 128:(tt_ + 1) * 128], w4[:, (2 * o) * 512:(2 * o + 1) * 512])
                p.mm(psb, h3[:, tt_ * 128:(tt_ + 1) * 128], w4[:, (2 * o + 1) * 512:(2 * o + 2) * 512])
                hf, hb = hf_r.next(), hb_r.next()
                p.tt("dve", hf, psf, wt, ALU.mult)
                p.tt("dve", hb, psb, wt, ALU.mult)
                if tt_ == 0:
                    p.tt("dve", hf[0:1, :], hf[0:1, :], brow[0:1, o * 512:(o + 1) * 512], ALU.add)
                    p.memset("dve", hb[0:1, :], 0.0)
                a, d = ad_r.next(), ad_r.next()
                p.tt("pool", a, hf, hb, ALU.add)
                p.tt("pool", d, hb, hf, ALU.subtract)
                p.dma("sp", fa_d[2 * o, tt_ * 128:(tt_ + 1) * 128, :], a)
                p.dma("act", fa_d[2 * o + 1, tt_ * 128:(tt_ + 1) * 128, :], d)
        p.barrier()
        p.release(mk)

        U = p.alloc(n_t * 512, BF16, name="U")
        U3 = U.rearrange("p (t c) -> p t c", c=512)
        Yr = p.alloc(n_t * 512, BF16, name="Yr")
        Yi = p.alloc(n_t * 512, BF16, name="Yi")
        Yr3 = Yr.rearrange("p (t c) -> p t c", c=512)
        Yi3 = Yi.rearrange("p (t c) -> p t c", c=512)
        FQ = min(8, n_t)
        tab_r = Ring(p, 4, max(n_t * 128, FQ * TB), BF16, name="tab")
        sp_r = Ring(p, 4, 512, name="spec")
        x_r = Ring(p, 4, 512, name="xcs")
        t_r = Ring(p, 4, 512, name="ytmp")
        xt_r = Ring(p, 2, TB, name="xT")
        zo_r = Ring(p, 2, TB, name="zo")
        zb_r = Ring(p, 2, TB, BF16, name="zob")

        def load_U(src_rows):
            p.dma("sp", U3, src_rows.rearrange("(t p) c -> p t c", p=128))

        def fwd_pass(which, consumer):
            for ft in range(n_t):
                pss = {}
                for i, w in enumerate(which):
                    tb_ = tab_r.next()
                    p.dma("sp" if i == 0 else "act", tb_[:, 0:n_t * 128], tabs[w + "fw"][ft])
                    ps = p.psum[(2 * ft + i) % 4]
                    for tc in range(n_t):
                        p.mm(ps, tb_[:, tc * 128:(tc + 1) * 128], U3[:, tc, :], start=(tc == 0), stop=(tc == n_t - 1))
                    pss[w] = ps
                consumer(ft, pss)

        for o in range(2):
            for j, w in enumerate(("C", "S")):
                load_U(fa_d[2 * o + j])

                def store_spec(ft, pss, j=j, w=w, o=o):
                    st = sp_r.next()
                    p.act(st, pss[w], AF.Copy, scale=2.0 / N)
                    p.dma("act", spec_d[2 * o + j, ft * 128:(ft + 1) * 128, :], st)
                fwd_pass((w,), store_spec)
        for o in range(2):
            if o == 0:
                load_U(S["hyv"][t0:t0 + L, :])

            def make_Y(ft, pss, o=o):
                ac, ds = sp_r.next(), sp_r.next()
                p.dma("sp", ac, spec_d[2 * o, ft * 128:(ft + 1) * 128, :])
                p.dma("act", ds, spec_d[2 * o + 1, ft * 128:(ft + 1) * 128, :])
                xc, xs_ = x_r.next(), x_r.next()
                p.copy("act", xc, pss["C"])
                p.copy("act", xs_, pss["S"])
                t1, t2 = t_r.next(), t_r.next()
                p.tt("pool", t1, xc, ac, ALU.mult)
                p.tt("dve", t2, xs_, ds, ALU.mult)
                p.tt("dve", Yr3[:, ft, :], t1, t2, ALU.add)
                t3, t4 = t_r.next(), t_r.next()
                p.tt("pool", t3, xs_, ac, ALU.mult)
                p.tt("dve", t4, xc, ds, ALU.mult)
                p.tt("pool", Yi3[:, ft, :], t3, t4, ALU.subtract)
            fwd_pass(("C", "S"), make_Y)
            for tb in range(n_tb):
                for fq in range(n_t // FQ):
                    ct, st_ = tab_r.next(), tab_r.next()
                    p.dma("sp", ct[:, 0:FQ * TB], tabs["Cinv"][tb][:, fq * FQ * TB:(fq + 1) * FQ * TB])
                    p.dma("act", st_[:, 0:FQ * TB], tabs["Sinv"][tb][:, fq * FQ * TB:(fq + 1) * FQ * TB])
                    for cgp in range(4):
                        ps = p.psum[4 + cgp]
                        for j in range(FQ):
                            fc = fq * FQ + j
                            p.mm(ps[:, 0:TB], Yr3[:, fc, cgp * 128:(cgp + 1) * 128], ct[:, j * TB:(j + 1) * TB],
                                 start=(fc == 0), stop=False)
                            p.mm(ps[:, 0:TB], Yi3[:, fc, cgp * 128:(cgp + 1) * 128], st_[:, j * TB:(j + 1) * TB],
                                 start=False, stop=(fc == n_t - 1))
                for cgp in range(4):
                    ps = p.psum[4 + cgp]
                    xT = xt_r.next()
                    p.dma("sp", xT, S["hyx"][o * 4 + cgp, :, t0 + tb * TB:t0 + (tb + 1) * TB])
                    if o == 0:
                        zo = zo_r.next()
                        p.tt("dve", zo, ps[:, 0:TB], xT, ALU.mult)
                        pst = p.psum[cgp % 2]
                        nb = TB // 128
                        for b in range(nb):
                            p.tr(pst[:, b * 128:(b + 1) * 128], zo[:, b * 128:(b + 1) * 128], ident)
                        p.copy("act", U3[:, tb * nb:(tb + 1) * nb, cgp * 128:(cgp + 1) * 128],
                               pst[:, 0:nb * 128].rearrange("p (b c) -> p b c", b=nb))
                    else:
                        zb = zb_r.next()
                        p.tt("dve", zb, ps[:, 0:TB], xT, ALU.mult)
                        p.dma("act", S["actT"][4 + cgp, :, t0 + tb * TB:t0 + (tb + 1) * TB], zb)
        p.barrier()
        p.release(mk)
    p.release(mk0)


def phase_even_out(cx, layer, xs, m_scr, S):
    p = cx.p
    e = layer // 2
    mk0 = p.mark()
    w_out = cx.inp("w_out_even", [2, 1024, 1024])
    actT = p.alloc(8 * NTOK, BF16, name="actT")
    aT3 = actT.rearrange("p (g t) -> p g t", g=8)
    for g in range(8):
        p.dma("sp" if g % 2 == 0 else "act", aT3[:, g, :], S["actT"][g])
    wo = p.alloc(8 * 1024, BF16, name="wo_b")
    wo3 = wo.rearrange("p (h c) -> p h c", h=8)
    stg = Ring(p, 2, 8 * 512, name="wstage2")
    cast_weight(p, wo3, w_out[e].rearrange("(h p) c -> p h c", p=128), 1024, stg)
    G1 = {}
    for v in (0, 1):
        G1[v] = p.alloc(1024, name="G1")
        load_row_bc(p, "act", G1[v], m_scr[layer, v:v + 1, 2 * 1024:3 * 1024])
    xr = Ring(p, 2, 1024, name="xres")
    yr = Ring(p, 2, 1024, name="yres")
    for ti in range(NT_ALL):
        v = 1 if ti < 2 else 0
        xt = xr.next()
        p.dma("sp", xt, xs[ti * 128:(ti + 1) * 128, :])
        yt = yr.next()
        for cb in range(2):
            ps = p.psum[(ti * 2 + cb) % 4]
            for h in range(8):
                p.mm(ps, aT3[:, h, ti * 128:(ti + 1) * 128], wo3[:, h, cb * 512:(cb + 1) * 512], start=(h == 0), stop=(h == 7))
            p.tt("dve", yt[:, cb * 512:(cb + 1) * 512], ps, G1[v][:, cb * 512:(cb + 1) * 512], ALU.mult)
        p.tt("pool", yt, yt, xt, ALU.add)
        p.dma("act", xs[ti * 128:(ti + 1) * 128, :], yt)
    p.barrier()
    p.release(mk0)


def phase_final(cx, xs, out):
    p = cx.p
    mk0 = p.mark()
    fn_d = cx.inp("final_norm", [1, 1024])
    g = p.alloc(1024, name="fn_row")
    load_row_bc(p, "act", g, fn_d[0:1, :])
    eps_t = p.alloc(1, name="eps")
    p.memset("dve", eps_t, EPS)
    xr = Ring(p, 2, 1024, name="xt")
    yr = Ring(p, 2, 1024, name="yt")
    junk = p.alloc(1024, name="junk")
    st_r = Ring(p, 2, 4, name="st")
    for ti in range(2, NT_ALL):
        xt = xr.next()
        p.dma("sp", xt, xs[ti * 128:(ti + 1) * 128, :])
        st = st_r.next()
        p.act(junk, xt, AF.Square, accum=st[:, 0:1])
        p.act(st[:, 1:2], st[:, 0:1], AF.Sqrt, scale=1.0 / D, bias=eps_t)
        p.recip(st[:, 2:3], st[:, 1:2])
        yt = yr.next()
        p.stt("dve", yt, xt, st[:, 2:3], g, ALU.mult, ALU.mult)
        p.dma("act", out[(ti - 2) * 128:(ti - 1) * 128, :], yt)
    p.barrier()
    p.release(mk0)


def build_full(cx, xs, m_scr, out, layers=(0, 1, 2, 3)):
    p = cx.p
    for layer in layers:
        last = layer == 3
        phase_mod(cx, layer, m_scr)
        if layer % 2 == 0:
            S = even_scratch(p, layer)
            phase_even_proj(cx, layer, xs, m_scr, S)
            phase_gdn(cx, layer, S)
            phase_gdn_out(cx, layer, S)
            phase_hyena(cx, layer, S)
            phase_even_out(cx, layer, xs, m_scr, S)
        else:
            phase_odd(cx, layer, xs, m_scr, not last)
        phase_peer(cx, layer, xs, m_scr, list(range(2 if last else 0, NT_ALL)))
    phase_final(cx, xs, out)


_BUILD_CACHE = {}


def kernel(**inputs):
    inputs = {k: np.asarray(v) for k, v in inputs.items()}
    if "full" not in _BUILD_CACHE:
        _BUILD_CACHE["full"] = build({})
    cx, nc = _BUILD_CACHE["full"]
    n = 8
    in_maps = [host_inputs(cx, inputs, b) for b in range(n)]
    res = run_bass_kernel_spmd(nc, in_maps, core_ids=list(range(n)))
    return np.stack([np.asarray(res.results[b]["out"]) for b in range(n)], axis=0).astype(np.float32)
```

```python
import numpy as np
import concourse.bass as bass
import concourse.mybir as mybir

F32 = mybir.dt.float32
BF16 = mybir.dt.bfloat16
I32 = mybir.dt.int32
U32 = mybir.dt.uint32
AF = mybir.ActivationFunctionType
ALU = mybir.AluOpType
AX = mybir.AxisListType

ARENA_COLS = 52000
N_DMA_SEMS = 8


class V:
    def __init__(self, ap, key):
        self.ap = ap
        self.key = key

    def __getitem__(self, idx):
        return V(self.ap[idx], self.key)

    def bitcast(self, dt):
        return V(self.ap.bitcast(dt), self.key)

    def rearrange(self, pat, **kw):
        return V(self.ap.rearrange(pat, **kw), self.key)

    def bc(self, shape):
        return V(self.ap.to_broadcast(list(shape)), self.key)

    def k(self, sub):
        return V(self.ap, (self.key, sub))

    def unsq(self, axis):
        return V(self.ap.unsqueeze(axis), self.key)

    def pbc(self, n):
        return V(self.ap.partition_broadcast(n), self.key)

    @property
    def shape(self):
        return self.ap.shape


class Op:
    __slots__ = ("eng", "fn", "deps", "signal", "semkey", "semval", "is_dma")

    def __init__(self, eng, fn, is_dma=False):
        self.eng = eng
        self.fn = fn
        self.deps = []
        self.signal = False
        self.semkey = None
        self.semval = None
        self.is_dma = is_dma


def _ap(x):
    return x.ap if isinstance(x, V) else x


class Prog:
    ENGS = ("pe", "dve", "act", "pool", "sp")

    def __init__(self):
        self.nc = bass.Bass("TRN2", target_bir_lowering=False)
        nc = self.nc
        self.ops = {e: [] for e in self.ENGS}
        self.lastw = {}
        self.readers = {}
        self.arena = nc.alloc_sbuf_tensor("arena", [128, ARENA_COLS], F32)
        self.top = 0
        self.psum = []
        for i in range(8):
            t = nc.alloc_psum_tensor(f"psb{i}", [128, 512], F32)
            self.psum.append(V(t.ap(), f"psb{i}"))
        self.dma_last = {}
        self.dma_rr = {q: 0 for q in ("sp", "act", "pool")}
        self.dma_cnt = {}
        self.uid = 0
        self.drams = {}

    def dram(self, name, shape, dt, kind="Internal"):
        t = self.nc.dram_tensor(name, list(shape), dt, kind=kind)
        v = V(t.ap(), name)
        self.drams[name] = v
        return v

    def alloc(self, cols, dt=F32, name=None, parts=128):
        self.uid += 1
        nbytes = cols * mybir.dt.size(dt)
        c32 = (nbytes + 3) // 4
        c32 = (c32 + 7) // 8 * 8
        a = self.top
        self.top += c32
        assert self.top <= ARENA_COLS, f"SBUF arena overflow: {self.top} > {ARENA_COLS}"
        ap = self.arena[0:parts, a:a + c32]
        if dt != F32:
            ap = ap.bitcast(dt)
        ap = ap[:, 0:cols]
        return V(ap, f"{name or 't'}#{self.uid}")

    def mark(self):
        return self.top

    def release(self, m):
        self.top = m

    def _track(self, op, r, w):
        deps = op.deps
        for v in r:
            k = v.key if isinstance(v, V) else v
            lw = self.lastw.get(k)
            if lw is not None:
                deps.append(lw)
            self.readers.setdefault(k, {})
        for v in w:
            k = v.key if isinstance(v, V) else v
            lw = self.lastw.get(k)
            if lw is not None:
                deps.append(lw)
            for rd in self.readers.get(k, {}).values():
                deps.append(rd)
        for v in r:
            k = v.key if isinstance(v, V) else v
            self.readers[k][self._semslot(op)] = op
        for v in w:
            k = v.key if isinstance(v, V) else v
            self.lastw[k] = op
            self.readers[k] = {}

    def _semslot(self, op):
        return op.semkey

    def op(self, eng, fn, r=(), w=()):
        o = Op(eng, fn)
        o.semkey = eng
        self._track(o, r, w)
        self.ops[eng].append(o)
        return o

    def dma(self, q, out, in_, **kw):
        i = self.dma_rr[q]
        self.dma_rr[q] = (i + 1) % N_DMA_SEMS
        o = Op(q, None, is_dma=True)
        o.semkey = ("dma", q, i)
        o.signal = True
        prev = self.dma_last.get((q, i))
        if prev is not None:
            o.deps.append(prev)
        self.dma_last[(q, i)] = o
        oa, ia = _ap(out), _ap(in_)
        eng = self._eng(q)
        o.fn = lambda: eng.dma_start(out=oa, in_=ia, **kw)
        self._track(o, [in_], [out])
        self.ops[q].append(o)
        return o

    def barrier(self):
        lasts = []
        for e in self.ENGS:
            for o in reversed(self.ops[e]):
                if not o.is_dma and o.fn is not None:
                    lasts.append(o)
                    break
        lasts += list(self.dma_last.values())
        for e in self.ENGS:
            o = Op(e, None)
            o.semkey = e
            o.deps = [d for d in lasts]
            self.ops[e].append(o)
        self.lastw = {}
        self.readers = {}

    def _eng(self, e):
        nc = self.nc
        return {"pe": nc.tensor, "dve": nc.vector, "act": nc.scalar, "pool": nc.gpsimd, "sp": nc.sync}[e]

    def mm(self, out, lhsT, rhs, start=True, stop=True, **kw):
        oa, la, ra = _ap(out), _ap(lhsT), _ap(rhs)
        pe = self.nc.tensor
        return self.op("pe", lambda: pe.matmul(oa, la, ra, start=start, stop=stop, **kw), r=[lhsT, rhs], w=[out])

    def tr(self, out, in_, ident):
        oa, ia, da = _ap(out), _ap(in_), _ap(ident)
        pe = self.nc.tensor
        return self.op("pe", lambda: pe.transpose(oa, ia, da), r=[in_, ident], w=[out])

    def act(self, out, in_, func, bias=None, scale=None, accum=None, eng="act"):
        oa, ia = _ap(out), _ap(in_)
        kw = {}
        r = [in_]
        w = [out]
        if bias is not None:
            kw["bias"] = _ap(bias)
            if isinstance(bias, V):
                r.append(bias)
        if scale is not None:
            kw["scale"] = _ap(scale)
            if isinstance(scale, V):
                r.append(scale)
        if accum is not None:
            kw["accum_out"] = _ap(accum)
            w.append(accum)
        sc = self.nc.scalar
        return self.op("act", lambda: sc.activation(oa, ia, func, **kw), r=r, w=w)

    def ts(self, eng, out, in0, s1, s2, op0, op1=None, accum=None):
        oa, ia = _ap(out), _ap(in0)
        r = [in0]
        w = [out]
        for s in (s1, s2):
            if isinstance(s, V):
                r.append(s)
        kw = {}
        if op1 is not None:
            kw["op1"] = op1
        if accum is not None:
            kw["accum_out"] = _ap(accum)
            w.append(accum)
        e = self._eng(eng)
        a1, a2 = _ap(s1), _ap(s2)
        return self.op(eng, lambda: e.tensor_scalar(oa, ia, a1, a2, op0, **kw), r=r, w=w)

    def tt(self, eng, out, in0, in1, op):
        oa, a0, a1 = _ap(out), _ap(in0), _ap(in1)
        e = self._eng(eng)
        return self.op(eng, lambda: e.tensor_tensor(oa, a0, a1, op), r=[in0, in1], w=[out])

    def stt(self, eng, out, in0, scalar, in1, op0, op1):
        oa, a0, a1, sa = _ap(out), _ap(in0), _ap(in1), _ap(scalar)
        r = [in0, in1]
        if isinstance(scalar, V):
            r.append(scalar)
        e = self._eng(eng)
        return self.op(eng, lambda: e.scalar_tensor_tensor(oa, a0, sa, a1, op0, op1), r=r, w=[out])

    def copy(self, eng, out, in_):
        oa, ia = _ap(out), _ap(in_)
        if eng == "act":
            sc = self.nc.scalar
            return self.op("act", lambda: sc.copy(oa, ia), r=[in_], w=[out])
        e = self._eng(eng)
        return self.op(eng, lambda: e.tensor_copy(oa, ia), r=[in_], w=[out])

    def memset(self, eng, out, val):
        oa = _ap(out)
        e = self._eng(eng)
        return self.op(eng, lambda: e.memset(oa, val), r=[], w=[out])

    def reduce(self, eng, out, in_, op, axis=AX.X):
        oa, ia = _ap(out), _ap(in_)
        e = self._eng(eng)
        return self.op(eng, lambda: e.tensor_reduce(oa, ia, axis, op), r=[in_], w=[out])

    def recip(self, out, in_):
        oa, ia = _ap(out), _ap(in_)
        e = self.nc.vector
        return self.op("dve", lambda: e.reciprocal(oa, ia), r=[in_], w=[out])

    def emit(self):
        nc = self.nc
        for e in self.ENGS:
            for o in self.ops[e]:
                for d in o.deps:
                    if d.eng == "pe" and o.eng == "pe" and not d.is_dma and not o.is_dma:
                        continue
                    d.signal = True
        cnt = {}
        n_ins = 0
        for e in self.ENGS:
            for o in self.ops[e]:
                if o.fn is None:
                    continue
                n_ins += 1
                if o.signal:
                    inc = 16 if o.is_dma else 1
                    cnt[o.semkey] = cnt.get(o.semkey, 0) + inc
                    o.semval = cnt[o.semkey]
        self.n_ins = n_ins
        self.sem_final = cnt
        semkeys = list(cnt.keys())
        from contextlib import ExitStack
        with ExitStack() as es:
            sems = {}
            for i, k in enumerate(semkeys):
                sems[k] = es.enter_context(nc.semaphore(f"s{i}"))
            block = es.enter_context(nc.Block())

            def replay(ename):
                def f(eng):
                    waited = {}
                    for o in self.ops[ename]:
                        need = {}
                        for d in o.deps:
                            if d.semval is None:
                                continue
                            if ename == "pe" and d.eng == "pe" and not d.is_dma and not o.is_dma:
                                continue
                            if need.get(d.semkey, 0) < d.semval:
                                need[d.semkey] = d.semval
                        for k, v in need.items():
                            if waited.get(k, 0) < v:
                                eng.wait_ge(sems[k], v)
                                waited[k] = v
                        if o.fn is None:
                            continue
                        ins = o.fn()
                        if o.signal:
                            ins.then_inc(sems[o.semkey], 16 if o.is_dma else 1)
                return f

            block.tensor(replay("pe"))
            block.vector(replay("dve"))
            block.scalar(replay("act"))
            block.gpsimd(replay("pool"))
            block.sync(replay("sp"))
        return nc


from concourse.bass_utils import run_bass_kernel_spmd
IndirectOffsetOnAxis = bass.IndirectOffsetOnAxis

D = 1024
LAT = 4096
CTX = 256
NT_ALL = (LAT + CTX) // 128
EPS = 1e-6
NEG = -1.0e30


class Ring:
    def __init__(self, p, n, cols, dt=F32, name="ring", parts=128):
        self.bufs = [p.alloc(cols, dt, name=f"{name}{i}", parts=parts) for i in range(n)]
        self.i = 0

    def next(self):
        b = self.bufs[self.i % len(self.bufs)]
        self.i += 1
        return b


class Ctx:
    def __init__(self):
        self.p = Prog()
        self.inputs = {}
        self.in_dt = {}

    def inp(self, name, shape, dt=F32):
        if name not in self.p.drams:
            self.p.dram(name, shape, dt, kind="ExternalInput")
            self.inputs[name] = tuple(shape)
            self.in_dt[name] = dt
        return self.p.drams[name]


def load_row_bc(p, q, dst, src_row):
    return p.dma(q, dst, V(src_row.ap.to_broadcast([dst.shape[0], src_row.shape[1]]), src_row.key))


def phase_mod(cx, layer, m_scr):
    p = cx.p
    mk = p.mark()
    cc = cx.inp("cc", [128, 16])
    w_ada = cx.inp("w_ada", [4, 1024, 6144])
    b_ada = cx.inp("b_ada", [4, 6144])
    cct = p.alloc(16)
    sil = p.alloc(16)
    p.dma("sp", cct, cc)
    p.act(sil, cct, AF.Silu)
    silv = sil.rearrange("p (a k) -> p a k", a=2)
    wr = Ring(p, 2, 8 * 512, name="wada")
    br = Ring(p, 2, 512, name="bada")
    mr = Ring(p, 2, 512, name="mrow")
    wsrc = w_ada[layer].rearrange("(k p) c -> p k c", p=128)
    for cb in range(12):
        wt = wr.next()
        p.dma("sp", wt.rearrange("p (k c) -> p k c", k=8), wsrc[:, :, cb * 512:(cb + 1) * 512])
        bt = br.next()
        load_row_bc(p, "act", bt[0:2, :], b_ada[layer:layer + 1, cb * 512:(cb + 1) * 512])
        ps = p.psum[cb % 2]
        for k in range(8):
            p.mm(ps[0:2, :], silv[:, :, k], wt[:, k * 512:(k + 1) * 512], start=(k == 0), stop=(k == 7))
        mt = mr.next()
        p.tt("dve", mt[0:2, :], ps[0:2, :], bt[0:2, :], ALU.add)
        p.dma("sp", m_scr[layer, :, cb * 512:(cb + 1) * 512], mt[0:2, :])
    p.barrier()
    p.release(mk)


class NormMod:
    def __init__(self, cx, layer, xs, m_scr, norm_name, i_shift, i_scale, nbuf=2, shared_ab=False, junk=None):
        p = cx.p
        self.p = p
        self.xs = xs
        nrm = cx.inp(norm_name, [4, 1024])
        self.A = {}
        self.B = {}
        self.shared_ab = shared_ab
        self.layer, self.m_scr, self.i_shift, self.i_scale = layer, m_scr, i_shift, i_scale
        g = p.alloc(1024, name="g_row")
        self.g = g
        load_row_bc(p, "act", g, nrm[layer:layer + 1, :])
        if shared_ab:
            self.Ash = p.alloc(1024, name="A_row")
            self.Bsh = p.alloc(1024, name="B_row")
            self.cur_v = None
        else:
            for v in (0, 1):
                a = p.alloc(1024, name="A_row")
                b = p.alloc(1024, name="B_row")
                self._load_ab(a, b, v)
                self.A[v] = a
                self.B[v] = b
        self.xr = Ring(p, nbuf, 1024, name="xt")
        self.hr = Ring(p, nbuf, 1024, name="ht")
        self.junk = junk if junk is not None else p.alloc(1024, name="junk")
        self.st = Ring(p, nbuf, 4, name="stat")

    def _load_ab(self, a, b, v):
        p = self.p
        load_row_bc(p, "act", a, self.m_scr[self.layer, v:v + 1, self.i_scale * 1024:(self.i_scale + 1) * 1024])
        load_row_bc(p, "act", b, self.m_scr[self.layer, v:v + 1, self.i_shift * 1024:(self.i_shift + 1) * 1024])
        p.stt("dve", a, a, 1.0, self.g, ALU.add, ALU.mult)

    def tile(self, ti):
        p = self.p
        v = 1 if ti < 2 else 0
        if self.shared_ab:
            if self.cur_v != v:
                self._load_ab(self.Ash, self.Bsh, v)
                self.cur_v = v
            self.A[v] = self.Ash
            self.B[v] = self.Bsh
        xt = self.xr.next()
        p.dma("sp", xt, self.xs[ti * 128:(ti + 1) * 128, :])
        st = self.st.next()
        p.act(self.junk, xt, AF.Square, accum=st[:, 0:1])
        p.act(st[:, 1:2], st[:, 0:1], AF.Sqrt, scale=1.0 / D, bias=self.eps_ap())
        p.recip(st[:, 2:3], st[:, 1:2])
        ht = self.hr.next()
        p.stt("dve", ht, xt, st[:, 2:3], self.A[v], ALU.mult, ALU.mult)
        p.tt("pool", ht, ht, self.B[v], ALU.add)
        return xt, ht

    def eps_ap(self):
        if not hasattr(self, "_eps"):
            self._eps = self.p.alloc(1, name="eps")
            self.p.memset("dve", self._eps, EPS)
        return self._eps


def transpose_tile(p, ident, src, dst_fn, banks, evac_eng="act"):
    for j in range(2):
        ps = p.psum[banks[j]]
        for c in range(4):
            k = 4 * j + c
            p.tr(ps[:, c * 128:(c + 1) * 128], src[:, k * 128:(k + 1) * 128], ident)
        dst = dst_fn(j)
        p.copy(evac_eng, dst, ps.rearrange("p (c t) -> p c t", c=4))


def phase_peer(cx, layer, xs, m_scr, tiles):
    p = cx.p
    mk = p.mark()
    ident_d = cx.inp("ident", [128, 128])
    iota_d = cx.inp("iota16", [128, 16])
    wq_d = cx.inp("peer_wq", [4, 1024, 2048])
    keysT_d = cx.inp("peer_keysT", [4, 128, 2048])
    pu = cx.inp("peer_u", [4, 16384, 1024])
    pv = cx.inp("peer_v", [4, 16384, 1024])
    pu_flat = pu.rearrange("l e d -> (l e) d")
    pv_flat = pv.rearrange("l e d -> (l e) d")

    uvb_d = p.dram(f"peer_uvb{layer}", [16384, 2048], BF16)
    mkc = p.mark()
    RB = 8
    cin = Ring(p, 2, RB * 1024, name="cast_in")
    cout = Ring(p, 2, RB * 1024, BF16, name="cast_out")
    ci = 0
    for (src, dst) in ((pu[layer], uvb_d[:, 0:1024]), (pv[layer], uvb_d[:, 1024:2048])):
        for r0 in range(0, 16384, 128 * RB):
            a = cin.next()
            b = cout.next()
            p.dma("sp", a, src[r0:r0 + 128 * RB, :].rearrange("(p r) d -> p (r d)", r=RB))
            eng = ("dve", "act", "pool")[ci % 3]
            p.copy(eng, b, a)
            p.dma("act", dst[r0:r0 + 128 * RB, :].rearrange("(p r) d -> p r d", r=RB), b.rearrange("p (r d) -> p r d", r=RB))
            ci += 1
    p.barrier()
    p.release(mkc)
    ident = p.alloc(128, name="ident")
    p.dma("act", ident, ident_d)
    iota = p.alloc(16, name="iota")
    p.dma("act", iota, iota_d)
    wq = p.alloc(8 * 2048, name="wq")
    p.dma("sp", wq.rearrange("p (k c) -> p k c", k=8), wq_d[layer].rearrange("(k p) c -> p k c", p=128))
    keysT = p.alloc(2048, name="keysT")
    p.dma("act", keysT, keysT_d[layer])
    G2 = {}
    for v in (0, 1):
        if v == 1 and tiles[0] >= 2:
            continue
        G2[v] = p.alloc(1024, name="G2")
        load_row_bc(p, "act", G2[v], m_scr[layer, v:v + 1, 5 * 1024:6 * 1024])
    junk = p.alloc(1024, name="pjunk")
    nm = NormMod(cx, layer, xs, m_scr, "norm2", 3, 4, nbuf=2, shared_ab=True, junk=junk)

    h2T = p.alloc(1024, name="h2T")
    qTt = p.alloc(2048, name="qTt")
    sc = p.alloc(2048, name="sc")
    sc2 = Ring(p, 2, 128, name="sc2")
    sv = p.alloc(256, name="sv")
    si = p.alloc(256, U32, name="si")
    sif = p.alloc(256, name="sif")
    cand = sc
    cand2 = Ring(p, 2, 256, name="cand2")
    ts_ = p.alloc(128, name="ts")
    pos = p.alloc(128, U32, name="pos")
    ipos = p.alloc(128, U32, name="ipos")
    jpos = p.alloc(128, U32, name="jpos")
    iposf = p.alloc(128, name="iposf")
    jposf = p.alloc(128, name="jposf")
    eq = qTt
    asel = p.alloc(128, name="asel")
    bsel = p.alloc(128, name="bsel")
    eidf = p.alloc(128, name="eidf")
    eid_r = Ring(p, 2, 128, U32, name="eid")
    gate_r = Ring(p, 2, 128, name="gate")
    gz = p.alloc(16, name="gz")
    actv = p.alloc(128, name="actv")
    coef = p.alloc(128, name="coef")
    acc = p.alloc(1024, name="acc")
    ug = Ring(p, 12, 2048, BF16, name="uvg")
    gl = p.alloc(128, name="gelu_a")

    sv4 = sv.rearrange("p (h a i) -> p h a i", h=8, a=2)
    sif4 = sif.rearrange("p (h a i) -> p h a i", h=8, a=2)
    cand4 = cand.rearrange("p (h i j) -> p h i j", h=8, i=16)
    ts3 = ts_.rearrange("p (h k) -> p h k", h=8)
    eq4 = eq.rearrange("p (h k i) -> p h k i", h=8, k=16)
    iota_b = iota.unsq(1).unsq(1).bc([128, 8, 16, 16])

    def routing(ti, R):
        v = 1 if ti < 2 else 0
        xt, h2 = nm.tile(ti)
        eid = eid_r.next()
        gate = gate_r.next()
        R.update(ti=ti, v=v, xt=xt, h2=h2, eid=eid, gate=gate)
        yield
        transpose_tile(p, ident, h2, lambda j: h2T.rearrange("p (k t) -> p k t", k=8)[:, 4 * j:4 * j + 4, :], (0, 1))
        yield
        for g4 in range(4):
            ps = p.psum[2 + g4]
            for c in range(4):
                hp = 4 * g4 + c
                for k in range(8):
                    p.mm(ps[:, c * 128:(c + 1) * 128], wq[:, k * 2048 + hp * 128:k * 2048 + (hp + 1) * 128],
                         h2T[:, k * 128:(k + 1) * 128], start=(k == 0), stop=(k == 7))
                yield
            p.copy("act", qTt[:, g4 * 512:(g4 + 1) * 512], ps)
        for g4 in range(4):
            ps = p.psum[g4]
            for c in range(4):
                hp = 4 * g4 + c
                p.mm(ps[:, c * 128:(c + 1) * 128], qTt[:, hp * 128:(hp + 1) * 128], keysT[:, hp * 128:(hp + 1) * 128])
            p.copy("act", sc[:, g4 * 512:(g4 + 1) * 512], ps)
            yield
        for hp in range(16):
            s_hp = sc[:, hp * 128:(hp + 1) * 128]
            s2_hp = sc2.next()
            o8a = sv[:, hp * 16:hp * 16 + 8]
            o8b = sv[:, hp * 16 + 8:hp * 16 + 16]
            _max8(p, o8a, s_hp)
            _match_replace(p, s2_hp, o8a, s_hp)
            _max8(p, o8b, s2_hp)
            _max_index(p, si[:, hp * 16:hp * 16 + 8], o8a, s_hp)
            _max_index(p, si[:, hp * 16 + 8:hp * 16 + 16], o8b, s_hp)
            yield
        p.copy("dve", sif, si)
        p.tt("dve", cand4, sv4[:, :, 0, :].unsq(3).bc([128, 8, 16, 16]), sv4[:, :, 1, :].unsq(2).bc([128, 8, 16, 16]), ALU.add)
        for h in range(8):
            c_h = cand[:, h * 256:(h + 1) * 256]
            c2_h = cand2.next()
            o8a = ts_[:, h * 16:h * 16 + 8]
            o8b = ts_[:, h * 16 + 8:h * 16 + 16]
            _max8(p, o8a, c_h)
            _match_replace(p, c2_h, o8a, c_h)
            _max8(p, o8b, c2_h)
            _max_index(p, pos[:, h * 16:h * 16 + 8], o8a, c_h)
            _max_index(p, pos[:, h * 16 + 8:h * 16 + 16], o8b, c_h)
            yield
        p.ts("dve", ipos, pos, 4, None, ALU.logical_shift_right)
        p.ts("dve", jpos, pos, 15, None, ALU.bitwise_and)
        p.copy("dve", iposf, ipos)
        p.copy("dve", jposf, jpos)
        for (dst, posf, a) in ((asel, iposf, 0), (bsel, jposf, 1)):
            pf3 = posf.rearrange("p (h k) -> p h k", h=8)
            p.tt("dve", eq4, iota_b, pf3.unsq(3).bc([128, 8, 16, 16]), ALU.is_equal)
            p.tt("dve", eq4, eq4, sif4[:, :, a, :].unsq(2).bc([128, 8, 16, 16]), ALU.mult)
            p.reduce("dve", dst.rearrange("p (h k) -> p h k", h=8), eq4, ALU.add, AX.X)
            yield
        p.stt("dve", eidf, asel, 128.0, bsel, ALU.mult, ALU.add)
        p.copy("dve", eid, eidf)
        p.tt("dve", gate.rearrange("p (h k) -> p h k", h=8), ts3, ts3[:, :, 0:1].bc([128, 8, 16]), ALU.subtract)
        p.act(gate, gate, AF.Exp)
        p.reduce("dve", gz[:, 0:8], gate.rearrange("p (h k) -> p h k", h=8), ALU.add, AX.X)
        p.recip(gz[:, 8:16], gz[:, 0:8])
        p.tt("dve", gate.rearrange("p (h k) -> p h k", h=8), gate.rearrange("p (h k) -> p h k", h=8),
             gz[:, 8:16].unsq(2).bc([128, 8, 16]), ALU.mult)
        yield

    def slots(R, gen):
        ti, v, xt, h2, eid, gate = R["ti"], R["v"], R["xt"], R["h2"], R["eid"], R["gate"]
        accA, accB = p.psum[6], p.psum[7]
        GS = 4
        pend = None

        def finish(g):
            s0_, bs_ = g
            p.tt("dve", coef[:, s0_:s0_ + GS], gl[:, s0_:s0_ + GS], gate[:, s0_:s0_ + GS], ALU.mult)
            dg = dg_r.next()
            dg3 = dg.rearrange("p (g c) -> p g c", g=GS)
            p.tt("dve", dg3, ident_b.unsq(1).bc([128, GS, 128]), coef[:, s0_:s0_ + GS].unsq(2).bc([128, GS, 128]), ALU.mult)
            for j, s in enumerate(range(s0_, s0_ + GS)):
                p.mm(accA, dg[:, j * 128:(j + 1) * 128], bs_[j][:, 1024:1536], start=(s == 0), stop=(s == 127))
                p.mm(accB, dg[:, j * 128:(j + 1) * 128], bs_[j][:, 1536:2048], start=(s == 0), stop=(s == 127))
                if gen is not None:
                    next(gen, None)

        for s0 in range(0, 128, GS):
            bs = []
            for s in range(s0, s0 + GS):
                b = ug.next()
                bs.append(b)
                _gather(p, b, uvb_d, eid[:, s:s + 1])
                _ttr(p, junk, b[:, 0:1024], h2, actv[:, s:s + 1])
            p.act(gl[:, s0:s0 + GS], actv[:, s0:s0 + GS], AF.Gelu)
            if pend is not None:
                finish(pend)
            pend = (s0, bs)
        finish(pend)
        p.tt("dve", acc[:, 0:512], accA, G2[v][:, 0:512], ALU.mult)
        p.tt("dve", acc[:, 512:1024], accB, G2[v][:, 512:1024], ALU.mult)
        p.tt("dve", acc, acc, xt, ALU.add)
        p.dma("sp", xs[ti * 128:(ti + 1) * 128, :], acc)

    ident_b = p.alloc(128, BF16, name="ident_b")
    p.copy("dve", ident_b, ident)
    dg_r = Ring(p, 3, 4 * 128, BF16, name="diag")
    cur = {}
    for _ in routing(tiles[0], cur):
        pass
    for i in range(len(tiles)):
        nxt = {}
        gen = routing(tiles[i + 1], nxt) if i + 1 < len(tiles) else None
        slots(cur, gen)
        if gen is not None:
            for _ in gen:
                pass
        cur = nxt
    p.barrier()
    p.release(mk)


def _max8(p, out, in_):
    oa, ia = out.ap, in_.ap
    e = p.nc.vector
    return p.op("dve", lambda: e.max(oa, ia), r=[in_], w=[out])


def _max_index(p, out, in_max, in_values):
    oa, ma, va = out.ap, in_max.ap, in_values.ap
    e = p.nc.vector
    return p.op("dve", lambda: e.max_index(oa, ma, va), r=[in_max, in_values], w=[out])


def _match_replace(p, out, in_to_replace, in_values):
    oa, ra, va = out.ap, in_to_replace.ap, in_values.ap
    e = p.nc.vector
    return p.op("dve", lambda: e.match_replace(oa, ra, va, NEG), r=[in_to_replace, in_values], w=[out])


def _ttr(p, out, in0, in1, accum):
    oa, a0, a1, ac = out.ap, in0.ap, in1.ap, accum.ap
    e = p.nc.vector
    return p.op("dve", lambda: e.scalar_tensor_tensor(oa, a0, 1.0, a1, ALU.mult, ALU.mult, accum_out=ac),
                r=[in0, in1], w=[out, accum])


def _gather(p, out, table, idx):
    q = "pool"
    i = p.dma_rr[q]
    p.dma_rr[q] = (i + 1) % N_DMA_SEMS
    o = Op(q, None, is_dma=True)
    o.semkey = ("dma", q, i)
    o.signal = True
    prev = p.dma_last.get((q, i))
    if prev is not None:
        o.deps.append(prev)
    p.dma_last[(q, i)] = o
    oa, ta, ia = out.ap, table.ap, idx.ap
    g = p.nc.gpsimd
    o.fn = lambda: g.indirect_dma_start(oa, None, ta, IndirectOffsetOnAxis(ia, 0))
    p._track(o, [table, idx], [out])
    p.ops[q].append(o)
    return o


def dram_copy(p, dst, src, rows, q="sp", chunk=512):
    for r0 in range(0, rows, chunk):
        r1 = min(rows, r0 + chunk)
        p.dma(q, dst[r0:r1, :], src[r0:r1, :])


def build(cfg):
    cx = Ctx()
    p = cx.p
    xs = p.dram("xs", [LAT + CTX, D], F32)
    m_scr = p.dram("m_scr", [4, 2, 6144], F32)
    x_in = cx.inp("x", [LAT, D])
    ctx_in = cx.inp("ctx", [CTX, D])
    out = p.dram("out", [LAT, D], F32, kind="ExternalOutput")
    dram_copy(p, xs[CTX:, :], x_in, LAT)
    dram_copy(p, xs[0:CTX, :], ctx_in, CTX, q="act")
    p.barrier()
    test = cfg.get("test")
    if test is None:
        build_full(cx, xs, m_scr, out)
    if test == "layers":
        build_full(cx, xs, m_scr, out, layers=cfg["layers"])
        dbg = p.dram("dbg_xs", [LAT + CTX, D], F32, kind="ExternalOutput")
        dram_copy(p, dbg, xs, LAT + CTX)
    if test == "odd":
        layer = cfg["layer"]
        phase_mod(cx, layer, m_scr)
        phase_odd(cx, layer, xs, m_scr, layer != 3)
        dbg = p.dram("dbg_xs", [LAT + CTX, D], F32, kind="ExternalOutput")
        dram_copy(p, dbg, xs, LAT + CTX)
    if test == "even":
        layer = cfg["layer"]
        S = even_scratch(p, layer)
        phase_mod(cx, layer, m_scr)
        for ph in cfg["phases"]:
            {"proj": phase_even_proj, "gdn": phase_gdn, "gdn_out": phase_gdn_out, "hy": phase_hyena}[ph](*((cx, layer, xs, m_scr, S) if ph == "proj" else (cx, layer, S)))
        for nm in cfg.get("dump", []):
            src = S[nm]
            shp = list(src.shape)
            dd = p.dram("dbg_" + nm, shp, src.ap.dtype, kind="ExternalOutput")
            if len(shp) == 3:
                for g in range(shp[0]):
                    p.dma("sp", dd[g], src[g])
            else:
                dram_copy(p, dd, src, shp[0], chunk=1024)
    if test == "peer":
        layer = cfg["layer"]
        phase_mod(cx, layer, m_scr)
        phase_peer(cx, layer, xs, m_scr, cfg["tiles"])
        dbg = p.dram("dbg_xs", [LAT + CTX, D], F32, kind="ExternalOutput")
        dram_copy(p, dbg, xs, LAT + CTX)
        dbm = p.dram("dbg_m", [2, 6144], F32, kind="ExternalOutput")
        p.dma("sp", dbm, m_scr[layer])
    p.barrier()
    nc = p.emit()
    return cx, nc


def host_inputs(cx, inputs, b):
    m = {}
    f32 = np.float32
    for name in cx.inputs:
        if name == "x":
            a = inputs["x"][b]
        elif name == "ctx":
            a = inputs["ctx"][b]
        elif name == "cc":
            a = np.concatenate([inputs["c"][b].reshape(8, 128).T, inputs["c_ctx"].reshape(8, 128).T], axis=1)
        elif name == "ident":
            a = np.eye(128, dtype=f32)
        elif name == "iota16":
            a = np.tile(np.arange(16, dtype=f32)[None, :], (128, 1))
        elif name == "rope_cs":
            a = rope_table()
        elif name == "conv_wT":
            a = inputs["conv_w"].reshape(2, 3, 24, 128).transpose(0, 3, 2, 1).reshape(2, 128, 72)
        elif name == "gdn_masks":
            i = np.arange(64)[:, None]; j = np.arange(64)[None, :]
            a = np.concatenate([(i <= j), (i >= j), (i < j), (i > j)], axis=1).astype(f32)
        elif name == "final_norm":
            a = inputs["final_norm"].reshape(1, 1024)
        elif name == "peer_keysT":
            a = inputs["peer_keys"].transpose(0, 4, 1, 2, 3).reshape(4, 128, 2048)
        elif name.startswith("dft"):
            L = int(name[3:].split("_")[0])
            a = dft_tables(L)[name.split("_")[1]]
        elif name.startswith("hy_zT"):
            a = hyena_consts(int(name[5:]))[0]
        elif name.startswith("hy_win"):
            a = hyena_consts(int(name[6:]))[1]
        else:
            a = inputs[name]
        if cx.in_dt[name] == BF16:
            m[name] = np.ascontiguousarray(a)
            assert m[name].dtype == _mld.bfloat16
        else:
            m[name] = np.ascontiguousarray(a, dtype=f32)
        assert m[name].shape == cx.inputs[name], (name, m[name].shape, cx.inputs[name])
    return m


def make_hT(cx, layer, xs, m_scr, ident, hT):
    p = cx.p
    mk = p.mark()
    nm = NormMod(cx, layer, xs, m_scr, "norm1", 0, 1, nbuf=2)
    hT3 = hT.rearrange("p (k t) -> p k t", k=8)
    for ti in range(NT_ALL):
        xt, h = nm.tile(ti)
        b0 = (ti % 2) * 2
        transpose_tile(p, ident, h, lambda j: hT3[:, 4 * j:4 * j + 4, ti * 128:(ti + 1) * 128], (b0, b0 + 1),
                       evac_eng=("act" if ti % 2 == 0 else "dve"))
    p.barrier()
    p.release(mk)


def cast_weight(p, dst3, src3, ncols, stage_ring, blk=512):
    K = dst3.shape[1]
    i = 0
    for c0 in range(0, ncols, blk):
        c1 = min(ncols, c0 + blk)
        st = stage_ring.next()
        sv = st.rearrange("p (k c) -> p k c", k=K)[:, :, 0:c1 - c0]
        p.dma("sp" if i % 2 == 0 else "act", sv, src3[:, :, c0:c1])
        p.copy("pool" if i % 2 == 0 else "dve", dst3[:, :, c0:c1], sv)
        i += 1


def phase_odd(cx, layer, xs, m_scr, need_ctx):
    import math
    p = cx.p
    o = layer // 2
    lam_init = 0.8 - 0.6 * math.exp(-0.3 * layer)
    mk0 = p.mark()
    ident_d = cx.inp("ident", [128, 128])
    w_qkv = cx.inp("w_qkv", [2, 1024, 3072])
    w_out = cx.inp("w_out_odd", [2, 1024, 1024])
    rope_d = cx.inp("rope_cs", [LAT, 64])
    subln_d = cx.inp("subln", [2, 128])
    lam_d = {n: cx.inp(n, [2, 64]) for n in ("lam_q1", "lam_k1", "lam_q2", "lam_k2")}
    NTOK = LAT + CTX
    qkT_d = p.dram(f"qkT_d{layer}", [16, 128, NTOK], BF16)
    v_d = p.dram(f"v_d{layer}", [NTOK, 1024], BF16)

    ident = p.alloc(128, name="ident")
    p.dma("act", ident, ident_d)
    hT = p.alloc(8 * NTOK, BF16, name="hT")
    make_hT(cx, layer, xs, m_scr, ident, hT)
    hT3 = hT.rearrange("p (k t) -> p k t", k=8)

    mk = p.mark()
    wb = p.alloc(8 * 3072, BF16, name="wqkv_b")
    wb3 = wb.rearrange("p (k c) -> p k c", k=8)
    stg = Ring(p, 2, 8 * 512, name="wstage")
    cast_weight(p, wb3, w_qkv[o].rearrange("(k p) c -> p k c", p=128), 3072, stg)
    qk_r = Ring(p, 2, 2048, name="qk_t")
    rot_r = Ring(p, 2, 2048, name="qk_rot")
    tmp_r = Ring(p, 2, 1024, name="rtmp")
    cs_r = Ring(p, 2, 64, name="cs")
    qkTs_r = Ring(p, 2, 16 * 128, BF16, name="qkTs")
    vs_r = Ring(p, 2, 1024, BF16, name="vs")
    for ti in range(NT_ALL):
        qk = qk_r.next()
        for cb in range(6):
            ps = p.psum[(ti * 6 + cb) % 4]
            for k in range(8):
                p.mm(ps, hT3[:, k, ti * 128:(ti + 1) * 128], wb3[:, k, cb * 512:(cb + 1) * 512], start=(k == 0), stop=(k == 7))
            if cb < 4:
                p.copy("act", qk[:, cb * 512:(cb + 1) * 512], ps)
            else:
                if cb == 4:
                    vs = vs_r.next()
                p.copy("act", vs[:, (cb - 4) * 512:(cb - 3) * 512], ps)
        p.dma("act", v_d[ti * 128:(ti + 1) * 128, :], vs)
        if ti >= 2:
            cs = cs_r.next()
            p.dma("sp", cs, rope_d[(ti - 2) * 128:(ti - 1) * 128, :])
            rot = rot_r.next()
            x5 = qk.rearrange("p (g a h f) -> p g a h f", g=32, a=2, h=2)
            r5 = rot.rearrange("p (g a h f) -> p g a h f", g=32, a=2, h=2)
            cosb = cs[:, 0:32].rearrange("p (a f) -> p a f", a=2).unsq(1).bc([128, 32, 2, 16])
            sinb = cs[:, 32:64].rearrange("p (a f) -> p a f", a=2).unsq(1).bc([128, 32, 2, 16])
            x1, x2 = x5[:, :, :, 0, :], x5[:, :, :, 1, :]
            t = tmp_r.next().rearrange("p (g a f) -> p g a f", g=32, a=2)
            t2 = tmp_r.next().rearrange("p (g a f) -> p g a f", g=32, a=2)
            p.tt("dve", t, x2, sinb, ALU.mult)
            p.tt("pool", t2, x1, sinb, ALU.mult)
            p.tt("dve", r5[:, :, :, 0, :], x1, cosb, ALU.mult)
            p.tt("pool", r5[:, :, :, 1, :], x2, cosb, ALU.mult)
            p.tt("dve", r5[:, :, :, 0, :], r5[:, :, :, 0, :], t, ALU.subtract)
            p.tt("pool", r5[:, :, :, 1, :], r5[:, :, :, 1, :], t2, ALU.add)
            src = rot
        else:
            src = qk
        qkTs = qkTs_r.next()
        for g4 in range(4):
            ps = p.psum[4 + g4]
            for c in range(4):
                blk = 4 * g4 + c
                p.tr(ps[:, c * 128:(c + 1) * 128], src[:, blk * 128:(blk + 1) * 128], ident)
            p.copy("dve" if g4 % 2 == 0 else "act", qkTs[:, g4 * 512:(g4 + 1) * 512], ps)
        p.dma("sp", qkT_d[:, :, ti * 128:(ti + 1) * 128].rearrange("g p t -> p g t"),
              qkTs.rearrange("p (g t) -> p g t", g=16))
    p.barrier()
    p.release(mk)
    p.release(mk0)

    mk = p.mark()
    onT = p.alloc(8 * NTOK, BF16, name="onT")
    onT3 = onT.rearrange("p (h t) -> p h t", h=8)
    ones_b = p.alloc(128, BF16, name="ones_b")
    p.memset("dve", ones_b, 1.0)
    eps_t = p.alloc(1, name="eps")
    p.memset("dve", eps_t, EPS)
    lt = p.alloc(4 * 64, name="lamv")
    for i, n in enumerate(("lam_q1", "lam_k1", "lam_q2", "lam_k2")):
        load_row_bc(p, "act", lt[:, i * 64:(i + 1) * 64], lam_d[n][o:o + 1, :])
    ls = p.alloc(8, name="lams")
    lj = p.alloc(64, name="lamj")
    _ttr(p, lj, lt[:, 0:64], lt[:, 64:128], ls[:, 0:1])
    _ttr(p, lj, lt[:, 128:192], lt[:, 192:256], ls[:, 1:2])
    p.act(ls[:, 2:4], ls[:, 0:2], AF.Exp)
    p.stt("dve", ls[:, 4:5], ls[:, 3:4], -lam_init, ls[:, 2:3], ALU.add, ALU.subtract)
    neg_lam = ls[:, 4:5]
    sg = p.alloc(2, name="sg")
    p.dma("act", sg[:, 0:1], subln_d[o].rearrange("(p o) -> p o", o=1))
    p.ts("dve", sg[:, 1:2], sg[:, 0:1], 1.0 - lam_init, None, ALU.mult)
    sgs = sg[:, 1:2]

    mk_att = p.mark()
    kT_r = Ring(p, 2, NTOK, BF16, name="kT_h")
    qT_r = Ring(p, 2, NTOK, BF16, name="qT_h")
    v_r = Ring(p, 2, NT_ALL * 128, BF16, name="v_h")
    pt_r = Ring(p, 4, 512, BF16, name="PT")
    rz_r = Ring(p, 2, 512, name="rz")
    o0_r = Ring(p, 2, 512, name="o0")
    o1_r = Ring(p, 2, 512, name="o1")
    sq_r = Ring(p, 2, 512, BF16, name="sq")
    rs_r = Ring(p, 2, 512, name="rs")
    scale = 0.125
    it = 0
    for h in range(8):
        kT = kT_r.next()
        qT = qT_r.next()
        vh = v_r.next()
        p.dma("sp", kT, qkT_d[8 + h])
        p.dma("act", qT, qkT_d[h])
        p.dma("sp", vh.rearrange("p (t d) -> p t d", d=128),
              v_d[:, h * 128:(h + 1) * 128].rearrange("(t p) d -> p t d", p=128))
        blocks = [(CTX + qb * 512, 512, list(range(NT_ALL))) for qb in range(LAT // 512)]
        if need_ctx:
            blocks.append((0, CTX, [0, 1]))
        for (q0, nq, kts) in blocks:
            o0 = o0_r.next()
            o1 = o1_r.next()
            for m in range(2):
                psO = p.psum[2 + (it % 2)]
                psZ = p.psum[4 + (it % 2)]
                it += 1
                def s_mm(i):
                    kt_ = kts[i]
                    p.mm(p.psum[i % 2][:, 0:nq], kT[m * 64:(m + 1) * 64, kt_ * 128:(kt_ + 1) * 128], qT[m * 64:(m + 1) * 64, q0:q0 + nq])
                s_mm(0)
                for i, kt in enumerate(kts):
                    psS = p.psum[i % 2]
                    if i + 1 < len(kts):
                        s_mm(i + 1)
                    pt = pt_r.next()
                    p.act(pt[:, 0:nq], psS[:, 0:nq], AF.Exp, scale=scale)
                    p.mm(psO[:, 0:nq], vh[:, kt * 128:(kt + 1) * 128], pt[:, 0:nq], start=(i == 0), stop=(i == len(kts) - 1))
                    p.mm(psZ[:, 0:nq], ones_b, pt[:, 0:nq], start=(i == 0), stop=(i == len(kts) - 1))
                rz = rz_r.next()
                p.recip(rz[:, 0:nq], psZ[:, 0:nq])
                if m == 0:
                    p.tt("dve", o0[:, 0:nq], psO[:, 0:nq], rz[:, 0:nq], ALU.mult)
                else:
                    p.tt("dve", o1[:, 0:nq], psO[:, 0:nq], rz[:, 0:nq], ALU.mult)
                    p.stt("dve", o0[:, 0:nq], o1[:, 0:nq], neg_lam, o0[:, 0:nq], ALU.mult, ALU.add)
            sq = sq_r.next()
            p.tt("pool", sq[:, 0:nq], o0[:, 0:nq], o0[:, 0:nq], ALU.mult)
            psR = p.psum[6]
            p.mm(psR[:, 0:nq], ones_b, sq[:, 0:nq])
            rs = rs_r.next()
            p.act(rs[:, 0:nq], psR[:, 0:nq], AF.Sqrt, scale=1.0 / 128, bias=eps_t)
            p.recip(rs[:, 0:nq], rs[:, 0:nq])
            p.stt("dve", onT3[:, h, q0:q0 + nq], o0[:, 0:nq], sgs, rs[:, 0:nq], ALU.mult, ALU.mult)

    p.barrier()
    p.release(mk_att)
    wo = p.alloc(8 * 1024, BF16, name="wo_b")
    wo3 = wo.rearrange("p (h c) -> p h c", h=8)
    stg = Ring(p, 2, 8 * 512, name="wstage2")
    cast_weight(p, wo3, w_out[o].rearrange("(h p) c -> p h c", p=128), 1024, stg)
    G1 = {}
    for v in ((0, 1) if need_ctx else (0,)):
        G1[v] = p.alloc(1024, name="G1")
        load_row_bc(p, "act", G1[v], m_scr[layer, v:v + 1, 2 * 1024:3 * 1024])
    xr = Ring(p, 2, 1024, name="xres")
    yr = Ring(p, 2, 1024, name="yres")
    for ti in range(0 if need_ctx else 2, NT_ALL):
        v = 1 if ti < 2 else 0
        xt = xr.next()
        p.dma("sp", xt, xs[ti * 128:(ti + 1) * 128, :])
        yt = yr.next()
        for cb in range(2):
            ps = p.psum[(ti * 2 + cb) % 4]
            for h in range(8):
                p.mm(ps, onT3[:, h, ti * 128:(ti + 1) * 128], wo3[:, h, cb * 512:(cb + 1) * 512], start=(h == 0), stop=(h == 7))
            p.tt("dve", yt[:, cb * 512:(cb + 1) * 512], ps, G1[v][:, cb * 512:(cb + 1) * 512], ALU.mult)
        p.tt("pool", yt, yt, xt, ALU.add)
        p.dma("act", xs[ti * 128:(ti + 1) * 128, :], yt)
    p.barrier()
    p.release(mk)
    p.release(mk0)


def rope_table():
    rows = LAT // 64
    r, c = np.meshgrid(np.arange(rows), np.arange(64), indexing="ij")
    pos = np.stack([r.reshape(-1), c.reshape(-1)], axis=-1).astype(np.float32)
    inv = (10000.0 ** (-np.arange(16, dtype=np.float32) / 16)).astype(np.float32)
    ang = pos[:, :, None] * inv
    return np.concatenate([np.cos(ang).reshape(LAT, 32), np.sin(ang).reshape(LAT, 32)], axis=1).astype(np.float32)


NTOK = LAT + CTX
NCH = NTOK // 64
SEQS = ((0, CTX), (CTX, LAT))


def even_scratch(p, layer):
    s = {}
    s["qT"] = p.dram(f"e{layer}_qT", [4, 128, NTOK], F32)
    s["kT"] = p.dram(f"e{layer}_kT", [4, 128, NTOK], F32)
    s["k_tok"] = p.dram(f"e{layer}_ktok", [NTOK, 512], F32)
    s["v_tok"] = p.dram(f"e{layer}_vtok", [NTOK, 512], F32)
    s["zs"] = p.dram(f"e{layer}_zs", [NTOK, 512], F32)
    s["ba"] = p.dram(f"e{layer}_ba", [64, NCH * 16], F32)
    s["hyx"] = p.dram(f"e{layer}_hyx", [8, 128, NTOK], F32)
    s["hyv"] = p.dram(f"e{layer}_hyv", [NTOK, 512], BF16)
    s["o_f"] = p.dram(f"e{layer}_of", [NTOK, 512], F32)
    s["o_b"] = p.dram(f"e{layer}_ob", [NTOK, 512], F32)
    s["actT"] = p.dram(f"e{layer}_actT", [8, 128, NTOK], BF16)
    return s


def phase_even_proj(cx, layer, xs, m_scr, S):
    p = cx.p
    e = layer // 2
    mk0 = p.mark()
    ident_d = cx.inp("ident", [128, 128])
    w_in = cx.inp("w_in", [2, 1024, 3600])
    convT_d = cx.inp("conv_wT", [2, 128, 72])
    ident = p.alloc(128, name="ident")
    p.dma("act", ident, ident_d)
    hT = p.alloc(8 * NTOK, BF16, name="hT")
    make_hT(cx, layer, xs, m_scr, ident, hT)
    hT3 = hT.rearrange("p (k t) -> p k t", k=8)
    wb = p.alloc(8 * 3600, BF16, name="win_b")
    wb3 = wb.rearrange("p (k c) -> p k c", k=8)
    mks = p.mark()
    stg = Ring(p, 2, 8 * 512, name="wstage")
    cast_weight(p, wb3, w_in[e].rearrange("(k p) c -> p k c", p=128), 3600, stg)
    p.barrier()
    p.release(mks)
    cw = p.alloc(72, name="convw")
    p.dma("act", cw, convT_d[e])
    ones_f = p.alloc(128, name="ones_f")
    p.memset("dve", ones_f, 1.0)
    eps_t = p.alloc(1, name="eps")
    p.memset("dve", eps_t, EPS)

    PB = NTOK + 4
    pb_r = Ring(p, 1, PB, name="pbuf")
    cv_r = Ring(p, 2, NTOK, name="cv")
    sq_r = Ring(p, 2, 512, name="sq")
    rn_r = Ring(p, 2, 512, name="rn")
    tk_r = Ring(p, 2, 512, name="tok_stage")
    tkb_r = Ring(p, 2, 512, BF16, name="tokb_stage")
    blocks = [(0, CTX, 1)] + [(CTX + 512 * b, 512, CTX + 512 * b + 3) for b in range(LAT // 512)]
    for pb in pb_r.bufs:
        p.memset("pool", pb, 0.0)
    ib = 0
    for cg in range(24):
        pbuf = pb_r.next()
        for (t0, n, c0) in blocks:
            ps = p.psum[ib % 4]
            ib += 1
            for k in range(8):
                p.mm(ps[:, 0:n], wb3[:, k, cg * 128:(cg + 1) * 128], hT3[:, k, t0:t0 + n], start=(k == 0), stop=(k == 7))
            p.copy("act" if ib % 2 == 0 else "dve", pbuf[:, c0:c0 + n], ps[:, 0:n])
        cv = cv_r.next()
        for (t0, n) in SEQS:
            c0 = t0 + 1 if t0 == 0 else t0 + 3
            p.act(cv[:, t0:t0 + n], pbuf[:, c0:c0 + n], AF.Copy, scale=cw[:, cg * 3 + 1:cg * 3 + 2])
            p.stt("dve", cv[:, t0:t0 + n], pbuf[:, c0 - 1:c0 - 1 + n], cw[:, cg * 3:cg * 3 + 1], cv[:, t0:t0 + n], ALU.mult, ALU.add)
            p.stt("dve", cv[:, t0:t0 + n], pbuf[:, c0 + 1:c0 + 1 + n], cw[:, cg * 3 + 2:cg * 3 + 3], cv[:, t0:t0 + n], ALU.mult, ALU.add)
        if cg < 12:
            p.act(cv, cv, AF.Silu)
        if cg < 8:
            for (t0, n, _) in blocks:
                sq = sq_r.next()
                p.tt("pool", sq[:, 0:n], cv[:, t0:t0 + n], cv[:, t0:t0 + n], ALU.mult)
                ps = p.psum[4 + (ib % 2)]
                ib += 1
                p.mm(ps[:, 0:n], ones_f, sq[:, 0:n])
                rn = rn_r.next()
                p.act(rn[:, 0:n], ps[:, 0:n], AF.Sqrt, bias=eps_t)
                p.recip(rn[:, 0:n], rn[:, 0:n])
                if cg < 4:
                    p.stt("dve", cv[:, t0:t0 + n], cv[:, t0:t0 + n], 128.0 ** -0.5, rn[:, 0:n], ALU.mult, ALU.mult)
                else:
                    p.tt("dve", cv[:, t0:t0 + n], cv[:, t0:t0 + n], rn[:, 0:n], ALU.mult)
        if cg < 4:
            p.dma("sp", S["qT"][cg], cv)
        elif cg < 8:
            p.dma("sp", S["kT"][cg - 4], cv)
        elif 12 <= cg < 20:
            p.dma("sp", S["hyx"][cg - 12], cv)
        if 4 <= cg < 12 or cg >= 20:
            if cg < 8:
                dst, col, bf = S["k_tok"], (cg - 4) * 128, False
            elif cg < 12:
                dst, col, bf = S["v_tok"], (cg - 8) * 128, False
            else:
                dst, col, bf = S["hyv"], (cg - 20) * 128, True
            for t4 in range(0, NT_ALL, 4):
                nt = min(4, NT_ALL - t4)
                ps = p.psum[6 + (ib % 2)]
                ib += 1
                for c in range(nt):
                    p.tr(ps[:, c * 128:(c + 1) * 128], cv[:, (t4 + c) * 128:(t4 + c + 1) * 128], ident)
                st = (tkb_r if bf else tk_r).next()
                p.copy("act", st[:, 0:nt * 128], ps[:, 0:nt * 128])
                p.dma("act" if bf else "sp", dst[t4 * 128:(t4 + nt) * 128, col:col + 128].rearrange("(c p) d -> p c d", p=128),
                      st[:, 0:nt * 128].rearrange("p (c d) -> p c d", d=128))
    z_r = Ring(p, 2, 512, name="zt")
    for ti in range(NT_ALL):
        ps = p.psum[ti % 4]
        for k in range(8):
            p.mm(ps, hT3[:, k, ti * 128:(ti + 1) * 128], wb3[:, k, 3072:3584], start=(k == 0), stop=(k == 7))
        zt = z_r.next()
        p.act(zt, ps, AF.Silu)
        p.dma("sp", S["zs"][ti * 128:(ti + 1) * 128, :], zt)
    bas = p.alloc(NCH * 16, name="ba_s", parts=64)
    for c0 in range(0, NCH, 32):
        nc_ = min(32, NCH - c0)
        ps = p.psum[4 + (c0 // 32) % 2]
        for c in range(nc_):
            ch = c0 + c
            for k in range(8):
                p.mm(ps[0:64, c * 16:(c + 1) * 16], hT3[:, k, ch * 64:(ch + 1) * 64], wb3[:, k, 3584:3600], start=(k == 0), stop=(k == 7))
        p.copy("dve", bas[:, c0 * 16:(c0 + nc_) * 16], ps[0:64, 0:nc_ * 16])
    p.dma("sp", S["ba"], bas)
    p.barrier()
    p.release(mk0)


class BankRR:
    def __init__(self, p, banks):
        self.p = p
        self.banks = list(banks)
        self.i = 0

    def next(self):
        b = self.p.psum[self.banks[self.i % len(self.banks)]]
        self.i += 1
        return b


class GdnDir:
    def __init__(self, p, d):
        self.d = d
        a = lambda cols, nm, parts=64: p.alloc(cols, name=f"g{d}_{nm}", parts=parts)
        self.ktok = a(8 * 128, "ktok")
        self.vtok = a(8 * 128, "vtok")
        self.gm1 = a(512, "gm1")
        self.gm2 = a(512, "gm2")
        self.dec = a(512, "dec")
        self.decT = a(512, "decT")
        self.A = a(512, "A")
        self.P = [a(512, "Pa"), a(512, "Pb")]
        self.Q = [a(512, "Qa"), a(512, "Qb")]
        self.RT = [a(512, "RTa"), a(512, "RTb")]
        self.IP = a(512, "IP")
        self.qkdT = a(512, "qkdT")
        self.bv = a(8 * 128, "bv")
        self.bk = a(8 * 128, "bk")
        self.kd = a(8 * 128, "kd")
        self.wi = a(8 * 128, "wi")
        self.kcT = a(512, "kcT", parts=128)
        self.sc = a(64, "scal")
        self.cdb = a(8, "cdb", parts=128)
        self.S = a(128, "S", parts=128)
        self.ws = [a(128, "w_s0"), a(128, "w_s1")]
        self.tmp = [a(128, "tmp0"), a(128, "tmp1")]
        self.ob = a(8 * 128, "ob")


def phase_gdn(cx, layer, S):
    p = cx.p
    e = layer // 2
    mk0 = p.mark()
    masks_d = cx.inp("gdn_masks", [64, 256])
    ident_d = cx.inp("ident", [128, 128])
    alog_d = cx.inp("a_log", [2, 2, 4])
    dtb_d = cx.inp("dt_bias", [2, 2, 4])
    ident = p.alloc(128, name="ident")
    p.dma("act", ident, ident_d)
    id64 = ident[0:64, 0:64]
    masks = p.alloc(256, name="masks", parts=64)
    p.dma("act", masks, masks_d)
    Um, Lom, SUm, SLm = (masks[:, i * 64:(i + 1) * 64] for i in range(4))
    ones_f = p.alloc(128, name="ones_f")
    p.memset("dve", ones_f, 1.0)
    one64 = ones_f[0:64, 0:1]
    bas = p.alloc(NCH * 16, name="bas", parts=64)
    p.dma("sp", bas, S["ba"])
    bas4 = bas.rearrange("p (c t x) -> p c t x", c=NCH, t=2)
    beta = p.alloc(NCH * 8, name="beta", parts=64)
    gt = p.alloc(NCH * 8, name="gt", parts=64)
    beta3 = beta.rearrange("p (c x) -> p c x", c=NCH)
    gt3 = gt.rearrange("p (c x) -> p c x", c=NCH)
    p.act(beta3, bas4[:, :, 0, :], AF.Exp, scale=-1.0)
    p.ts("dve", beta, beta, 1.0, None, ALU.add)
    p.recip(beta, beta)
    rows = p.alloc(16, name="rows", parts=64)
    load_row_bc(p, "act", rows[:, 0:8], alog_d[e:e + 1].rearrange("a d h -> a (d h)"))
    load_row_bc(p, "act", rows[:, 8:16], dtb_d[e:e + 1].rearrange("a d h -> a (d h)"))
    p.act(rows[:, 0:8], rows[:, 0:8], AF.Exp)
    p.ts("dve", rows[:, 0:8], rows[:, 0:8], -1.0, None, ALU.mult)
    p.tt("dve", gt3, bas4[:, :, 1, :], rows[:, 8:16].unsq(1).bc([64, NCH, 8]), ALU.add)
    p.act(gt, gt, AF.Exp)
    p.act(gt, gt, AF.Ln, bias=one64)
    p.tt("dve", gt3, gt3, rows[:, 0:8].unsq(1).bc([64, NCH, 8]), ALU.mult)

    qk_r = Ring(p, 2, NTOK, name="qT_h")
    kk_r = Ring(p, 2, NTOK, name="kT_h")
    dirs = [GdnDir(p, 0), GdnDir(p, 1)]
    brr = BankRR(p, range(8))
    lat_groups = [list(range(4 + 8 * g, 12 + 8 * g)) for g in range(8)]
    groups = {0: [[0, 1, 2, 3]] + lat_groups, 1: [[0, 1, 2, 3]] + lat_groups[::-1]}
    MS = {0: (Um, SLm, SLm, SLm, Um, Um), 1: (Lom, SUm, SUm, SUm, Lom, Lom)}

    def prep(dc, h, chunks, qT, kT):
        d = dc.d
        L, R, MSk, L2, R2, MB = MS[d]
        n = len(chunks)
        c0 = chunks[0]
        W = n * 64
        x = d * 4 + h
        gcols = gt3[:, c0:c0 + n, x]
        bcols = beta3[:, c0:c0 + n, x]
        v3 = lambda t, w=64: t[:, 0:n * w].rearrange("p (c j) -> p c j", c=n)
        p.dma("sp", v3(dc.ktok, 128), S["k_tok"][c0 * 64:(c0 + n) * 64, h * 128:(h + 1) * 128].rearrange("(c p) d -> p c d", p=64))
        p.dma("act", v3(dc.vtok, 128), S["v_tok"][c0 * 64:(c0 + n) * 64, h * 128:(h + 1) * 128].rearrange("(c p) d -> p c d", p=64))
        yield
        ps_kk = brr.next()
        ps_qk = brr.next()
        for c, ch in enumerate(chunks):
            p.mm(ps_kk[0:64, c * 64:(c + 1) * 64], kT[:, ch * 64:(ch + 1) * 64], kT[:, ch * 64:(ch + 1) * 64])
            p.mm(ps_qk[0:64, c * 64:(c + 1) * 64], kT[:, ch * 64:(ch + 1) * 64], qT[:, ch * 64:(ch + 1) * 64])
        yield
        p.tt("dve", v3(dc.gm1), R.unsq(1).bc([64, n, 64]), gcols.unsq(2).bc([64, n, 64]), ALU.mult)
        p.tt("pool", v3(dc.gm2), R2.unsq(1).bc([64, n, 64]), gcols.unsq(2).bc([64, n, 64]), ALU.mult)
        yield
        ps3 = brr.next()
        ps4 = brr.next()
        p.mm(ps3[0:64, 0:W], L, dc.gm1[:, 0:W])
        p.mm(ps4[0:64, 0:W], L2, dc.gm2[:, 0:W])
        yield
        p.act(dc.dec[:, 0:W], ps3[0:64, 0:W], AF.Exp)
        p.act(dc.decT[:, 0:W], ps4[0:64, 0:W], AF.Exp)
        yield
        p.tt("dve", v3(dc.dec), v3(dc.dec), MSk.unsq(1).bc([64, n, 64]), ALU.mult)
        p.tt("pool", v3(dc.decT), v3(dc.decT), MB.unsq(1).bc([64, n, 64]), ALU.mult)
        yield
        p.tt("dve", dc.A[:, 0:W], ps_kk[0:64, 0:W], dc.dec[:, 0:W], ALU.mult)
        p.tt("dve", v3(dc.A), v3(dc.A), bcols.unsq(2).bc([64, n, 64]), ALU.mult)
        p.tt("dve", dc.qkdT[:, 0:W], ps_qk[0:64, 0:W], dc.decT[:, 0:W], ALU.mult)
        yield
        ps5 = brr.next()
        for c in range(n):
            p.tr(ps5[0:64, c * 64:(c + 1) * 64], dc.A[:, c * 64:(c + 1) * 64], id64)
        Pc, Qc, RTc = dc.A, dc.Q[0], dc.RT[0]
        p.copy("act", Qc[:, 0:W], ps5[0:64, 0:W])
        p.tt("pool", v3(RTc), id64.unsq(1).bc([64, n, 64]), v3(Qc), ALU.subtract)
        yield
        for r in range(5):
            Pn, Qn, RTn = dc.P[r % 2], dc.Q[(r + 1) % 2], dc.RT[(r + 1) % 2]
            psP = brr.next()
            for c in range(n):
                p.mm(psP[0:64, c * 64:(c + 1) * 64], Qc[:, c * 64:(c + 1) * 64], Pc[:, c * 64:(c + 1) * 64])
            if r < 4:
                psQ = brr.next()
                for c in range(n):
                    p.mm(psQ[0:64, c * 64:(c + 1) * 64], Pc[:, c * 64:(c + 1) * 64], Qc[:, c * 64:(c + 1) * 64])
            p.copy("act", Pn[:, 0:W], psP[0:64, 0:W])
            if r < 4:
                p.copy("dve", Qn[:, 0:W], psQ[0:64, 0:W])
            p.tt("pool", v3(dc.IP), v3(Pn), id64.unsq(1).bc([64, n, 64]), ALU.add)
            yield
            psR = brr.next()
            for c in range(n):
                p.mm(psR[0:64, c * 64:(c + 1) * 64], dc.IP[:, c * 64:(c + 1) * 64], RTc[:, c * 64:(c + 1) * 64])
            p.copy("dve", RTn[:, 0:W], psR[0:64, 0:W])
            yield
            Pc, Qc, RTc = Pn, Qn, RTn
        psG = brr.next()
        p.mm(psG[0:64, 0:n], L, gcols)
        p.mm(psG[0:128, 8:8 + n], ones_f[0:64, :], gcols)
        Gs, eG, kds, bke = (dc.sc[:, i * 8:i * 8 + n] for i in range(4))
        p.copy("dve", Gs, psG[0:64, 0:n])
        p.act(eG, psG[0:64, 0:n], AF.Exp)
        p.act(dc.cdb[:, 0:n], psG[0:128, 8:8 + n], AF.Exp)
        p.tt("dve", kds, psG[0:64, 8:8 + n], Gs, ALU.subtract)
        p.act(kds, kds, AF.Exp)
        p.tt("dve", bke, bcols, eG, ALU.mult)
        yield
        p.tt("pool", v3(dc.bv, 128), v3(dc.vtok, 128), bcols.unsq(2).bc([64, n, 128]), ALU.mult)
        p.tt("dve", v3(dc.bk, 128), v3(dc.ktok, 128), bke.unsq(2).bc([64, n, 128]), ALU.mult)
        p.tt("pool", v3(dc.kd, 128), v3(dc.ktok, 128), kds.unsq(2).bc([64, n, 128]), ALU.mult)
        yield
        for half in range(0, n, 4):
            psW = brr.next()
            m = min(4, n - half)
            for c in range(half, half + m):
                p.mm(psW[0:64, (c - half) * 128:(c - half + 1) * 128], RTc[:, c * 64:(c + 1) * 64], dc.bv[:, c * 128:(c + 1) * 128])
            p.copy("act", dc.wi[:, half * 128:(half + m) * 128], psW[0:64, 0:m * 128])
        psK = brr.next()
        for c in range(n):
            p.mm(psK[0:128, c * 64:(c + 1) * 64], dc.bk[:, c * 128:(c + 1) * 128], RTc[:, c * 64:(c + 1) * 64])
        p.copy("dve", dc.kcT[:, 0:W], psK[0:128, 0:W])
        yield

    def scan_step(dc, ci, ch, qT, it):
        psA = brr.next()
        p.mm(psA[0:64, 0:128], dc.kcT[:, ci * 64:(ci + 1) * 64], dc.S)
        ws = dc.ws[it % 2]
        p.tt("dve", ws, dc.wi[:, ci * 128:(ci + 1) * 128], psA[0:64, 0:128], ALU.subtract)
        psO = brr.next()
        p.mm(psO[0:64, 0:128], qT[:, ch * 64:(ch + 1) * 64], dc.S)
        p.mm(psO[0:64, 128:256], dc.qkdT[:, ci * 64:(ci + 1) * 64], ws)
        tmp = dc.tmp[it % 2]
        p.act(tmp, psO[0:64, 0:128], AF.Copy, scale=dc.sc[:, 8 + ci:8 + ci + 1])
        p.tt("dve", dc.ob[:, ci * 128:(ci + 1) * 128], tmp, psO[0:64, 128:256], ALU.add)
        psS = brr.next()
        p.mm(psS[0:128, 0:128], dc.kd[:, ci * 128:(ci + 1) * 128], ws)
        p.stt("dve", dc.S, dc.S, dc.cdb[:, ci:ci + 1], psS[0:128, 0:128], ALU.mult, ALU.add)

    it = 0
    for h in range(4):
        qT = qk_r.next()
        kT = kk_r.next()
        p.dma("sp", qT, S["qT"][h])
        p.dma("act", kT, S["kT"][h])
        for dc in dirs:
            p.memset("pool", dc.S, 0.0)
        for gi in range(9):
            gens = [prep(dc, h, groups[dc.d][gi], qT, kT) for dc in dirs]
            live = list(gens)
            while live:
                for g_ in list(live):
                    try:
                        next(g_)
                    except StopIteration:
                        live.remove(g_)
            n = len(groups[0][gi])
            for s in range(n):
                for dc in dirs:
                    chunks = groups[dc.d][gi]
                    ci = s if dc.d == 0 else n - 1 - s
                    scan_step(dc, ci, chunks[ci], qT, it)
                it += 1
            for dc in dirs:
                chunks = groups[dc.d][gi]
                c0 = chunks[0]
                dst = S["o_f"] if dc.d == 0 else S["o_b"]
                p.dma("sp" if dc.d == 0 else "act",
                      dst[c0 * 64:(c0 + n) * 64, h * 128:(h + 1) * 128].rearrange("(c p) d -> p c d", p=64),
                      dc.ob[:, 0:n * 128].rearrange("p (c d) -> p c d", c=n))
    p.barrier()
    p.release(mk0)


def phase_gdn_out(cx, layer, S):
    p = cx.p
    e = layer // 2
    mk0 = p.mark()
    ident_d = cx.inp("ident", [128, 128])
    gain_d = cx.inp("gdn_gain", [2, 128])
    ident = p.alloc(128, name="ident")
    p.dma("act", ident, ident_d)
    gain = p.alloc(128, name="gain")
    load_row_bc(p, "act", gain, gain_d[e:e + 1, :])
    eps_t = p.alloc(1, name="eps")
    p.memset("dve", eps_t, EPS)
    of_r = Ring(p, 2, 512, name="of")
    ob_r = Ring(p, 2, 512, name="ob")
    z_r = Ring(p, 2, 512, name="zz")
    sq_r = Ring(p, 2, 512, name="sq")
    st_r = Ring(p, 2, 8, name="st")
    tb_r = Ring(p, 2, 512, BF16, name="tb")
    for ti in range(NT_ALL):
        of, ob, zz = of_r.next(), ob_r.next(), z_r.next()
        rows = slice(ti * 128, (ti + 1) * 128)
        p.dma("sp", of, S["o_f"][rows, :])
        p.dma("act", ob, S["o_b"][rows, :])
        p.dma("sp", zz, S["zs"][rows, :])
        p.tt("dve", of, of, ob, ALU.add)
        sq = sq_r.next()
        p.tt("pool", sq, of, of, ALU.mult)
        st = st_r.next()
        p.reduce("dve", st[:, 0:4], sq.rearrange("p (h d) -> p h d", h=4), ALU.add, AX.X)
        p.act(st[:, 4:8], st[:, 0:4], AF.Sqrt, scale=1.0 / 128, bias=eps_t)
        p.recip(st[:, 4:8], st[:, 4:8])
        o3 = of.rearrange("p (h d) -> p h d", h=4)
        p.tt("dve", o3, o3, st[:, 4:8].unsq(2).bc([128, 4, 128]), ALU.mult)
        p.tt("pool", o3, o3, gain.unsq(1).bc([128, 4, 128]), ALU.mult)
        p.tt("dve", of, of, zz, ALU.mult)
        ps = p.psum[ti % 4]
        for c in range(4):
            p.tr(ps[:, c * 128:(c + 1) * 128], of[:, c * 128:(c + 1) * 128], ident)
        tb = tb_r.next()
        p.copy("act", tb, ps)
        p.dma("act", S["actT"][0:4, :, ti * 128:(ti + 1) * 128].rearrange("g p t -> p g t"), tb.rearrange("p (g t) -> p g t", g=4))
    p.barrier()
    p.release(mk0)


import math as _math
import ml_dtypes as _mld

_TABLE_CACHE = {}


def dft_tables(L):
    if L in _TABLE_CACHE:
        return _TABLE_CACHE[L]
    N = 2 * L
    TB = min(512, L)
    n_t = L // 128
    t = np.arange(L, dtype=np.float64)[:, None]
    f = np.arange(L, dtype=np.float64)[None, :]
    ang = (2.0 * np.pi / N) * ((f + 0.5) * t)
    out = {}
    for nm, M in (("C", np.cos(ang)), ("S", np.sin(ang))):
        M = M.astype(np.float32)
        fw = M.reshape(n_t, 128, n_t, 128).transpose(2, 1, 0, 3).reshape(n_t, 128, n_t * 128)
        inv = M.reshape(L // TB, TB, n_t, 128).transpose(0, 3, 2, 1).reshape(L // TB, 128, n_t * TB)
        out[nm + "fw"] = np.ascontiguousarray(fw).astype(_mld.bfloat16)
        out[nm + "inv"] = np.ascontiguousarray(inv).astype(_mld.bfloat16)
    _TABLE_CACHE[L] = out
    return out


def hyena_consts(L):
    f32 = np.float32
    t = np.linspace(0.0, 1.0, L, dtype=f32)
    bands = 16
    wpos = (2.0 * _math.pi * np.arange(L, dtype=f32) / L).astype(f32)
    fb = np.linspace(1e-4, bands - 1, bands, dtype=f32)
    z = np.concatenate([t[:, None], np.cos(wpos[:, None] * fb), -np.sin(wpos[:, None] * fb)], axis=-1).astype(f32)
    mn = _math.log(1e-2) / 1.5
    mx = _math.log(1e-2) / 0.3
    deltas = np.linspace(mn, mx, 512, dtype=f32)
    win = np.exp(-t[:, None] * np.abs(deltas)).astype(f32)
    return np.ascontiguousarray(z.T), win


def phase_hyena(cx, layer, S):
    p = cx.p
    e = layer // 2
    mk0 = p.mark()
    ident_d = cx.inp("ident", [128, 128])
    ident = p.alloc(128, name="ident")
    p.dma("act", ident, ident_d)
    w1_d = cx.inp("hf_w1", [2, 33, 64])
    w2_d = cx.inp("hf_w2", [2, 64, 64])
    w3_d = cx.inp("hf_w3", [2, 64, 64])
    w4_d = cx.inp("hf_w4", [2, 64, 2048])
    vec_d = {n: cx.inp(n, [2, 64]) for n in ("hf_b1", "hf_b2", "hf_b3", "hf_freq")}
    hyb_d = cx.inp("hy_bias", [2, 2, 512])
    PI = _math.pi

    w1 = p.alloc(64, name="w1", parts=33)
    w2 = p.alloc(64, name="w2", parts=64)
    w3 = p.alloc(64, name="w3", parts=64)
    w4 = p.alloc(2048, name="w4", parts=64)
    p.dma("sp", w1, w1_d[e])
    p.dma("sp", w2, w2_d[e])
    p.dma("sp", w3, w3_d[e])
    p.dma("sp", w4, w4_d[e])
    vec = p.alloc(8, name="hvec", parts=64)
    for i, n in enumerate(("hf_b1", "hf_b2", "hf_b3", "hf_freq")):
        p.dma("act", vec[:, i:i + 1], vec_d[n][e].rearrange("(p o) -> p o", o=1))
    for i in range(3):
        p.tt("dve", vec[:, 4 + i:5 + i], vec[:, i:i + 1], vec[:, 3:4], ALU.mult)
    freq = vec[:, 3:4]
    brow = p.alloc(1024, name="hybias", parts=1)
    p.dma("act", brow, hyb_d[e:e + 1].rearrange("a o c -> a (o c)"))

    for (t0, L) in SEQS:
        N = 2 * L
        TB = min(512, L)
        n_t = L // 128
        n_tb = L // TB
        tabs = {k: cx.inp(f"dft{L}_{k}", list(shp), BF16) for k, shp in
                (("Cfw", (n_t, 128, n_t * 128)), ("Sfw", (n_t, 128, n_t * 128)),
                 ("Cinv", (n_tb, 128, n_t * TB)), ("Sinv", (n_tb, 128, n_t * TB)))}
        zT_d = cx.inp(f"hy_zT{L}", [33, L])
        win_d = cx.inp(f"hy_win{L}", [L, 512])
        fa_d = p.dram(f"e{layer}_fa{L}", [4, L, 512], BF16)
        spec_d = p.dram(f"e{layer}_spec{L}", [4, L, 512], F32)
        S[f"fa{L}"] = fa_d
        S[f"spec{L}"] = spec_d
        mk = p.mark()
        h3 = p.alloc(L, name="h3", parts=64)
        zr = Ring(p, 2, TB, name="zTt", parts=33)
        hr = Ring(p, 4, TB, name="hh", parts=64)
        tr_ = Ring(p, 2, TB, name="wrapt", parts=64)

        def sin_layer(dst, ps, fbcol, n):
            y = hr.next()
            p.ts("dve", y[:, 0:n], ps[0:64, 0:n], freq, fbcol, ALU.mult, ALU.add)
            for _ in range(2):
                t = tr_.next()
                p.ts("dve", t[:, 0:n], y[:, 0:n], PI, -2.0 * PI, ALU.is_gt, ALU.mult)
                p.tt("dve", y[:, 0:n], y[:, 0:n], t[:, 0:n], ALU.add)
                t = tr_.next()
                p.ts("dve", t[:, 0:n], y[:, 0:n], -PI, 2.0 * PI, ALU.is_lt, ALU.mult)
                p.tt("dve", y[:, 0:n], y[:, 0:n], t[:, 0:n], ALU.add)
            p.act(dst, y[:, 0:n], AF.Sin)

        for b in range(n_tb):
            zt = zr.next()
            p.dma("sp", zt, zT_d[:, b * TB:(b + 1) * TB])
            ps = p.psum[b % 2]
            p.mm(ps[0:64, 0:TB], w1, zt)
            h1 = hr.next()
            sin_layer(h1, ps, vec[:, 4:5], TB)
            ps = p.psum[2 + b % 2]
            p.mm(ps[0:64, 0:TB], w2, h1)
            h2 = hr.next()
            sin_layer(h2, ps, vec[:, 5:6], TB)
            ps = p.psum[4 + b % 2]
            p.mm(ps[0:64, 0:TB], w3, h2)
            sin_layer(h3[:, b * TB:(b + 1) * TB], ps, vec[:, 6:7], TB)
        win_r = Ring(p, 2, 512, name="win")
        hf_r = Ring(p, 2, 512, name="hf")
        hb_r = Ring(p, 2, 512, name="hb")
        ad_r = Ring(p, 4, 512, BF16, name="ad")
        for tt_ in range(n_t):
            wt = win_r.next()
            p.dma("sp", wt, win_d[tt_ * 128:(tt_ + 1) * 128, :])
            for o in range(2):
                psf = p.psum[(2 * o) % 4]
                psb = p.psum[(2 * o + 1) % 4]
                p.mm(psf, h3[:, tt_ * 128:(tt_ + 1) * 128], w4[:, (2 * o) * 512:(2 * o + 1) * 512])
                p.mm(psb, h3[:, tt_ * 128:(tt_ + 1) * 128], w4[:, (2 * o + 1) * 512:(2 * o + 2) * 512])
                hf, hb = hf_r.next(), hb_r.next()
                p.tt("dve", hf, psf, wt, ALU.mult)
                p.tt("dve", hb, psb, wt, ALU.mult)
                if tt_ == 0:
                    p.tt("dve", hf[0:1, :], hf[0:1, :], brow[0:1, o * 512:(o + 1) * 512], ALU.add)
                    p.memset("dve", hb[0:1, :], 0.0)
                a, d = ad_r.next(), ad_r.next()
                p.tt("pool", a, hf, hb, ALU.add)
                p.tt("pool", d, hb, hf, ALU.subtract)
                p.dma("sp", fa_d[2 * o, tt_ * 128:(tt_ + 1) * 128, :], a)
                p.dma("act", fa_d[2 * o + 1, tt_ * 128:(tt_ + 1) * 128, :], d)
        p.barrier()
        p.release(mk)

        U = p.alloc(n_t * 512, BF16, name="U")
        U3 = U.rearrange("p (t c) -> p t c", c=512)
        Yr = p.alloc(n_t * 512, BF16, name="Yr")
        Yi = p.alloc(n_t * 512, BF16, name="Yi")
        Yr3 = Yr.rearrange("p (t c) -> p t c", c=512)
        Yi3 = Yi.rearrange("p (t c) -> p t c", c=512)
        FQ = min(8, n_t)
        tab_r = Ring(p, 4, max(n_t * 128, FQ * TB), BF16, name="tab")
        sp_r = Ring(p, 4, 512, name="spec")
        x_r = Ring(p, 4, 512, name="xcs")
        t_r = Ring(p, 4, 512, name="ytmp")
        xt_r = Ring(p, 2, TB, name="xT")
        zo_r = Ring(p, 2, TB, name="zo")
        zb_r = Ring(p, 2, TB, BF16, name="zob")

        def load_U(src_rows):
            p.dma("sp", U3, src_rows.rearrange("(t p) c -> p t c", p=128))

        def fwd_pass(which, consumer):
            for ft in range(n_t):
                pss = {}
                for i, w in enumerate(which):
                    tb_ = tab_r.next()
                    p.dma("sp" if i == 0 else "act", tb_[:, 0:n_t * 128], tabs[w + "fw"][ft])
                    ps = p.psum[(2 * ft + i) % 4]
                    for tc in range(n_t):
                        p.mm(ps, tb_[:, tc * 128:(tc + 1) * 128], U3[:, tc, :], start=(tc == 0), stop=(tc == n_t - 1))
                    pss[w] = ps
                consumer(ft, pss)

        for o in range(2):
            for j, w in enumerate(("C", "S")):
                load_U(fa_d[2 * o + j])

                def store_spec(ft, pss, j=j, w=w, o=o):
                    st = sp_r.next()
                    p.act(st, pss[w], AF.Copy, scale=2.0 / N)
                    p.dma("act", spec_d[2 * o + j, ft * 128:(ft + 1) * 128, :], st)
                fwd_pass((w,), store_spec)
        for o in range(2):
            if o == 0:
                load_U(S["hyv"][t0:t0 + L, :])

            def make_Y(ft, pss, o=o):
                ac, ds = sp_r.next(), sp_r.next()
                p.dma("sp", ac, spec_d[2 * o, ft * 128:(ft + 1) * 128, :])
                p.dma("act", ds, spec_d[2 * o + 1, ft * 128:(ft + 1) * 128, :])
                xc, xs_ = x_r.next(), x_r.next()
                p.copy("act", xc, pss["C"])
                p.copy("act", xs_, pss["S"])
                t1, t2 = t_r.next(), t_r.next()
                p.tt("pool", t1, xc, ac, ALU.mult)
                p.tt("dve", t2, xs_, ds, ALU.mult)
                p.tt("dve", Yr3[:, ft, :], t1, t2, ALU.add)
                t3, t4 = t_r.next(), t_r.next()
                p.tt("pool", t3, xs_, ac, ALU.mult)
                p.tt("dve", t4, xc, ds, ALU.mult)
                p.tt("pool", Yi3[:, ft, :], t3, t4, ALU.subtract)
            fwd_pass(("C", "S"), make_Y)
            for tb in range(n_tb):
                for fq in range(n_t // FQ):
                    ct, st_ = tab_r.next(), tab_r.next()
                    p.dma("sp", ct[:, 0:FQ * TB], tabs["Cinv"][tb][:, fq * FQ * TB:(fq + 1) * FQ * TB])
                    p.dma("act", st_[:, 0:FQ * TB], tabs["Sinv"][tb][:, fq * FQ * TB:(fq + 1) * FQ * TB])
                    for cgp in range(4):
                        ps = p.psum[4 + cgp]
                        for j in range(FQ):
                            fc = fq * FQ + j
                            p.mm(ps[:, 0:TB], Yr3[:, fc, cgp * 128:(cgp + 1) * 128], ct[:, j * TB:(j + 1) * TB],
                                 start=(fc == 0), stop=False)
                            p.mm(ps[:, 0:TB], Yi3[:, fc, cgp * 128:(cgp + 1) * 128], st_[:, j * TB:(j + 1) * TB],
                                 start=False, stop=(fc == n_t - 1))
                for cgp in range(4):
                    ps = p.psum[4 + cgp]
                    xT = xt_r.next()
                    p.dma("sp", xT, S["hyx"][o * 4 + cgp, :, t0 + tb * TB:t0 + (tb + 1) * TB])
                    if o == 0:
                        zo = zo_r.next()
                        p.tt("dve", zo, ps[:, 0:TB], xT, ALU.mult)
                        pst = p.psum[cgp % 2]
                        nb = TB // 128
                        for b in range(nb):
                            p.tr(pst[:, b * 128:(b + 1) * 128], zo[:, b * 128:(b + 1) * 128], ident)
                        p.copy("act", U3[:, tb * nb:(tb + 1) * nb, cgp * 128:(cgp + 1) * 128],
                               pst[:, 0:nb * 128].rearrange("p (b c) -> p b c", b=nb))
                    else:
                        zb = zb_r.next()
                        p.tt("dve", zb, ps[:, 0:TB], xT, ALU.mult)
                        p.dma("act", S["actT"][4 + cgp, :, t0 + tb * TB:t0 + (tb + 1) * TB], zb)
        p.barrier()
        p.release(mk)
    p.release(mk0)


def phase_even_out(cx, layer, xs, m_scr, S):
    p = cx.p
    e = layer // 2
    mk0 = p.mark()
    w_out = cx.inp("w_out_even", [2, 1024, 1024])
    actT = p.alloc(8 * NTOK, BF16, name="actT")
    aT3 = actT.rearrange("p (g t) -> p g t", g=8)
    for g in range(8):
        p.dma("sp" if g % 2 == 0 else "act", aT3[:, g, :], S["actT"][g])
    wo = p.alloc(8 * 1024, BF16, name="wo_b")
    wo3 = wo.rearrange("p (h c) -> p h c", h=8)
    stg = Ring(p, 2, 8 * 512, name="wstage2")
    cast_weight(p, wo3, w_out[e].rearrange("(h p) c -> p h c", p=128), 1024, stg)
    G1 = {}
    for v in (0, 1):
        G1[v] = p.alloc(1024, name="G1")
        load_row_bc(p, "act", G1[v], m_scr[layer, v:v + 1, 2 * 1024:3 * 1024])
    xr = Ring(p, 2, 1024, name="xres")
    yr = Ring(p, 2, 1024, name="yres")
    for ti in range(NT_ALL):
        v = 1 if ti < 2 else 0
        xt = xr.next()
        p.dma("sp", xt, xs[ti * 128:(ti + 1) * 128, :])
        yt = yr.next()
        for cb in range(2):
            ps = p.psum[(ti * 2 + cb) % 4]
            for h in range(8):
                p.mm(ps, aT3[:, h, ti * 128:(ti + 1) * 128], wo3[:, h, cb * 512:(cb + 1) * 512], start=(h == 0), stop=(h == 7))
            p.tt("dve", yt[:, cb * 512:(cb + 1) * 512], ps, G1[v][:, cb * 512:(cb + 1) * 512], ALU.mult)
        p.tt("pool", yt, yt, xt, ALU.add)
        p.dma("act", xs[ti * 128:(ti + 1) * 128, :], yt)
    p.barrier()
    p.release(mk0)


def phase_final(cx, xs, out):
    p = cx.p
    mk0 = p.mark()
    fn_d = cx.inp("final_norm", [1, 1024])
    g = p.alloc(1024, name="fn_row")
    load_row_bc(p, "act", g, fn_d[0:1, :])
    eps_t = p.alloc(1, name="eps")
    p.memset("dve", eps_t, EPS)
    xr = Ring(p, 2, 1024, name="xt")
    yr = Ring(p, 2, 1024, name="yt")
    junk = p.alloc(1024, name="junk")
    st_r = Ring(p, 2, 4, name="st")
    for ti in range(2, NT_ALL):
        xt = xr.next()
        p.dma("sp", xt, xs[ti * 128:(ti + 1) * 128, :])
        st = st_r.next()
        p.act(junk, xt, AF.Square, accum=st[:, 0:1])
        p.act(st[:, 1:2], st[:, 0:1], AF.Sqrt, scale=1.0 / D, bias=eps_t)
        p.recip(st[:, 2:3], st[:, 1:2])
        yt = yr.next()
        p.stt("dve", yt, xt, st[:, 2:3], g, ALU.mult, ALU.mult)
        p.dma("act", out[(ti - 2) * 128:(ti - 1) * 128, :], yt)
    p.barrier()
    p.release(mk0)


def build_full(cx, xs, m_scr, out, layers=(0, 1, 2, 3)):
    p = cx.p
    for layer in layers:
        last = layer == 3
        phase_mod(cx, layer, m_scr)
        if layer % 2 == 0:
            S = even_scratch(p, layer)
            phase_even_proj(cx, layer, xs, m_scr, S)
            phase_gdn(cx, layer, S)
            phase_gdn_out(cx, layer, S)
            phase_hyena(cx, layer, S)
            phase_even_out(cx, layer, xs, m_scr, S)
        else:
            phase_odd(cx, layer, xs, m_scr, not last)
        phase_peer(cx, layer, xs, m_scr, list(range(2 if last else 0, NT_ALL)))
    phase_final(cx, xs, out)


_BUILD_CACHE = {}


def kernel(**inputs):
    inputs = {k: np.asarray(v) for k, v in inputs.items()}
    if "full" not in _BUILD_CACHE:
        _BUILD_CACHE["full"] = build({})
    cx, nc = _BUILD_CACHE["full"]
    n = 8
    in_maps = [host_inputs(cx, inputs, b) for b in range(n)]
    res = run_bass_kernel_spmd(nc, in_maps, core_ids=list(range(n)))
    return np.stack([np.asarray(res.results[b]["out"]) for b in range(n)], axis=0).astype(np.float32)
```
